# Optimizing a Trainium2 kernel written in Bass

```python
import math
import jax, jax.numpy as jnp
from jax import lax
import numpy as np

D_MODEL = 1024
BATCH = 32
SEQ = 2048
DEPTH = 1

H_A = 4
DK_A = 128
DV_A = 128
W_A = H_A * DV_A
CONV_W = 4
CHUNK = 64
H_B = 4
R_KV = 128
DV_B = 128
W_B = H_B * DV_B
H_IDX = 8
D_IDX = 64
TOPK_MAX = 256
Q_BLOCK = 128
N_BUCKETS = 32
MAX_EXACT = 16
MAX_DIST = 128
EPS = 1e-6
SPLITS = (3 * W_A, W_A, H_A, H_A, H_B * R_KV, R_KV, W_B, H_IDX * D_IDX, D_IDX, H_IDX)
SPLIT_OFFSETS = (3072 // 2, 2048, 2052, 2056, 2568, 2696, 3208, 3720, 3784)
D_IN = 3792

kernel_name = 'hybrid_gdn_dsa_parallel_heads'


def rmsnorm(x, g):
    xf = x.astype(jnp.float32)
    xf = xf * lax.rsqrt(jnp.mean(xf * xf, axis=-1, keepdims=True) + EPS)
    return xf.astype(x.dtype) * g


def l2norm(x):
    return x * lax.rsqrt(jnp.sum(x * x, axis=-1, keepdims=True) + EPS)


def causal_conv(u, w):
    L = u.shape[1]
    up = jnp.pad(u, ((0, 0), (CONV_W - 1, 0), (0, 0)))
    out = up[:, 0:L] * w[0]
    for j in range(1, CONV_W):
        out = out + up[:, j:j + L] * w[j]
    return out


def t5_bucket(dist):
    n = jnp.maximum(dist, 0)
    nf = jnp.maximum(n, 1).astype(jnp.float32)
    large = MAX_EXACT + (jnp.log(nf / MAX_EXACT) / math.log(MAX_DIST / MAX_EXACT)
                         * (N_BUCKETS - MAX_EXACT)).astype(jnp.int32)
    large = jnp.minimum(large, N_BUCKETS - 1)
    return jnp.where(n < MAX_EXACT, n, large)


def gated_deltanet(qkv, z, b, a, conv_w, a_log, dt_bias, g_norm):
    Bsz, L, _ = qkv.shape
    n_chunks = L // CHUNK
    qkv = jax.nn.silu(causal_conv(qkv, conv_w))
    q, k, v = jnp.split(qkv, 3, axis=-1)

    def heads(t, d):
        t = t.reshape(Bsz, L, H_A, d).transpose(0, 2, 1, 3).astype(jnp.float32)
        return t.reshape(Bsz, H_A, n_chunks, CHUNK, d)

    q = l2norm(heads(q, DK_A)) * (DK_A ** -0.5)
    k = l2norm(heads(k, DK_A))
    v = heads(v, DV_A)
    beta = jax.nn.sigmoid(b.astype(jnp.float32)).transpose(0, 2, 1).reshape(Bsz, H_A, n_chunks, CHUNK)
    g = -jnp.exp(a_log.astype(jnp.float32)) * jax.nn.softplus(a.astype(jnp.float32) + dt_bias.astype(jnp.float32))
    g = g.transpose(0, 2, 1).reshape(Bsz, H_A, n_chunks, CHUNK)
    gc = jnp.cumsum(g, axis=-1)
    pos = jnp.arange(CHUNK)
    causal = pos[:, None] >= pos[None, :]
    strict = pos[:, None] > pos[None, :]
    decay = jnp.exp(jnp.where(causal, gc[..., :, None] - gc[..., None, :], -jnp.inf))
    kb = k * beta[..., None]
    vb = v * beta[..., None]
    lmat = jnp.where(strict, jnp.einsum('bhnik,bhnjk->bhnij', kb, k) * decay, 0.0)
    eye = jnp.eye(CHUNK, dtype=jnp.float32)
    tmat = lax.linalg.triangular_solve(lmat + eye, jnp.broadcast_to(eye, lmat.shape),
                                       left_side=True, lower=True, unit_diagonal=True)
    u = jnp.einsum('bhnij,bhnjd->bhnid', tmat, vb)
    w = jnp.einsum('bhnij,bhnjd->bhnid', tmat, kb * jnp.exp(gc)[..., None])
    attn = jnp.einsum('bhnik,bhnjk->bhnij', q, k) * decay
    qg = q * jnp.exp(gc)[..., None]
    kg = k * jnp.exp(gc[..., -1:] - gc)[..., None]
    glast = jnp.exp(gc[..., -1])
    xs = tuple(jnp.moveaxis(t, 2, 0) for t in (qg, kg, u, w, attn, glast))

    def step(S, inp):
        qg_c, kg_c, u_c, w_c, attn_c, gl_c = inp
        v_new = u_c - jnp.einsum('bhck,bhkv->bhcv', w_c, S)
        o = jnp.einsum('bhck,bhkv->bhcv', qg_c, S) + jnp.einsum('bhij,bhjv->bhiv', attn_c, v_new)
        S = S * gl_c[..., None, None] + jnp.einsum('bhck,bhcv->bhkv', kg_c, v_new)
        return S, o

    S0 = jnp.zeros((Bsz, H_A, DK_A, DV_A), jnp.float32)
    _, o = lax.scan(step, S0, xs)
    o = jnp.moveaxis(o, 0, 2).reshape(Bsz, H_A, L, DV_A).transpose(0, 2, 1, 3)
    o = rmsnorm(o, g_norm.astype(jnp.float32))
    o = o.reshape(Bsz, L, W_A) * jax.nn.silu(z.astype(jnp.float32))
    return o.astype(qkv.dtype)


def dsa_sparse_attention(q_b, ckv, iq, ik, iw, g_kv, w_uv, rel_bias, z):
    Bsz, L, _ = q_b.shape
    n_blocks = L // Q_BLOCK
    k_top = min(TOPK_MAX, L // 4)
    ckv = rmsnorm(ckv, g_kv)
    q_b = q_b.reshape(Bsz, L, H_B, R_KV)
    iq = iq.reshape(Bsz, L, H_IDX, D_IDX)
    iw = iw * (H_IDX ** -0.5 * D_IDX ** -0.5)
    key_pos = jnp.arange(L, dtype=jnp.int32)

    def to_blocks(t):
        return jnp.moveaxis(t.reshape((Bsz, n_blocks, Q_BLOCK) + t.shape[2:]), 1, 0)

    def block(args):
        qb, iqb, iwb, t0 = args
        t = t0 + jnp.arange(Q_BLOCK, dtype=jnp.int32)
        rel = jnp.einsum('bqhd,bsd->bqhs', iqb, ik)
        score = jnp.einsum('bqh,bqhs->bqs', iwb.astype(jnp.float32), jax.nn.relu(rel).astype(jnp.float32))
        score = jnp.where(key_pos[None, None, :] <= t[None, :, None], score, -jnp.inf)
        _, sel = lax.top_k(score, k_top)
        kv_sel = jax.vmap(lambda kv, i: kv[i])(ckv, sel)
        dist = t[None, :, None] - sel
        bias = jnp.moveaxis(rel_bias[t5_bucket(dist)], 3, 2)
        logits = (jnp.einsum('bqhr,bqkr->bqhk', qb, kv_sel).astype(jnp.float32) * (R_KV ** -0.5)
                  + bias.astype(jnp.float32))
        logits = jnp.where((dist >= 0)[:, :, None, :], logits, -jnp.inf)
        p = jax.nn.softmax(logits, axis=-1).astype(kv_sel.dtype)
        return jnp.einsum('bqhk,bqkr->bqhr', p, kv_sel)

    t0s = jnp.arange(n_blocks, dtype=jnp.int32) * Q_BLOCK
    o = lax.map(block, (to_blocks(q_b), to_blocks(iq), to_blocks(iw), t0s))
    o = jnp.moveaxis(o, 0, 1).reshape(Bsz, L, H_B, R_KV)
    o = jnp.einsum('blhr,hrd->blhd', o, w_uv).reshape(Bsz, L, W_B)
    return o * jax.nn.silu(z)


def setup_inputs(seed: int = 0) -> dict:
    key = jax.random.key(seed)
    ks = jax.random.split(key, 16)
    nrm = jax.random.normal
    x = nrm(ks[0], (BATCH, SEQ, D_MODEL), jnp.float32)
    c = nrm(ks[1], (BATCH, D_MODEL), jnp.float32)
    w_ada = nrm(ks[2], (DEPTH, D_MODEL, 3 * D_MODEL), jnp.float32) * (0.5 * D_MODEL ** -0.5)
    b_ada = 0.01 * nrm(ks[3], (DEPTH, 3 * D_MODEL), jnp.float32)
    g_pre = 1.0 + 0.05 * nrm(ks[4], (DEPTH, D_MODEL), jnp.float32)
    w_in = nrm(ks[5], (DEPTH, D_MODEL, D_IN), jnp.float32) * (D_MODEL ** -0.5)
    conv_w = nrm(ks[6], (DEPTH, CONV_W, 3 * W_A), jnp.float32) * (CONV_W ** -0.5)
    a_log = jnp.log(jax.random.uniform(ks[7], (DEPTH, H_A), jnp.float32, 1.0, 16.0))
    dt = jnp.exp(jax.random.uniform(ks[8], (DEPTH, H_A), jnp.float32, math.log(1e-3), math.log(1e-1)))
    dt_bias = dt + jnp.log(-jnp.expm1(-dt))
    g_gdn = 1.0 + 0.05 * nrm(ks[9], (DEPTH, DV_A), jnp.float32)
    g_kv = 1.0 + 0.05 * nrm(ks[10], (DEPTH, R_KV), jnp.float32)
    w_uv = nrm(ks[11], (DEPTH, H_B, R_KV, DV_B), jnp.float32) * (R_KV ** -0.5)
    rel_bias = 0.5 * nrm(ks[12], (N_BUCKETS, H_B), jnp.float32)
    w_out = nrm(ks[13], (DEPTH, W_A + W_B, D_MODEL), jnp.float32) * ((W_A + W_B) ** -0.5)
    g_post = 1.0 + 0.05 * nrm(ks[14], (DEPTH, D_MODEL), jnp.float32)
    return {'x': x, 'c': c, 'w_ada': w_ada, 'b_ada': b_ada, 'g_pre': g_pre, 'w_in': w_in,
            'conv_w': conv_w, 'a_log': a_log, 'dt_bias': dt_bias, 'g_gdn': g_gdn, 'g_kv': g_kv,
            'w_uv': w_uv, 'rel_bias': rel_bias, 'w_out': w_out, 'g_post': g_post}


def reference(x, c, w_ada, b_ada, g_pre, w_in, conv_w, a_log, dt_bias, g_gdn, g_kv, w_uv, rel_bias, w_out, g_post):
    for layer in range(DEPTH):
        mod = jax.nn.silu(c) @ w_ada[layer] + b_ada[layer]
        shift, scale, gate = jnp.split(mod, 3, axis=-1)
        h = rmsnorm(x, g_pre[layer]) * (1.0 + scale[:, None, :]) + shift[:, None, :]
        proj = h @ w_in[layer]
        (qkv_a, z_a, b_a, a_a, q_b, ckv, z_b, iq, ik, iw) = jnp.split(proj, list(SPLIT_OFFSETS), axis=-1)
        o_a = gated_deltanet(qkv_a, z_a, b_a, a_a, conv_w[layer], a_log[layer], dt_bias[layer], g_gdn[layer])
        o_b = dsa_sparse_attention(q_b, ckv, iq, ik, iw, g_kv[layer], w_uv[layer], rel_bias, z_b)
        mix = jnp.concatenate([o_a, o_b], axis=-1) @ w_out[layer]
        x = x + gate[:, None, :] * rmsnorm(mix, g_post[layer])
    return x
```

```python
import math
import numpy as np
import concourse.bass as bass
import concourse.mybir as mybir
from concourse.bass_utils import run_bass_kernel_spmd

F32 = mybir.dt.float32
BF16 = mybir.dt.bfloat16
AF = mybir.ActivationFunctionType
ALU = mybir.AluOpType
AX = mybir.AxisListType

D = 1024
L = 2048
NBC = 4
NT = 16
EPS = 1e-6
NEG = -30000.0
NBIS = 16


class Res:
    __slots__ = ("name", "w", "r")

    def __init__(self, name):
        self.name = name
        self.w = None
        self.r = {}


class Buf:
    def __init__(self, t, name):
        self.t = t
        self.r = Res(name)

    def __getitem__(self, k):
        return self.t[k]


class Sched:
    def __init__(self, nc, ndma=8):
        self.nc = nc
        self.e = {"pe": nc.tensor, "act": nc.scalar, "dve": nc.vector, "pool": nc.gpsimd, "sp": nc.sync}
        self.sem = {k: nc.alloc_semaphore("sem_" + k) for k in self.e}
        self.cnt = {k: 0 for k in self.e}
        self.seen = {k: {} for k in self.e}
        self.dsem = [nc.alloc_semaphore(f"dsem{i}") for i in range(ndma)]
        self.dcnt = [0] * ndma
        self.dnext = 0
        self.nwait = 0

    def _wait(self, eng, key, val):
        if self.seen[eng].get(key, 0) >= val:
            return
        self.seen[eng][key] = val
        sem = self.sem[key] if isinstance(key, str) else self.dsem[key[1]]
        self.e[eng].wait_ge(sem, val)
        self.nwait += 1

    def _deps(self, eng, reads, writes):
        need = {}
        for b in reads:
            r = b.r
            if r.w is not None:
                k, v = r.w
                need[k] = max(need.get(k, 0), v)
        for b in writes:
            w = b.r
            if w.w is not None:
                k, v = w.w
                need[k] = max(need.get(k, 0), v)
            for k, v in w.r.items():
                need[k] = max(need.get(k, 0), v)
        for k, v in need.items():
            if eng == "pe" and k == "pe":
                continue
            self._wait(eng, k, v)

    def op(self, eng, fn, reads=(), writes=()):
        self._deps(eng, reads, writes)
        ins = fn(self.e[eng])
        self.cnt[eng] += 1
        ins.then_inc(self.sem[eng], 1)
        v = self.cnt[eng]
        for b in reads:
            b.r.r[eng] = v
        for b in writes:
            b.r.w = (eng, v)
            b.r.r = {}
        return ins

    def dma(self, out, in_, reads=(), writes=(), q="sp", **kw):
        slot = self.dnext
        self.dnext = (self.dnext + 1) % len(self.dsem)
        key = ("d", slot)
        if self.dcnt[slot] > 0:
            self._wait(q, key, self.dcnt[slot])
        self._deps(q, reads, writes)
        self.dcnt[slot] += 16
        self.e[q].dma_start(out=out, in_=in_, **kw).then_inc(self.dsem[slot], 16)
        v = self.dcnt[slot]
        for b in reads:
            b.r.r[key] = v
        for b in writes:
            b.r.w = (key, v)
            b.r.r = {}

    def finish(self, eng="sp"):
        for k in self.cnt:
            if self.cnt[k] > 0 and k != eng:
                self._wait(eng, k, self.cnt[k])
        for i, c in enumerate(self.dcnt):
            if c > 0:
                self._wait(eng, ("d", i), c)


def t5_bucket_np(n):
    n = np.maximum(n, 0)
    nf = np.maximum(n, 1).astype(np.float32)
    large = 16 + (np.log(nf / np.float32(16)) / np.float32(math.log(128 / 16)) * np.float32(16)).astype(np.int32)
    large = np.minimum(large, 31)
    return np.where(n < 16, n, large)


O_QKV, O_ZA, O_B, O_A, O_QB, O_CKV, O_ZB, O_IQ, O_IK, O_IW = 0, 1536, 2048, 2052, 2056, 2568, 2696, 3208, 3720, 3784
NW16 = 3208
NIDX = 648


def build(debug=None):
    nc = bass.Bass("TRN2", target_bir_lowering=False)
    din = lambda n, sh: nc.dram_tensor(n, sh, F32, kind="ExternalInput").ap()
    x_d = din("x", [NBC, L, D])
    cT_d = din("cT", [128, 8, NBC])
    wada_d = din("w_ada", [128, 8, 3072])
    bcol_d = din("b_col", [128, 24])
    bgate_d = din("b_gate", [1, 1024])
    gpre_d = din("g_pre", [128, 8])
    win_d = din("w_in", [128, 8, NW16])
    widx_d = din("w_idx", [128, 8, NIDX])
    conv_d = din("conv_w", [128, 12, 4])
    alog_d = din("a_log", [1, 4])
    dtb_d = din("dt_bias", [1, 4])
    ggdn_d = din("g_gdn", [1, 128])
    gkv_d = din("g_kv", [1, 128])
    wuv_d = din("w_uv", [128, 4, 128])
    rb_d = din("rel_bias", [1, 128])
    wout_d = din("w_out", [128, 8, 1024])
    gpost_d = din("g_post", [1, 1024])
    msk_d = din("masks", [128, 8, 128])
    out_d = nc.dram_tensor("out", [NBC, L, D], F32, kind="ExternalOutput").ap()
    dbg_d = {}
    if debug:
        for n, sh in debug.items():
            dbg_d[n] = nc.dram_tensor("dbg_" + n, list(sh), F32, kind="ExternalOutput").ap()

    S = Sched(nc)
    cnt = [0]

    def sb(shape, dt=F32, name=None):
        cnt[0] += 1
        name = "s_" + (name or f"t{cnt[0]}")
        return Buf(nc.alloc_sbuf_tensor(name, list(shape), dt), name)

    def ps(shape, dt=F32, name=None):
        cnt[0] += 1
        name = "p_" + (name or f"p{cnt[0]}")
        return Buf(nc.alloc_psum_tensor(name, list(shape), dt), name)

    def mm(out, lhsT, rhs, start=True, stop=True, reads=(), writes=()):
        S.op("pe", lambda e: e.matmul(out, lhsT, rhs, start=start, stop=stop), reads, writes)

    def tr(out, in_, ident, reads=(), writes=()):
        S.op("pe", lambda e: e.transpose(out, in_, ident), reads, writes)

    def act(out, in_, func, reads=(), writes=(), **kw):
        S.op("act", lambda e: e.activation(out, in_, func, **kw), reads, writes)

    def tt(eng, out, in0, in1, op, reads=(), writes=()):
        S.op(eng, lambda e: e.tensor_tensor(out, in0, in1, op=op), reads, writes)

    def ts(eng, out, in0, s1, s2, op0, op1=None, reads=(), writes=(), accum_out=None):
        if op1 is None:
            S.op(eng, lambda e: e.tensor_scalar(out, in0, s1, None, op0=op0), reads, writes)
        elif accum_out is not None:
            S.op(eng, lambda e: e.tensor_scalar(out, in0, s1, s2, op0=op0, op1=op1, accum_out=accum_out), reads, writes)
        else:
            S.op(eng, lambda e: e.tensor_scalar(out, in0, s1, s2, op0=op0, op1=op1), reads, writes)

    def stt(out, in0, scalar, in1, op0, op1, reads=(), writes=()):
        S.op("dve", lambda e: e.scalar_tensor_tensor(out, in0, scalar, in1, op0=op0, op1=op1), reads, writes)

    def cp(eng, out, in_, reads=(), writes=()):
        if eng == "act":
            S.op("act", lambda e: e.copy(out, in_), reads, writes)
        else:
            S.op(eng, lambda e: e.tensor_copy(out, in_), reads, writes)

    def dump(name, ap, buf):
        if name in dbg_d:
            S.dma(dbg_d[name], ap, reads=[buf], q="pool")

    big4 = sb([128, 1024], name="big4")
    rtmp2 = sb([128, 512], name="rtmp2")

    def view(buf, ap):
        v = Buf.__new__(Buf)
        v.t = ap
        v.r = buf.r
        return v
    io = view(big4, big4[:, 0:128])
    S.op("pool", lambda e: e.iota(io[:], [[1, 128]], base=0, channel_multiplier=-1,
                                  allow_small_or_imprecise_dtypes=True), writes=[io])
    ident = sb([128, 128], name="ident")
    ident16 = sb([128, 128], BF16, name="ident16")
    Umat = sb([128, 128], name="Umat")
    SLmat = sb([128, 128], name="SLmat")
    NEGs = sb([128, 128], name="NEGs")
    NEGsT = sb([128, 128], name="NEGsT")
    NEGC = sb([128, 128], name="NEGC")
    ones32 = sb([128, 128], name="ones32")
    ones16 = sb([128, 128], BF16, name="ones16")
    mhalf = sb([128, 8], name="mhalf")
    ts("dve", ident[:], io[:], 0.0, None, ALU.is_equal, reads=[io], writes=[ident])
    ts("dve", ident16[:], io[:], 0.0, None, ALU.is_equal, reads=[io], writes=[ident16])
    ts("dve", Umat[:], io[:], 0.0, None, ALU.is_ge, reads=[io], writes=[Umat])
    ts("dve", SLmat[:], io[:], 0.0, None, ALU.is_lt, reads=[io], writes=[SLmat])
    ts("dve", NEGs[:], io[:], 0.0, NEG, ALU.is_ge, ALU.mult, reads=[io], writes=[NEGs])
    ts("dve", NEGsT[:], io[:], 0.0, NEG, ALU.is_le, ALU.mult, reads=[io], writes=[NEGsT])
    ts("dve", NEGC[:], io[:], 0.0, -1e30, ALU.is_gt, ALU.mult, reads=[io], writes=[NEGC])
    S.op("pool", lambda e: e.memset(ones32[:], 1.0), writes=[ones32])
    S.op("pool", lambda e: e.memset(ones16[:], 1.0), writes=[ones16])
    S.op("pool", lambda e: e.memset(mhalf[:], -0.5), writes=[mhalf])
    pow2 = sb([128, NBIS + 1], name="pow2")
    for k in range(NBIS + 1):
        S.op("pool", lambda e, k=k: e.memset(pow2[:, k:k + 1], 0.5 ** (k + 1)), writes=[pow2])

    PT2a = ps([128, 512], name="PT2a")
    PT2b = ps([128, 512], name="PT2b")
    PT2h = [PT2a, PT2b]
    rotg = [0]

    def pabG():
        rotg[0] ^= 1
        return PT2a if rotg[0] else PT2b
    PA = ps([128, 512], name="PA")
    PB = ps([128, 512], name="PB")
    PC = ps([128, 512], name="PC")
    PO = ps([128, 512], name="PO")
    PO2 = ps([128, 512], name="PO2")
    PS_ = ps([128, 512], name="PS_")
    rot = [0]

    def pab():
        rot[0] ^= 1
        return PA if rot[0] else PB

    stage = [sb([128, 8, 256], name="stage0"), sb([128, 8, 256], name="stage1")]
    w16 = sb([128, 8, NW16], BF16, name="w16")
    widx = sb([128, 8, NIDX], name="widx")
    wout16 = sb([128, 8, 1024], BF16, name="wout16")
    wuv16 = sb([128, 4, 128], BF16, name="wuv16")
    S.dma(widx[:], widx_d, writes=[widx])
    si = 0
    for c0 in range(0, NW16, 256):
        w = min(256, NW16 - c0)
        st = stage[si % 2]; si += 1
        S.dma(st[:, :, 0:w], win_d[:, :, c0:c0 + w], writes=[st])
        cp("dve" if si % 2 else "pool", w16[:, :, c0:c0 + w], st[:, :, 0:w], reads=[st], writes=[w16])
    for c0 in range(0, 1024, 256):
        st = stage[si % 2]; si += 1
        S.dma(st[:], wout_d[:, :, c0:c0 + 256], writes=[st])
        cp("dve" if si % 2 else "pool", wout16[:, :, c0:c0 + 256], st[:], reads=[st], writes=[wout16])
    st = stage[si % 2]; si += 1
    S.dma(st[:, 0:4, 0:128], wuv_d, writes=[st])
    cp("dve", wuv16[:], st[:, 0:4, 0:128], reads=[st], writes=[wuv16])

    st = stage[si % 2]; si += 1
    S.dma(st[:, :, 0:128], msk_d, writes=[st])
    msk = sb([128, 8, 128], BF16, name="msk")
    cp("dve", msk[:], st[:, :, 0:128], reads=[st], writes=[msk])
    convw = sb([128, 12, 4], name="convw")
    S.dma(convw[:], conv_d, writes=[convw])
    diagw = sb([128, 12, 4, 128], BF16, name="diagw")
    for ch in range(12):
        for j in range(4):
            ts("dve" if (ch + j) % 2 else "pool", diagw[:, ch, j, :], ident[:], convw[:, ch, j:j + 1], None, ALU.mult,
               reads=[ident, convw], writes=[diagw])

    def bcast_row(src, n, name):
        t = sb([128, n], name=name)
        S.dma(t[:], src.partition_broadcast(128), writes=[t])
        return t
    alogB = bcast_row(alog_d, 4, "alogB")
    dtbB = bcast_row(dtb_d, 4, "dtbB")
    ggdnB = bcast_row(ggdn_d, 128, "ggdnB")
    gkvB = bcast_row(gkv_d, 128, "gkvB")
    rbB = bcast_row(rb_d, 128, "rbB")
    negA = sb([128, 4], name="negA")
    act(negA[:], alogB[:], AF.Exp, reads=[alogB], writes=[negA])
    ts("dve", negA[:], negA[:], -1.0, None, ALU.mult, reads=[negA], writes=[negA])
    ggdnH = ggdnB

    cT = sb([128, 8, NBC], name="cT")
    S.dma(cT[:], cT_d, writes=[cT])
    sc = sb([128, 8, NBC], name="sc")
    act(sc[:], cT[:], AF.Silu, reads=[cT], writes=[sc])
    bcol = sb([128, 24], name="bcol")
    S.dma(bcol[:], bcol_d, writes=[bcol])
    gpre = sb([128, 8], name="gpre")
    S.dma(gpre[:], gpre_d, writes=[gpre])
    shiftc = sb([128, 8, NBC], name="shiftc")
    Gc = sb([128, 8, NBC], name="Gc")
    GATE = sb([128, 1024], name="GATE")
    for n8 in range(8):
        st = stage[si % 2]; si += 1
        S.dma(st[:], wada_d[:, :, n8 * 256:(n8 + 1) * 256], writes=[st])
        for q2 in range(2):
            dch = (n8 % 4) * 2 + q2
            for kc in range(8):
                mm(PC[:, q2 * 4:q2 * 4 + 4], st[:, kc, q2 * 128:(q2 + 1) * 128], sc[:, kc, :],
                   start=(kc == 0), stop=(kc == 7), reads=[st, sc], writes=[PC])
            dst = shiftc if n8 < 4 else Gc
            ts("dve", dst[:, dch, :], PC[:, q2 * 4:q2 * 4 + 4], bcol[:, n8 * 2 + q2:n8 * 2 + q2 + 1], None, ALU.add,
               reads=[PC, bcol], writes=[dst])
    ts("dve", Gc[:], Gc[:], 1.0, None, ALU.add, reads=[Gc], writes=[Gc])
    tt("dve", Gc[:], Gc[:], gpre[:].unsqueeze(2).to_broadcast([128, 8, NBC]), ALU.mult, reads=[Gc, gpre], writes=[Gc])

    bk = t5_bucket_np(np.arange(256))
    lo_b = [int(np.argmax(bk >= b)) if (bk >= b).any() else 100000 for b in range(32)]
    rb3 = rbB[:].rearrange("p (b h) -> p b h", h=4)
    dlt = sb([128, 32, 4], name="dlt")
    cp("dve", dlt[:, 0:1, :], rb3[:, 0:1, :], reads=[rbB], writes=[dlt])
    tt("dve", dlt[:, 1:32, :], rb3[:, 1:32, :], rb3[:, 0:31, :], ALU.subtract, reads=[rbB], writes=[dlt])
    EB = [sb([128, 4, 128], BF16, name=f"EB{t}") for t in range(2)]
    EBf = view(rtmp2, rtmp2[:, 0:128])
    rtmp = sb([128, 512], name="rtmp")
    gUb = [sb([128, 128], name=f"gU{i}") for i in range(2)]
    distT = gUb[0]
    tmpb = gUb[1]
    for typ in range(2):
        ts("dve", distT[:], io[:], float(128 * typ), None, ALU.add, reads=[io], writes=[distT])
        for h in range(4):
            acc = EBf
            ts("dve", acc[:], ones32[:], dlt[:, 0, h:h + 1], rb3[:, 31, h:h + 1], ALU.mult, ALU.subtract,
               reads=[ones32, dlt, rbB], writes=[acc])
            for bb in range(1, 32):
                if lo_b[bb] > 255:
                    continue
                ts("dve", tmpb[:], distT[:], float(lo_b[bb]) - 0.5, dlt[:, bb, h:h + 1], ALU.is_ge, ALU.mult,
                   reads=[distT, dlt], writes=[tmpb])
                tt("dve", acc[:], acc[:], tmpb[:], ALU.add, reads=[acc, tmpb], writes=[acc])
            ts("dve", acc[:], acc[:], 128.0 ** 0.5, None, ALU.mult, reads=[acc], writes=[acc])
            cp("dve", EB[typ][:, h, :], acc[:], reads=[acc], writes=[EB[typ]])

    xt = sb([128, 1024], name="xt")
    junkA = big4; xs = big4; otmp = big4
    hT32 = sb([128, 8, 128], name="hT32")
    hT16 = sb([128, 8, 128], BF16, name="hT16")
    uT = sb([128, 12, 131], BF16, name="uT")
    qkvs = sb([128, 1536], BF16, name="qkvs")
    col = lambda n, name: sb([128, n], name=name)
    ssq1 = col(1, "ssq1"); rstd1 = col(1, "rstd1"); ssqP2 = col(2, "ssqP2"); ssqP = col(1, "ssqP"); rstdP = col(1, "rstdP")
    xs2 = sb([128, 512], name="xs2")
    ssq8 = col(8, "ssq8"); rs8 = col(8, "rs8")
    ba = col(8, "ba"); beta = col(4, "beta"); gcol = col(4, "gcol"); gc = col(4, "gc"); glB = col(4, "glB")
    egc = col(4, "egc"); ekg = col(4, "ekg"); egl = col(4, "egl"); tmp4 = col(4, "tmp4"); nbeta = col(4, "nbeta")
    cf = {n: col(4, "cf_" + n) for n in ("kbg", "kg", "qg")}
    khat = sb([128, 4, 128], BF16, name="khat"); qhat = sb([128, 4, 128], BF16, name="qhat")
    qg = sb([128, 4, 128], BF16, name="qg"); kbg = sb([128, 4, 128], BF16, name="kbg")
    kg = sb([128, 4, 128], BF16, name="kg"); vb = sb([128, 4, 128], BF16, name="vb")
    khT = sb([128, 4, 128], BF16, name="khT"); qhT = sb([128, 4, 128], BF16, name="qhT"); qgT = sb([128, 4, 128], BF16, name="qgT")
    Es = sb([128, 4, 128], BF16, name="Es"); EsT = sb([128, 4, 128], BF16, name="EsT")
    Xb = [sb([128, 4, 128], BF16, name="X0")]
    XTb = [sb([128, 4, 128], BF16, name="XT0")]
    Tb = [sb([128, 4, 128], BF16, name=f"T{i}") for i in range(2)]
    TTb = [sb([128, 4, 128], BF16, name=f"TT{i}") for i in range(2)]
    Wp = khat
    attnT = sb([128, 4, 128], BF16, name="attnT")
    negwT = qhat
    vnew = qg
    S32 = sb([128, 4, 128], name="S32"); S16 = sb([128, 4, 128], BF16, name="S16")
    zas = sb([128, 512], BF16, name="zas"); G1 = zas
    osq4 = col(4, "osq4"); ors4 = col(4, "ors4")
    og = sb([128, 4, 128], BF16, name="og"); ogT = sb([128, 4, 128], BF16, name="ogT")
    qbT = sb([128, 4, 128], BF16, name="qbT"); szbT = sb([128, 4, 128], BF16, name="szbT")
    iqT = sb([128, 4, 128], name="iqT"); iw = col(8, "iw")
    ckvn = sb([128, NT, 128], BF16, name="ckvn"); ckvnT = sb([128, L], BF16, name="ckvnT")
    ikTb = stage[1]
    ikT = ikTb[:].rearrange("p a b -> p (a b)")
    score = stage[0]
    scoreF = score[:].rearrange("p a b -> p (a b)")
    mk = sb([128, L], BF16, name="mk")
    junkD = mk
    junkDF = mk
    lo = col(1, "lo"); hw0 = col(1, "hw0"); hwk = col(NBIS + 1, "hwk"); nhwk = col(NBIS + 1, "nhwk"); mid = col(1, "mid"); mid2 = col(1, "mid2"); sgnc = col(1, "sgnc"); cbc = col(1, "cbc"); cntc = col(1, "cntc"); tstep = col(1, "tstep")
    Eb = [sb([128, 4, 128], BF16, name=f"Eb{i}") for i in range(2)]
    negmk = sb([128, NT, 128], BF16, name="negmk"); mkT = negmk
    obT = sb([128, 4, 128], BF16, name="obT")
    rden = rtmp; Rg = rden
    ygT = sb([128, 4, 128], BF16, name="ygT")
    msq = col(2, "msq"); mrs = col(1, "mrs")

    def rsqrt_col(dst, src, n, scale, eps):
        ts("dve", dst[:, 0:n], src[:, 0:n], scale, eps, ALU.mult, ALU.add, reads=[src], writes=[dst])
        tt("pool", dst[:, 0:n], dst[:, 0:n], mhalf[:, 0:n], ALU.pow, reads=[dst, mhalf], writes=[dst])

    def pre_gen(b, i):
        t0 = i * 128
        uc = uT
        S.dma(xt[:], x_d[b, t0:t0 + 128, :], writes=[xt])
        yield
        for hf in range(2):
            act(xs2[:], xt[:, hf * 512:(hf + 1) * 512], AF.Square, reads=[xt], writes=[xs2, ssqP2], accum_out=ssqP2[:, hf:hf + 1])
        tt("pool", ssqP[:], ssqP2[:, 0:1], ssqP2[:, 1:2], ALU.add, reads=[ssqP2], writes=[ssqP])
        rsqrt_col(rstdP, ssqP, 1, 1.0 / D, EPS)
        yield
        for hf in range(2):
            act(xs2[:], xt[:, hf * 512:(hf + 1) * 512], AF.Identity, reads=[xt, rstdP], writes=[xs2], scale=rstdP[:, 0:1])
            for c4 in range(4):
                tr(PC[:, c4 * 128:(c4 + 1) * 128], xs2[:, c4 * 128:(c4 + 1) * 128], ident[:], reads=[xs2, ident], writes=[PC])
            for c4 in range(4):
                c = hf * 4 + c4
                ts("dve", hT32[:, c, :], PC[:, c4 * 128:(c4 + 1) * 128], Gc[:, c, b:b + 1], shiftc[:, c, b:b + 1], ALU.mult, ALU.add,
                   reads=[PC, Gc, shiftc], writes=[hT32])
            yield
        cp("pool", hT16[:], hT32[:], reads=[hT32], writes=[hT16])
        if b == 0 and i == 0:
            dump("hT", hT32[:], hT32)
        yield
        for g3 in range(3):
            for q4 in range(4):
                ch = g3 * 4 + q4
                for kc in range(8):
                    mm(PC[:, q4 * 128:(q4 + 1) * 128], w16[:, kc, O_QKV + ch * 128:O_QKV + (ch + 1) * 128], hT16[:, kc, :],
                       start=(kc == 0), stop=(kc == 7), reads=[w16, hT16], writes=[PC])
            cp("act", uc[:, g3 * 4:(g3 + 1) * 4, 3:131], PC[:].rearrange("p (a b) -> p a b", a=4), reads=[PC], writes=[uc])
            yield
        for g3 in range(3):
            for q4 in range(4):
                ch = g3 * 4 + q4
                for j in range(4):
                    mm(PC[:, q4 * 128:(q4 + 1) * 128], uc[:, ch, j:j + 128], diagw[:, ch, j, :],
                       start=(j == 0), stop=(j == 3), reads=[uc, diagw], writes=[PC])
            act(qkvs[:, g3 * 512:(g3 + 1) * 512], PC[:], AF.Silu, reads=[PC], writes=[qkvs])
            yield
        cp("pool", uc[:, :, 0:3], uc[:, :, 128:131], reads=[uc], writes=[uc])
        if b == 0 and i == 0:
            dump("qkvs", qkvs[:], qkvs)
        for hf in range(2):
            act(xs2[:], qkvs[:, hf * 512:(hf + 1) * 512], AF.Square, reads=[qkvs], writes=[xs2])
            S.op("dve", lambda e, hf=hf: e.tensor_reduce(ssq8[:, hf * 4:(hf + 1) * 4], xs2[:].rearrange("p (a b) -> p a b", a=4), axis=AX.X, op=ALU.add),
                 reads=[xs2], writes=[ssq8])
        rsqrt_col(rs8, ssq8, 8, 1.0, EPS)
        ts("dve", rs8[:, 0:4], rs8[:, 0:4], 128.0 ** -0.5, None, ALU.mult, reads=[rs8], writes=[rs8])
        yield

    tix = 0
    screp = xt[:].rearrange("p (a b) -> p a b", a=8)
    for b in range(NBC):
        S.op("pool", lambda e: e.memset(S32[:], 0.0), writes=[S32])
        S.op("pool", lambda e: e.memset(S16[:], 0.0), writes=[S16])
        S.op("pool", lambda e: e.memset(uT[:, :, 0:3], 0.0), writes=[uT])
        for kc in range(8):
            cp("dve" if kc % 2 else "pool", screp[:, kc, :], sc[:, kc, b:b + 1].to_broadcast([128, 128]), reads=[sc], writes=[xt])
        for g4 in range(4):
            st = stage[g4 % 2]
            g0 = g4 * 256
            S.dma(st[:], wada_d[:, :, 2048 + g0:2048 + g0 + 256], writes=[st])
            S.dma(rtmp[:, 0:256], bgate_d[:, g0:g0 + 256].partition_broadcast(128), writes=[rtmp])
            S.dma(rtmp[:, 256:512], gpost_d[:, g0:g0 + 256].partition_broadcast(128), writes=[rtmp])
            P = pab()
            for kc in range(8):
                mm(P[:, 0:256], screp[:, kc, :], st[:, kc, :], start=(kc == 0), stop=(kc == 7), reads=[xt, st], writes=[P])
            tt("dve", GATE[:, g0:g0 + 256], P[:, 0:256], rtmp[:, 0:256], ALU.add, reads=[P, rtmp], writes=[GATE])
            tt("pool", GATE[:, g0:g0 + 256], GATE[:, g0:g0 + 256], rtmp[:, 256:512], ALU.mult, reads=[GATE, rtmp], writes=[GATE])
        for i in range(NT):
            t0 = i * 128
            uc = uT
            if i == 0:
                for _ in pre_gen(b, 0):
                    pass
            for kc in range(8):
                mm(PC[:, 0:8], hT16[:, kc, :], w16[:, kc, O_B:O_B + 8], start=(kc == 0), stop=(kc == 7), reads=[hT16, w16], writes=[PC])
            for kc in range(8):
                mm(PC[:, 128:256], hT16[:, kc, :], w16[:, kc, O_CKV:O_CKV + 128], start=(kc == 0), stop=(kc == 7), reads=[hT16, w16], writes=[PC])
            for kc in range(8):
                mm(PC[:, 8:16], hT32[:, kc, :], widx[:, kc, 640:648], start=(kc == 0), stop=(kc == 7), reads=[hT32, widx], writes=[PC])
            cp("act", ba[:], PC[:, 0:8], reads=[PC], writes=[ba])
            ts("dve", iw[:], PC[:, 8:16], (8.0 * 64.0) ** -0.5, None, ALU.mult, reads=[PC], writes=[iw])
            act(junkA[:, 0:128], PC[:, 128:256], AF.Square, reads=[PC], writes=[junkA, ssq1], accum_out=ssq1[:, 0:1])
            rsqrt_col(rstd1, ssq1, 1, 1.0 / 128, EPS)
            stt(ckvn[:, i, :], PC[:, 128:256], rstd1[:, 0:1], gkvB[:], ALU.mult, ALU.mult, reads=[PC, rstd1, gkvB], writes=[ckvn])
            Pq = pab()
            Pq16 = Pq[:].bitcast(BF16)
            tr(Pq16[:, 0:128], ckvn[:, i, :], ident16[:], reads=[ckvn, ident16], writes=[Pq])
            cp("act", ckvnT[:, t0:t0 + 128], Pq16[:, 0:128], reads=[Pq], writes=[ckvnT])
            P = pab()
            for kc in range(8):
                mm(P[:], hT16[:, kc, :], w16[:, kc, O_ZA:O_ZA + 512], start=(kc == 0), stop=(kc == 7), reads=[hT16, w16], writes=[P])
            act(zas[:], P[:], AF.Silu, reads=[P], writes=[zas])
            tt("pool", zas[:].rearrange("p (h v) -> p h v", h=4), zas[:].rearrange("p (h v) -> p h v", h=4),
               ggdnH[:].unsqueeze(1).to_broadcast([128, 4, 128]), ALU.mult, reads=[zas, ggdnH], writes=[zas])
            P = pab()
            for h in range(4):
                for kc in range(8):
                    mm(P[:, h * 128:(h + 1) * 128], w16[:, kc, O_QB + h * 128:O_QB + (h + 1) * 128], hT16[:, kc, :],
                       start=(kc == 0), stop=(kc == 7), reads=[w16, hT16], writes=[P])
            cp("act", qbT[:], P[:].rearrange("p (a b) -> p a b", a=4), reads=[P], writes=[qbT])
            P = pab()
            for h in range(4):
                for kc in range(8):
                    mm(P[:, h * 128:(h + 1) * 128], w16[:, kc, O_ZB + h * 128:O_ZB + (h + 1) * 128], hT16[:, kc, :],
                       start=(kc == 0), stop=(kc == 7), reads=[w16, hT16], writes=[P])
            act(szbT[:], P[:].rearrange("p (a b) -> p a b", a=4), AF.Silu, reads=[P], writes=[szbT])
            P = pab()
            for c4 in range(4):
                for kc in range(8):
                    mm(P[:, c4 * 128:(c4 + 1) * 128], widx[:, kc, c4 * 128:(c4 + 1) * 128], hT32[:, kc, :],
                       start=(kc == 0), stop=(kc == 7), reads=[widx, hT32], writes=[P])
            cp("act", iqT[:], P[:].rearrange("p (a b) -> p a b", a=4), reads=[P], writes=[iqT])
            P = pab()
            for kc in range(8):
                mm(P[:, 0:128], widx[:, kc, 512:640], hT32[:, kc, :], start=(kc == 0), stop=(kc == 7), reads=[widx, hT32], writes=[P])
            cp("act", ikT[:, t0:t0 + 128], P[:, 0:128], reads=[P], writes=[ikTb])

            def gdn_stream():
                act(beta[:], ba[:, 0:4], AF.Exp, reads=[ba], writes=[beta], scale=-1.0)
                yield
                ts("dve", beta[:], beta[:], 1.0, None, ALU.add, reads=[beta], writes=[beta])
                yield
                S.op("dve", lambda e: e.reciprocal(beta[:], beta[:]), reads=[beta], writes=[beta])
                yield
                ts("dve", nbeta[:], beta[:], -1.0, None, ALU.mult, reads=[beta], writes=[nbeta])
                yield
                tt("dve", tmp4[:], ba[:, 4:8], dtbB[:], ALU.add, reads=[ba, dtbB], writes=[tmp4])
                yield
                act(tmp4[:], tmp4[:], AF.Exp, reads=[tmp4], writes=[tmp4])
                yield
                act(tmp4[:], tmp4[:], AF.Ln, reads=[tmp4], writes=[tmp4], bias=1.0)
                yield
                tt("dve", gcol[:], tmp4[:], negA[:], ALU.mult, reads=[tmp4, negA], writes=[gcol])
                yield
                mm(PC[:, 16:20], Umat[:], gcol[:], reads=[Umat, gcol], writes=[PC])
                yield
                mm(PC[:, 20:24], ones32[:], gcol[:], reads=[ones32, gcol], writes=[PC])
                yield
                cp("dve", gc[:], PC[:, 16:20], reads=[PC], writes=[gc])
                yield
                cp("dve", glB[:], PC[:, 20:24], reads=[PC], writes=[glB])
                yield
                act(egc[:], gc[:], AF.Exp, reads=[gc], writes=[egc])
                yield
                act(egl[:], glB[:], AF.Exp, reads=[glB], writes=[egl])
                yield
                tt("dve", tmp4[:], glB[:], gc[:], ALU.subtract, reads=[glB, gc], writes=[tmp4])
                yield
                act(ekg[:], tmp4[:], AF.Exp, reads=[tmp4], writes=[ekg])
                yield
                tt("dve", cf["kbg"][:], rs8[:, 4:8], beta[:], ALU.mult, reads=[rs8, beta], writes=[cf["kbg"]])
                yield
                tt("dve", cf["kbg"][:], cf["kbg"][:], egc[:], ALU.mult, reads=[cf["kbg"], egc], writes=[cf["kbg"]])
                yield
                tt("dve", cf["kg"][:], rs8[:, 4:8], ekg[:], ALU.mult, reads=[rs8, ekg], writes=[cf["kg"]])
                yield
                tt("dve", cf["qg"][:], rs8[:, 0:4], egc[:], ALU.mult, reads=[rs8, egc], writes=[cf["qg"]])
                yield
                q3 = qkvs[:, 0:512].rearrange("p (h d) -> p h d", h=4)
                yield
                k3 = qkvs[:, 512:1024].rearrange("p (h d) -> p h d", h=4)
                yield
                v3 = qkvs[:, 1024:1536].rearrange("p (h d) -> p h d", h=4)
                yield
                bc = lambda c, lo_=0: c[:, lo_:lo_ + 4].unsqueeze(2).to_broadcast([128, 4, 128])
                yield
                tt("dve", khat[:], k3, bc(rs8, 4), ALU.mult, reads=[qkvs, rs8], writes=[khat])
                yield
                tt("pool", qhat[:], q3, bc(rs8, 0), ALU.mult, reads=[qkvs, rs8], writes=[qhat])
                yield
                tt("dve", qg[:], q3, bc(cf["qg"]), ALU.mult, reads=[qkvs, cf["qg"]], writes=[qg])
                yield
                tt("pool", kbg[:], k3, bc(cf["kbg"]), ALU.mult, reads=[qkvs, cf["kbg"]], writes=[kbg])
                yield
                tt("dve", kg[:], k3, bc(cf["kg"]), ALU.mult, reads=[qkvs, cf["kg"]], writes=[kg])
                yield
                tt("pool", vb[:], v3, bc(beta), ALU.mult, reads=[qkvs, beta], writes=[vb])
                yield "M"
                for src, dst in ((khat, khT), (qhat, qhT), (qg, qgT)):
                    P = pabG()
                    P16 = P[:].bitcast(BF16)
                    for h in range(4):
                        tr(P16[:, h * 128:(h + 1) * 128], src[:, h, :], ident16[:], reads=[src, ident16], writes=[P])
                    cp("act", dst[:], P16[:, 0:512].rearrange("p (a b) -> p a b", a=4), reads=[P], writes=[dst])
                yield
                PD = pabG(); PDT = pabG()
                yield
                for h in range(4):
                    gU = gUb[h % 2]
                    ts("dve" if h % 2 else "pool", gU[:], Umat[:], gcol[:, h:h + 1], None, ALU.mult, reads=[Umat, gcol], writes=[gU])
                    mm(PD[:, h * 128:(h + 1) * 128], gU[:], SLmat[:], start=True, stop=False, reads=[gU, SLmat], writes=[PD])
                    mm(PD[:, h * 128:(h + 1) * 128], ident[:], NEGs[:], start=False, stop=True, reads=[ident, NEGs], writes=[PD])
                    mm(PDT[:, h * 128:(h + 1) * 128], SLmat[:], gU[:], start=True, stop=False, reads=[gU, SLmat], writes=[PDT])
                    mm(PDT[:, h * 128:(h + 1) * 128], ident[:], NEGsT[:], start=False, stop=True, reads=[ident, NEGsT], writes=[PDT])
                yield
                act(Es[:], PD[:].rearrange("p (a b) -> p a b", a=4), AF.Exp, reads=[PD], writes=[Es])
                yield
                act(EsT[:], PDT[:].rearrange("p (a b) -> p a b", a=4), AF.Exp, reads=[PDT], writes=[EsT])
                yield
                if b == 0 and i == 0:
                    dump("Es", Es[:].rearrange("p a b -> p (a b)"), Es)
                    dump("beta4", beta[:], beta); dump("gc4", gc[:], gc); dump("rs8", rs8[:], rs8)
                yield
                P = pabG()
                yield
                for h in range(4):
                    mm(P[:, h * 128:(h + 1) * 128], khT[:, h, :], khT[:, h, :], reads=[khT], writes=[P])
                yield
                X, XT = Xb[0], XTb[0]
                yield
                for h in range(4):
                    stt(X[:, h, :], P[:, h * 128:(h + 1) * 128], nbeta[:, h:h + 1], Es[:, h, :], ALU.mult, ALU.mult,
                        reads=[P, nbeta, Es], writes=[X])
                yield
                if b == 0 and i == 0:
                    dump("X0", X[:].rearrange("p a b -> p (a b)"), X)
                yield
                P = pabG()
                yield
                for h in range(4):
                    mm(P[:, h * 128:(h + 1) * 128], khT[:, h, :], qhT[:, h, :], reads=[khT, qhT], writes=[P])
                yield
                tt("pool", EsT[:], EsT[:], ident16[:].unsqueeze(1).to_broadcast([128, 4, 128]), ALU.add, reads=[EsT, ident16], writes=[EsT])
                yield
                tt("dve", attnT[:], P[:].rearrange("p (a b) -> p a b", a=4), EsT[:], ALU.mult, reads=[P, EsT], writes=[attnT])
                yield
                P = pabG()
                yield
                P16 = P[:].bitcast(BF16)
                yield
                for h in range(4):
                    tr(P16[:, h * 128:(h + 1) * 128], X[:, h, :], ident16[:], reads=[X, ident16], writes=[P])
                yield
                cp("act", XT[:], P16[:, 0:512].rearrange("p (a b) -> p a b", a=4), reads=[P], writes=[XT])
                yield
                bcm = lambda ls: msk[:, ls, :].unsqueeze(1).to_broadcast([128, 4, 128])
                yield
                Tc, TT = Tb[0], TTb[0]
                yield
                tt("pool", Tc[:], X[:], bcm(0), ALU.mult, reads=[X, msk], writes=[Tc])
                yield
                tt("pool", Tc[:], Tc[:], ident16[:].unsqueeze(1).to_broadcast([128, 4, 128]), ALU.add, reads=[Tc, ident16], writes=[Tc])
                yield
                tt("dve", TT[:], XT[:], bcm(7), ALU.mult, reads=[XT, msk], writes=[TT])
                yield
                tt("dve", TT[:], TT[:], ident16[:].unsqueeze(1).to_broadcast([128, 4, 128]), ALU.add, reads=[TT, ident16], writes=[TT])
                yield
                gen = 0
                yield
                for ls in range(1, 7):
                    Tn, TTn = Tb[1 - gen], TTb[1 - gen]
                    P1 = pabG()
                    for h in range(4):
                        mm(P1[:, h * 128:(h + 1) * 128], XT[:, h, :], Tc[:, h, :], reads=[XT, Tc], writes=[P1])
                    tt("dve", Wp[:], P1[:].rearrange("p (a b) -> p a b", a=4), bcm(ls), ALU.mult, reads=[P1, msk], writes=[Wp])
                    if ls < 6:
                        P2 = pabG()
                        for h in range(4):
                            mm(P2[:, h * 128:(h + 1) * 128], TT[:, h, :], Wp[:, h, :], reads=[TT, Wp], writes=[P2])
                        tt("dve", Tn[:], P2[:].rearrange("p (a b) -> p a b", a=4), Tc[:], ALU.add, reads=[P2, Tc], writes=[Tn])
                    P3 = PS_
                    for h in range(4):
                        mm(P3[:, h * 128:(h + 1) * 128], Wp[:, h, :], TT[:, h, :], reads=[Wp, TT], writes=[P3])
                    tt("dve", TTn[:], P3[:].rearrange("p (a b) -> p a b", a=4), TT[:], ALU.add, reads=[P3, TT], writes=[TTn])
                    Tc, TT = Tn, TTn
                    gen = 1 - gen
                    yield
                yield
                if b == 0 and i == 0:
                    dump("TTf", TT[:].rearrange("p a b -> p (a b)"), TT)
                    dump("attnT0", attnT[:].rearrange("p a b -> p (a b)"), attnT)
                yield
                P = pabG()
                yield
                for h in range(4):
                    mm(P[:, h * 128:(h + 1) * 128], kbg[:, h, :], TT[:, h, :], reads=[kbg, TT], writes=[P])
                yield
                ts("dve", negwT[:], P[:].rearrange("p (a b) -> p a b", a=4), -1.0, None, ALU.mult, reads=[P], writes=[negwT])
                yield
                PV = pabG()
                for h in range(4):
                    hs = slice(h * 128, (h + 1) * 128)
                    mm(PV[:, hs], TT[:, h, :], vb[:, h, :], start=True, stop=False, reads=[TT, vb], writes=[PV])
                    mm(PV[:, hs], negwT[:, h, :], S16[:, h, :], start=False, stop=True, reads=[negwT, S16], writes=[PV])
                yield
                cp("act", vnew[:], PV[:].rearrange("p (a b) -> p a b", a=4), reads=[PV], writes=[vnew])
                yield
                if b == 0 and i == 0:
                    dump("vnew0", vnew[:].rearrange("p a b -> p (a b)"), vnew)
                yield
                for h in range(4):
                    hs = slice(h * 128, (h + 1) * 128)
                    mm(PS_[:, hs], qgT[:, h, :], S16[:, h, :], start=True, stop=False, reads=[qgT, S16], writes=[PS_])
                    mm(PS_[:, hs], attnT[:, h, :], vnew[:, h, :], start=False, stop=True, reads=[attnT, vnew], writes=[PS_])
                yield
                PDS = pabG()
                for h in range(4):
                    hs = slice(h * 128, (h + 1) * 128)
                    mm(PDS[:, hs], kg[:, h, :], vnew[:, h, :], reads=[kg, vnew], writes=[PDS])
                yield
                for h in range(4):
                    hs = slice(h * 128, (h + 1) * 128)
                    stt(S32[:, h, :], S32[:, h, :], egl[:, h:h + 1], PDS[:, hs], ALU.mult, ALU.add, reads=[S32, egl, PDS], writes=[S32])
                yield
                cp("pool", S16[:], S32[:], reads=[S32], writes=[S16])
                yield
                act(junkA[:, 0:512], PS_[:], AF.Square, reads=[PS_], writes=[junkA])
                yield
                S.op("dve", lambda e: e.tensor_reduce(osq4[:], junkA[:, 0:512].rearrange("p (a b) -> p a b", a=4), axis=AX.X, op=ALU.add),
                     reads=[junkA], writes=[osq4])
                yield
                rsqrt_col(ors4, osq4, 4, 1.0 / 128, EPS)
                yield
                for h in range(4):
                    hs = slice(h * 128, (h + 1) * 128)
                    stt(og[:, h, :], PS_[:, hs], ors4[:, h:h + 1], G1[:, hs], ALU.mult, ALU.mult, reads=[PS_, ors4, G1], writes=[og])
                yield
                if b == 0 and i <= 1:
                    dump(f"og{i}", og[:].rearrange("p a b -> p (a b)"), og)
                yield
                P = pabG()
                yield
                P16 = P[:].bitcast(BF16)
                yield
                for h in range(4):
                    tr(P16[:, h * 128:(h + 1) * 128], og[:, h, :], ident16[:], reads=[og, ident16], writes=[P])
                yield
                cp("act", ogT[:], P16[:, 0:512].rearrange("p (a b) -> p a b", a=4), reads=[P], writes=[ogT])

                yield
            def dsa_stream():
                n = t0 + 128
                yield
                for s0 in range(0, n, 512):
                    w = min(512, n - s0)
                    for h in range(8):
                        P = pab()
                        pr = slice((h % 2) * 64, (h % 2) * 64 + 64)
                        mm(P[:, 0:w], iqT[pr, h // 2, :], ikT[pr, s0:s0 + w], reads=[iqT, ikTb], writes=[P])
                        if h == 0:
                            ts("dve", scoreF[:, s0:s0 + w], P[:, 0:w], 0.0, iw[:, 0:1], ALU.max, ALU.mult, reads=[P, iw], writes=[score])
                        else:
                            rt = rtmp if h % 2 else rtmp2
                            act(rt[:, 0:w], P[:, 0:w], AF.Relu, reads=[P], writes=[rt])
                            stt(scoreF[:, s0:s0 + w], rt[:, 0:w], iw[:, h:h + 1], scoreF[:, s0:s0 + w], ALU.mult, ALU.add,
                                reads=[rt, iw, score], writes=[score])
                        yield
                yield
                tt("dve", scoreF[:, t0:t0 + 128], scoreF[:, t0:t0 + 128], NEGC[:], ALU.add, reads=[score, NEGC], writes=[score])
                yield
                if b == 0 and i == 2:
                    dump("score2", scoreF[:, 0:384], score)
                yield
                if i >= 2:
                    S.op("dve", lambda e: e.tensor_reduce(lo[:], scoreF[:, 0:t0], axis=AX.X, op=ALU.min), reads=[score], writes=[lo])
                    S.op("dve", lambda e: e.tensor_reduce(hw0[:], scoreF[:, 0:n], axis=AX.X, op=ALU.max), reads=[score], writes=[hw0])
                    tt("dve", hw0[:], hw0[:], lo[:], ALU.subtract, reads=[hw0, lo], writes=[hw0])
                    ts("dve", hw0[:], hw0[:], 1.0001, 1e-6, ALU.mult, ALU.add, reads=[hw0], writes=[hw0])
                    ts("dve", hwk[:], pow2[:], hw0[:, 0:1], None, ALU.mult, reads=[pow2, hw0], writes=[hwk])
                    ts("dve", nhwk[:], hwk[:], -1.0, None, ALU.mult, reads=[hwk], writes=[nhwk])
                    ts("dve", mid[:], lo[:], hwk[:, 0:1], -1.0, ALU.add, ALU.mult, reads=[lo, hwk], writes=[mid])
                    S.op("pool", lambda e: e.memset(cbc[:], float(n) - 511.5), writes=[cbc])
                    nmc, nmn = mid, mid2
                    for k in range(NBIS):
                        act(mk[:, 0:n], scoreF[:, 0:n], AF.Sign, reads=[score, nmc], writes=[junkD, cntc], bias=nmc[:, 0:1],
                            accum_out=cntc[:, 0:1])
                        act(sgnc[:], cntc[:], AF.Sign, reads=[cntc, cbc], writes=[sgnc], bias=cbc[:, 0:1])
                        if k < NBIS - 1:
                            act(nmn[:], sgnc[:], AF.Identity, reads=[sgnc, nhwk, nmc], writes=[nmn], scale=nhwk[:, k + 1:k + 2], bias=nmc[:, 0:1])
                            nmc, nmn = nmn, nmc
                        yield
                    act(nmn[:], nmc[:], AF.Identity, reads=[nmc, nhwk], writes=[nmn], scale=-1.0, bias=nhwk[:, NBIS:NBIS + 1])
                    act(lo[:], sgnc[:], AF.Identity, reads=[sgnc, hwk, nmn], writes=[lo], scale=hwk[:, NBIS:NBIS + 1], bias=nmn[:, 0:1])
                else:
                    S.op("dve", lambda e: e.memset(lo[:], -1e29), writes=[lo])
                yield
                ts("dve", mk[:, 0:n], scoreF[:, 0:n], lo[:, 0:1], None, ALU.is_ge, reads=[score, lo], writes=[mk])
                yield
                if b == 0 and i == 2:
                    dump("thr2", lo[:], lo)
                yield
                for j0 in range(0, i + 1, 4):
                    nj = min(4, i + 1 - j0)
                    P = pab()
                    P16 = P[:].bitcast(BF16)
                    for jj in range(nj):
                        j = j0 + jj
                        tr(P16[:, jj * 128:(jj + 1) * 128], mk[:, j * 128:(j + 1) * 128], ident16[:], reads=[mk, ident16], writes=[P])
                    ts("dve", negmk[:, j0:j0 + nj, :], P16[:, 0:nj * 128].rearrange("p (a b) -> p a b", a=nj), 30000.0, -30000.0, ALU.mult, ALU.add,
                       reads=[P], writes=[negmk])
                yield
                qb2 = qbT[:].rearrange("p a b -> p (a b)")
                yield
                for j in range(i + 1):
                    P = pab()
                    P3v = P[:].rearrange("p (a b) -> p a b", a=4)
                    near = j >= i - 1
                    mm(P3v, ckvnT[:, j * 128:(j + 1) * 128], qbT[:], start=True, stop=False, reads=[ckvnT, qbT], writes=[P])
                    mm(P3v, ident16[:], negmk[:, j, :].unsqueeze(1).to_broadcast([128, 4, 128]), start=False, stop=not near,
                       reads=[ident16, negmk], writes=[P])
                    if near:
                        mm(P3v, ident16[:], EB[i - j][:], start=False, stop=True, reads=[ident16, EB[i - j]], writes=[P])
                    E = Eb[j % 2]
                    act(E[:], P3v, AF.Exp, reads=[P], writes=[E], scale=128.0 ** -0.5)
                    pm2 = E[:].rearrange("p a b -> p (a b)")
                    mm(PO[:], ckvn[:, j, :], pm2, start=(j == 0), stop=(j == i), reads=[ckvn, E], writes=[PO])
                    mm(PO2[:], ones16[:], pm2, start=(j == 0), stop=(j == i), reads=[ones16, E], writes=[PO2])
                    yield
                yield
                cp("act", obT[:], PO[:].rearrange("p (a b) -> p a b", a=4), reads=[PO], writes=[obT])
                yield
                act(rden[:], PO2[:], AF.Ln, reads=[PO2], writes=[rden])
                yield
                act(rden[:], rden[:], AF.Exp, reads=[rden], writes=[rden], scale=-1.0)
                yield
                tt("pool", rden[:], rden[:], szbT[:].rearrange("p a b -> p (a b)"), ALU.mult, reads=[rden, szbT], writes=[rden])
                yield
                PY = pab()
                for h in range(4):
                    hs = slice(h * 128, (h + 1) * 128)
                    mm(PY[:, hs], wuv16[:, h, :], obT[:, h, :], reads=[wuv16, obT], writes=[PY])
                yield
                tt("dve", ygT[:].rearrange("p a b -> p (a b)"), PY[:], Rg[:], ALU.mult, reads=[PY, Rg], writes=[ygT])
                yield
                if b == 0 and i <= 2:
                    dump(f"yg{i}", ygT[:].rearrange("p a b -> p (a b)"), ygT)
                yield
                yield
            streams = [[gdn_stream(), 3], [dsa_stream(), 1]]
            pre = pre_gen(b, i + 1) if i + 1 < NT else None
            pre_ok = False
            while streams or pre is not None:
                for ent in list(streams):
                    for _ in range(ent[1]):
                        try:
                            if next(ent[0]) == "M":
                                pre_ok = True
                        except StopIteration:
                            streams.remove(ent)
                            break
                if pre is not None and (pre_ok or not streams):
                    for _ in range(2):
                        try:
                            next(pre)
                        except StopIteration:
                            pre = None
                            break
            for nh in range(2):
                for c in range(8):
                    lhs = ogT[:, c, :] if c < 4 else ygT[:, c - 4, :]
                    mm(PT2h[nh][:], lhs, wout16[:, c, nh * 512:(nh + 1) * 512],
                       start=(c == 0), stop=(c == 7), reads=[ogT, ygT, wout16], writes=[PT2h[nh]])
            for nh in range(2):
                act(junkA[:, nh * 512:(nh + 1) * 512], PT2h[nh][:], AF.Square, reads=[PT2h[nh]],
                    writes=[junkA, msq], accum_out=msq[:, nh:nh + 1])
            tt("dve", ssq1[:], msq[:, 0:1], msq[:, 1:2], ALU.add, reads=[msq], writes=[ssq1])
            rsqrt_col(mrs, ssq1, 1, 1.0 / D, EPS)
            for nh in range(2):
                sl = slice(nh * 512, (nh + 1) * 512)
                stt(otmp[:, sl], PT2h[nh][:], mrs[:, 0:1], GATE[:, sl], ALU.mult, ALU.mult, reads=[PT2h[nh], mrs, GATE], writes=[otmp])
            xr = negmk[:].rearrange("p a b -> p (a b)").bitcast(F32)
            S.dma(xr, x_d[b, t0:t0 + 128, :], writes=[negmk])
            tt("pool", otmp[:], otmp[:], xr, ALU.add, reads=[otmp, negmk], writes=[otmp])
            S.dma(out_d[b, t0:t0 + 128, :], otmp[:], reads=[otmp])
            tix += 1
    S.finish("sp")
    return nc, S


def _masks():
    i = np.arange(128)[:, None]; j = np.arange(128)[None, :]
    m = np.zeros((128, 8, 128), np.float32)
    for ls in range(7):
        sz = 1 << ls
        m[:, ls, :] = (((i // sz) % 2 == 1) & ((j // sz) == (i // sz) - 1)).astype(np.float32)
    m[:, 7, :] = m[:, 0, :].T
    return m


def _layout(inputs, core):
    f = lambda a: np.ascontiguousarray(a, dtype=np.float32)
    bs = slice(core * NBC, (core + 1) * NBC)
    kp = lambda w: w.reshape(8, 128, -1).transpose(1, 0, 2)
    w_in = inputs["w_in"][0]
    ik = w_in[:, O_IK:O_IK + 64]
    w_idx = np.concatenate([w_in[:, O_IQ:O_IQ + 512], ik, ik, w_in[:, O_IW:O_IW + 8]], axis=1)
    b_ada = inputs["b_ada"][0]
    return {
        "x": f(inputs["x"][bs]),
        "cT": f(inputs["c"][bs].T.reshape(8, 128, NBC).transpose(1, 0, 2)),
        "w_ada": f(kp(inputs["w_ada"][0])),
        "b_col": f(b_ada.reshape(24, 128).T),
        "b_gate": f(b_ada[2048:3072].reshape(1, 1024)),
        "g_pre": f(inputs["g_pre"][0].reshape(8, 128).T),
        "w_in": f(kp(w_in[:, :NW16])),
        "w_idx": f(kp(w_idx)),
        "conv_w": f(inputs["conv_w"][0].reshape(4, 12, 128).transpose(2, 1, 0)),
        "a_log": f(inputs["a_log"].reshape(1, 4)),
        "dt_bias": f(inputs["dt_bias"].reshape(1, 4)),
        "g_gdn": f(inputs["g_gdn"].reshape(1, 128)),
        "g_kv": f(inputs["g_kv"].reshape(1, 128)),
        "w_uv": f(inputs["w_uv"][0].transpose(1, 0, 2)),
        "rel_bias": f(inputs["rel_bias"].reshape(1, 128)),
        "w_out": f(kp(inputs["w_out"][0])),
        "g_post": f(inputs["g_post"].reshape(1, 1024)),
        "masks": _masks(),
    }


def kernel(**inputs):
    inputs = {k: np.asarray(v) for k, v in inputs.items()}
    nc, _ = build()
    in_maps = [_layout(inputs, c) for c in range(8)]
    res = run_bass_kernel_spmd(nc, in_maps, core_ids=list(range(8)))
    return np.concatenate([r["out"] for r in res.results], axis=0).astype(np.float32)
```

```python
import math
import numpy as np
import concourse.bass as bass
import concourse.mybir as mybir
from concourse.bass_utils import run_bass_kernel_spmd

F32 = mybir.dt.float32
BF16 = mybir.dt.bfloat16
AF = mybir.ActivationFunctionType
ALU = mybir.AluOpType
AX = mybir.AxisListType

D = 1024
L = 2048
NBC = 4
NT = 16
EPS = 1e-6
NEG = -30000.0
NBIS = 14


class Res:
    __slots__ = ("name", "w", "r")

    def __init__(self, name):
        self.name = name
        self.w = None
        self.r = {}


class Buf:
    def __init__(self, t, name):
        self.t = t
        self.r = Res(name)

    def __getitem__(self, k):
        return self.t[k]


class Sched:
    def __init__(self, nc, ndma=8):
        self.nc = nc
        self.e = {"pe": nc.tensor, "act": nc.scalar, "dve": nc.vector, "pool": nc.gpsimd, "sp": nc.sync}
        self.sem = {k: nc.alloc_semaphore("sem_" + k) for k in self.e}
        self.cnt = {k: 0 for k in self.e}
        self.seen = {k: {} for k in self.e}
        self.dsem = [nc.alloc_semaphore(f"dsem{i}") for i in range(ndma)]
        self.dcnt = [0] * ndma
        self.dnext = 0
        self.nwait = 0

    def _wait(self, eng, key, val):
        if self.seen[eng].get(key, 0) >= val:
            return
        self.seen[eng][key] = val
        sem = self.sem[key] if isinstance(key, str) else self.dsem[key[1]]
        self.e[eng].wait_ge(sem, val)
        self.nwait += 1

    def _deps(self, eng, reads, writes):
        need = {}
        for b in reads:
            r = b.r
            if r.w is not None:
                k, v = r.w
                need[k] = max(need.get(k, 0), v)
        for b in writes:
            w = b.r
            if w.w is not None:
                k, v = w.w
                need[k] = max(need.get(k, 0), v)
            for k, v in w.r.items():
                need[k] = max(need.get(k, 0), v)
        for k, v in need.items():
            if eng == "pe" and k == "pe":
                continue
            self._wait(eng, k, v)

    def op(self, eng, fn, reads=(), writes=()):
        self._deps(eng, reads, writes)
        ins = fn(self.e[eng])
        self.cnt[eng] += 1
        ins.then_inc(self.sem[eng], 1)
        v = self.cnt[eng]
        for b in reads:
            b.r.r[eng] = v
        for b in writes:
            b.r.w = (eng, v)
            b.r.r = {}
        return ins

    def dma(self, out, in_, reads=(), writes=(), q="sp", **kw):
        slot = self.dnext
        self.dnext = (self.dnext + 1) % len(self.dsem)
        key = ("d", slot)
        if self.dcnt[slot] > 0:
            self._wait(q, key, self.dcnt[slot])
        self._deps(q, reads, writes)
        self.dcnt[slot] += 16
        self.e[q].dma_start(out=out, in_=in_, **kw).then_inc(self.dsem[slot], 16)
        v = self.dcnt[slot]
        for b in reads:
            b.r.r[key] = v
        for b in writes:
            b.r.w = (key, v)
            b.r.r = {}

    def finish(self, eng="sp"):
        for k in self.cnt:
            if self.cnt[k] > 0 and k != eng:
                self._wait(eng, k, self.cnt[k])
        for i, c in enumerate(self.dcnt):
            if c > 0:
                self._wait(eng, ("d", i), c)


def t5_bucket_np(n):
    n = np.maximum(n, 0)
    nf = np.maximum(n, 1).astype(np.float32)
    large = 16 + (np.log(nf / np.float32(16)) / np.float32(math.log(128 / 16)) * np.float32(16)).astype(np.int32)
    large = np.minimum(large, 31)
    return np.where(n < 16, n, large)


O_QKV, O_ZA, O_B, O_A, O_QB, O_CKV, O_ZB, O_IQ, O_IK, O_IW = 0, 1536, 2048, 2052, 2056, 2568, 2696, 3208, 3720, 3784
NW16 = 3208
NIDX = 648


def build(debug=None):
    nc = bass.Bass("TRN2", target_bir_lowering=False)
    din = lambda n, sh: nc.dram_tensor(n, sh, F32, kind="ExternalInput").ap()
    x_d = din("x", [NBC, L, D])
    cT_d = din("cT", [128, 8, NBC])
    wada_d = din("w_ada", [128, 8, 3072])
    bcol_d = din("b_col", [128, 24])
    bgate_d = din("b_gate", [1, 1024])
    gpre_d = din("g_pre", [128, 8])
    win_d = din("w_in", [128, 8, NW16])
    widx_d = din("w_idx", [128, 8, NIDX])
    conv_d = din("conv_w", [128, 12, 4])
    alog_d = din("a_log", [1, 4])
    dtb_d = din("dt_bias", [1, 4])
    ggdn_d = din("g_gdn", [1, 128])
    gkv_d = din("g_kv", [1, 128])
    wuv_d = din("w_uv", [128, 4, 128])
    rb_d = din("rel_bias", [1, 128])
    wout_d = din("w_out", [128, 8, 1024])
    gpost_d = din("g_post", [1, 1024])
    msk_d = din("masks", [128, 8, 128])
    out_d = nc.dram_tensor("out", [NBC, L, D], F32, kind="ExternalOutput").ap()
    dbg_d = {}
    if debug:
        for n, sh in debug.items():
            dbg_d[n] = nc.dram_tensor("dbg_" + n, list(sh), F32, kind="ExternalOutput").ap()

    S = Sched(nc)
    cnt = [0]

    def sb(shape, dt=F32, name=None):
        cnt[0] += 1
        name = "s_" + (name or f"t{cnt[0]}")
        return Buf(nc.alloc_sbuf_tensor(name, list(shape), dt), name)

    def ps(shape, dt=F32, name=None):
        cnt[0] += 1
        name = "p_" + (name or f"p{cnt[0]}")
        return Buf(nc.alloc_psum_tensor(name, list(shape), dt), name)

    def mm(out, lhsT, rhs, start=True, stop=True, reads=(), writes=()):
        S.op("pe", lambda e: e.matmul(out, lhsT, rhs, start=start, stop=stop), reads, writes)

    def tr(out, in_, ident, reads=(), writes=()):
        S.op("pe", lambda e: e.transpose(out, in_, ident), reads, writes)

    def act(out, in_, func, reads=(), writes=(), **kw):
        S.op("act", lambda e: e.activation(out, in_, func, **kw), reads, writes)

    def tt(eng, out, in0, in1, op, reads=(), writes=()):
        S.op(eng, lambda e: e.tensor_tensor(out, in0, in1, op=op), reads, writes)

    def ts(eng, out, in0, s1, s2, op0, op1=None, reads=(), writes=(), accum_out=None):
        if op1 is None:
            S.op(eng, lambda e: e.tensor_scalar(out, in0, s1, None, op0=op0), reads, writes)
        elif accum_out is not None:
            S.op(eng, lambda e: e.tensor_scalar(out, in0, s1, s2, op0=op0, op1=op1, accum_out=accum_out), reads, writes)
        else:
            S.op(eng, lambda e: e.tensor_scalar(out, in0, s1, s2, op0=op0, op1=op1), reads, writes)

    def stt(out, in0, scalar, in1, op0, op1, reads=(), writes=()):
        S.op("dve", lambda e: e.scalar_tensor_tensor(out, in0, scalar, in1, op0=op0, op1=op1), reads, writes)

    def cp(eng, out, in_, reads=(), writes=()):
        if eng == "act":
            S.op("act", lambda e: e.copy(out, in_), reads, writes)
        else:
            S.op(eng, lambda e: e.tensor_copy(out, in_), reads, writes)

    def dump(name, ap, buf):
        if name in dbg_d:
            S.dma(dbg_d[name], ap, reads=[buf], q="pool")

    big4 = sb([128, 1024], name="big4")
    rtmp2 = sb([128, 512], name="rtmp2")

    def view(buf, ap):
        v = Buf.__new__(Buf)
        v.t = ap
        v.r = buf.r
        return v
    io = view(big4, big4[:, 0:128])
    S.op("pool", lambda e: e.iota(io[:], [[1, 128]], base=0, channel_multiplier=-1,
                                  allow_small_or_imprecise_dtypes=True), writes=[io])
    ident = sb([128, 128], name="ident")
    ident16 = sb([128, 128], BF16, name="ident16")
    Umat = sb([128, 128], name="Umat")
    SLmat = sb([128, 128], name="SLmat")
    NEGs = sb([128, 128], name="NEGs")
    NEGsT = sb([128, 128], name="NEGsT")
    NEGC = sb([128, 128], name="NEGC")
    ones32 = sb([128, 128], name="ones32")
    ones16 = sb([128, 128], BF16, name="ones16")
    mhalf = sb([128, 8], name="mhalf")
    ts("dve", ident[:], io[:], 0.0, None, ALU.is_equal, reads=[io], writes=[ident])
    ts("dve", ident16[:], io[:], 0.0, None, ALU.is_equal, reads=[io], writes=[ident16])
    ts("dve", Umat[:], io[:], 0.0, None, ALU.is_ge, reads=[io], writes=[Umat])
    ts("dve", SLmat[:], io[:], 0.0, None, ALU.is_lt, reads=[io], writes=[SLmat])
    ts("dve", NEGs[:], io[:], 0.0, NEG, ALU.is_ge, ALU.mult, reads=[io], writes=[NEGs])
    ts("dve", NEGsT[:], io[:], 0.0, NEG, ALU.is_le, ALU.mult, reads=[io], writes=[NEGsT])
    ts("dve", NEGC[:], io[:], 0.0, -1e30, ALU.is_gt, ALU.mult, reads=[io], writes=[NEGC])
    S.op("pool", lambda e: e.memset(ones32[:], 1.0), writes=[ones32])
    S.op("pool", lambda e: e.memset(ones16[:], 1.0), writes=[ones16])
    S.op("pool", lambda e: e.memset(mhalf[:], -0.5), writes=[mhalf])
    pow2 = sb([128, NBIS + 1], name="pow2")
    for k in range(NBIS + 1):
        S.op("pool", lambda e, k=k: e.memset(pow2[:, k:k + 1], 0.5 ** (k + 1)), writes=[pow2])

    PT2a = ps([128, 512], name="PT2a")
    PT2b = ps([128, 512], name="PT2b")
    PT2h = [PT2a, PT2b]
    rotg = [0]

    def pabG():
        rotg[0] ^= 1
        return PT2a if rotg[0] else PT2b
    PA = ps([128, 512], name="PA")
    PB = ps([128, 512], name="PB")
    PC = ps([128, 512], name="PC")
    PO = ps([128, 512], name="PO")
    PO2 = ps([128, 512], name="PO2")
    PS_ = ps([128, 512], name="PS_")
    rot = [0]

    def pab():
        rot[0] ^= 1
        return PA if rot[0] else PB

    stage = [sb([128, 8, 256], name="stage0"), sb([128, 8, 256], name="stage1")]
    w16 = sb([128, 8, NW16], BF16, name="w16")
    widx = sb([128, 8, NIDX], name="widx")
    wout16 = sb([128, 8, 1024], BF16, name="wout16")
    wuv16 = sb([128, 4, 128], BF16, name="wuv16")
    S.dma(widx[:], widx_d, writes=[widx])
    si = 0
    for c0 in range(0, NW16, 256):
        w = min(256, NW16 - c0)
        st = stage[si % 2]; si += 1
        S.dma(st[:, :, 0:w], win_d[:, :, c0:c0 + w], writes=[st])
        cp("dve" if si % 2 else "pool", w16[:, :, c0:c0 + w], st[:, :, 0:w], reads=[st], writes=[w16])
    for c0 in range(0, 1024, 256):
        st = stage[si % 2]; si += 1
        S.dma(st[:], wout_d[:, :, c0:c0 + 256], writes=[st])
        cp("dve" if si % 2 else "pool", wout16[:, :, c0:c0 + 256], st[:], reads=[st], writes=[wout16])
    st = stage[si % 2]; si += 1
    S.dma(st[:, 0:4, 0:128], wuv_d, writes=[st])
    cp("dve", wuv16[:], st[:, 0:4, 0:128], reads=[st], writes=[wuv16])

    st = stage[si % 2]; si += 1
    S.dma(st[:, :, 0:128], msk_d, writes=[st])
    msk = sb([128, 8, 128], BF16, name="msk")
    cp("dve", msk[:], st[:, :, 0:128], reads=[st], writes=[msk])
    convw = sb([128, 12, 4], name="convw")
    S.dma(convw[:], conv_d, writes=[convw])
    diagw = sb([128, 12, 4, 128], BF16, name="diagw")
    for ch in range(12):
        for j in range(4):
            ts("dve" if (ch + j) % 2 else "pool", diagw[:, ch, j, :], ident[:], convw[:, ch, j:j + 1], None, ALU.mult,
               reads=[ident, convw], writes=[diagw])

    def bcast_row(src, n, name):
        t = sb([128, n], name=name)
        S.dma(t[:], src.partition_broadcast(128), writes=[t])
        return t
    alogB = bcast_row(alog_d, 4, "alogB")
    dtbB = bcast_row(dtb_d, 4, "dtbB")
    ggdnB = bcast_row(ggdn_d, 128, "ggdnB")
    gkvB = bcast_row(gkv_d, 128, "gkvB")
    rbB = bcast_row(rb_d, 128, "rbB")
    negA = sb([128, 4], name="negA")
    act(negA[:], alogB[:], AF.Exp, reads=[alogB], writes=[negA])
    ts("dve", negA[:], negA[:], -1.0, None, ALU.mult, reads=[negA], writes=[negA])
    ggdnH = ggdnB

    cT = sb([128, 8, NBC], name="cT")
    S.dma(cT[:], cT_d, writes=[cT])
    sc = sb([128, 8, NBC], name="sc")
    act(sc[:], cT[:], AF.Silu, reads=[cT], writes=[sc])
    bcol = sb([128, 24], name="bcol")
    S.dma(bcol[:], bcol_d, writes=[bcol])
    gpre = sb([128, 8], name="gpre")
    S.dma(gpre[:], gpre_d, writes=[gpre])
    shiftc = sb([128, 8, NBC], name="shiftc")
    Gc = sb([128, 8, NBC], name="Gc")
    GATE = sb([128, 1024], name="GATE")
    for n8 in range(8):
        st = stage[si % 2]; si += 1
        S.dma(st[:], wada_d[:, :, n8 * 256:(n8 + 1) * 256], writes=[st])
        for q2 in range(2):
            dch = (n8 % 4) * 2 + q2
            for kc in range(8):
                mm(PC[:, q2 * 4:q2 * 4 + 4], st[:, kc, q2 * 128:(q2 + 1) * 128], sc[:, kc, :],
                   start=(kc == 0), stop=(kc == 7), reads=[st, sc], writes=[PC])
            dst = shiftc if n8 < 4 else Gc
            ts("dve", dst[:, dch, :], PC[:, q2 * 4:q2 * 4 + 4], bcol[:, n8 * 2 + q2:n8 * 2 + q2 + 1], None, ALU.add,
               reads=[PC, bcol], writes=[dst])
    ts("dve", Gc[:], Gc[:], 1.0, None, ALU.add, reads=[Gc], writes=[Gc])
    tt("dve", Gc[:], Gc[:], gpre[:].unsqueeze(2).to_broadcast([128, 8, NBC]), ALU.mult, reads=[Gc, gpre], writes=[Gc])

    bk = t5_bucket_np(np.arange(256))
    lo_b = [int(np.argmax(bk >= b)) if (bk >= b).any() else 100000 for b in range(32)]
    rb3 = rbB[:].rearrange("p (b h) -> p b h", h=4)
    dlt = sb([128, 32, 4], name="dlt")
    cp("dve", dlt[:, 0:1, :], rb3[:, 0:1, :], reads=[rbB], writes=[dlt])
    tt("dve", dlt[:, 1:32, :], rb3[:, 1:32, :], rb3[:, 0:31, :], ALU.subtract, reads=[rbB], writes=[dlt])
    EB = [sb([128, 4, 128], BF16, name=f"EB{t}") for t in range(2)]
    EBf = view(rtmp2, rtmp2[:, 0:128])
    rtmp = sb([128, 512], name="rtmp")
    gUb = [sb([128, 128], name=f"gU{i}") for i in range(2)]
    distT = gUb[0]
    tmpb = gUb[1]
    for typ in range(2):
        ts("dve", distT[:], io[:], float(128 * typ), None, ALU.add, reads=[io], writes=[distT])
        for h in range(4):
            acc = EBf
            ts("dve", acc[:], ones32[:], dlt[:, 0, h:h + 1], rb3[:, 31, h:h + 1], ALU.mult, ALU.subtract,
               reads=[ones32, dlt, rbB], writes=[acc])
            for bb in range(1, 32):
                if lo_b[bb] > 255:
                    continue
                ts("dve", tmpb[:], distT[:], float(lo_b[bb]) - 0.5, dlt[:, bb, h:h + 1], ALU.is_ge, ALU.mult,
                   reads=[distT, dlt], writes=[tmpb])
                tt("dve", acc[:], acc[:], tmpb[:], ALU.add, reads=[acc, tmpb], writes=[acc])
            ts("dve", acc[:], acc[:], 128.0 ** 0.5, None, ALU.mult, reads=[acc], writes=[acc])
            cp("dve", EB[typ][:, h, :], acc[:], reads=[acc], writes=[EB[typ]])

    xt = sb([128, 1024], name="xt")
    junkA = big4; xs = big4; otmp = big4
    hT32 = sb([128, 8, 128], name="hT32")
    hT16 = sb([128, 8, 128], BF16, name="hT16")
    uT = sb([128, 12, 131], BF16, name="uT")
    qkvs = sb([128, 1536], BF16, name="qkvs")
    col = lambda n, name: sb([128, n], name=name)
    ssq1 = col(1, "ssq1"); rstd1 = col(1, "rstd1"); ssqP2 = col(2, "ssqP2"); ssqP = col(1, "ssqP"); rstdP = col(1, "rstdP")
    xs2 = sb([128, 512], name="xs2")
    ssq8 = col(8, "ssq8"); rs8 = col(8, "rs8")
    ba = col(8, "ba"); beta = col(4, "beta"); gcol = col(4, "gcol"); gc = col(4, "gc"); glB = col(4, "glB")
    egc = col(4, "egc"); ekg = col(4, "ekg"); egl = col(4, "egl"); tmp4 = col(4, "tmp4"); nbeta = col(4, "nbeta")
    cf = {n: col(4, "cf_" + n) for n in ("kbg", "kg", "qg")}
    khat = sb([128, 4, 128], BF16, name="khat"); qhat = sb([128, 4, 128], BF16, name="qhat")
    qg = sb([128, 4, 128], BF16, name="qg"); kbg = sb([128, 4, 128], BF16, name="kbg")
    kg = sb([128, 4, 128], BF16, name="kg"); vb = sb([128, 4, 128], BF16, name="vb")
    khT = sb([128, 4, 128], BF16, name="khT"); qhT = sb([128, 4, 128], BF16, name="qhT"); qgT = sb([128, 4, 128], BF16, name="qgT")
    Es = sb([128, 4, 128], BF16, name="Es"); EsT = sb([128, 4, 128], BF16, name="EsT")
    Xb = [sb([128, 4, 128], BF16, name="X0")]
    XTb = [sb([128, 4, 128], BF16, name="XT0")]
    Tb = [sb([128, 4, 128], BF16, name=f"T{i}") for i in range(2)]
    TTb = [sb([128, 4, 128], BF16, name=f"TT{i}") for i in range(2)]
    Wp = khat
    attnT = sb([128, 4, 128], BF16, name="attnT")
    negwT = qhat
    vnew = qg
    S32 = sb([128, 4, 128], name="S32"); S16 = sb([128, 4, 128], BF16, name="S16")
    zas = sb([128, 512], BF16, name="zas"); G1 = zas
    osq4 = col(4, "osq4"); ors4 = col(4, "ors4")
    og = sb([128, 4, 128], BF16, name="og"); ogT = sb([128, 4, 128], BF16, name="ogT")
    qbT = sb([128, 4, 128], BF16, name="qbT"); szbT = sb([128, 4, 128], BF16, name="szbT")
    iqT = sb([128, 4, 128], name="iqT"); iw = col(8, "iw")
    ckvn = sb([128, NT, 128], BF16, name="ckvn"); ckvnT = sb([128, L], BF16, name="ckvnT")
    ikTb = stage[1]
    ikT = ikTb[:].rearrange("p a b -> p (a b)")
    score = stage[0]
    scoreF = score[:].rearrange("p a b -> p (a b)")
    mk = sb([128, L], BF16, name="mk")
    junkD = mk
    junkDF = mk
    lo = col(1, "lo"); hw0 = col(1, "hw0"); hwk = col(NBIS + 1, "hwk"); nhwk = col(NBIS + 1, "nhwk"); mid = col(1, "mid"); mid2 = col(1, "mid2"); sgnc = col(1, "sgnc"); cbc = col(1, "cbc"); cntc = col(1, "cntc"); tstep = col(1, "tstep")
    Eb = [sb([128, 4, 128], BF16, name=f"Eb{i}") for i in range(2)]
    negmk = sb([128, NT, 128], BF16, name="negmk"); mkT = negmk
    obT = sb([128, 4, 128], BF16, name="obT")
    rden = rtmp; Rg = rden
    ygT = sb([128, 4, 128], BF16, name="ygT")
    msq = col(2, "msq"); mrs = col(1, "mrs")

    def rsqrt_col(dst, src, n, scale, eps):
        ts("dve", dst[:, 0:n], src[:, 0:n], scale, eps, ALU.mult, ALU.add, reads=[src], writes=[dst])
        tt("pool", dst[:, 0:n], dst[:, 0:n], mhalf[:, 0:n], ALU.pow, reads=[dst, mhalf], writes=[dst])

    def pre_gen(b, i):
        t0 = i * 128
        uc = uT
        S.dma(xt[:], x_d[b, t0:t0 + 128, :], writes=[xt])
        yield
        for hf in range(2):
            act(xs2[:], xt[:, hf * 512:(hf + 1) * 512], AF.Square, reads=[xt], writes=[xs2, ssqP2], accum_out=ssqP2[:, hf:hf + 1])
        tt("pool", ssqP[:], ssqP2[:, 0:1], ssqP2[:, 1:2], ALU.add, reads=[ssqP2], writes=[ssqP])
        rsqrt_col(rstdP, ssqP, 1, 1.0 / D, EPS)
        yield
        for hf in range(2):
            ts("dve", xs2[:], xt[:, hf * 512:(hf + 1) * 512], rstdP[:, 0:1], None, ALU.mult, reads=[xt, rstdP], writes=[xs2])
            for c4 in range(4):
                tr(PC[:, c4 * 128:(c4 + 1) * 128], xs2[:, c4 * 128:(c4 + 1) * 128], ident[:], reads=[xs2, ident], writes=[PC])
            for c4 in range(4):
                c = hf * 4 + c4
                ts("dve", hT32[:, c, :], PC[:, c4 * 128:(c4 + 1) * 128], Gc[:, c, b:b + 1], shiftc[:, c, b:b + 1], ALU.mult, ALU.add,
                   reads=[PC, Gc, shiftc], writes=[hT32])
            yield
        cp("pool", hT16[:], hT32[:], reads=[hT32], writes=[hT16])
        if b == 0 and i == 0:
            dump("hT", hT32[:], hT32)
        yield
        for g3 in range(3):
            for q4 in range(4):
                ch = g3 * 4 + q4
                for kc in range(8):
                    mm(PC[:, q4 * 128:(q4 + 1) * 128], w16[:, kc, O_QKV + ch * 128:O_QKV + (ch + 1) * 128], hT16[:, kc, :],
                       start=(kc == 0), stop=(kc == 7), reads=[w16, hT16], writes=[PC])
            cp("dve", uc[:, g3 * 4:(g3 + 1) * 4, 3:131], PC[:].rearrange("p (a b) -> p a b", a=4), reads=[PC], writes=[uc])
            yield
        for g3 in range(3):
            for q4 in range(4):
                ch = g3 * 4 + q4
                for j in range(4):
                    mm(PC[:, q4 * 128:(q4 + 1) * 128], uc[:, ch, j:j + 128], diagw[:, ch, j, :],
                       start=(j == 0), stop=(j == 3), reads=[uc, diagw], writes=[PC])
            act(qkvs[:, g3 * 512:(g3 + 1) * 512], PC[:], AF.Silu, reads=[PC], writes=[qkvs])
            yield
        cp("pool", uc[:, :, 0:3], uc[:, :, 128:131], reads=[uc], writes=[uc])
        if b == 0 and i == 0:
            dump("qkvs", qkvs[:], qkvs)
        for hf in range(2):
            tt("dve", xs2[:], qkvs[:, hf * 512:(hf + 1) * 512], qkvs[:, hf * 512:(hf + 1) * 512], ALU.mult, reads=[qkvs], writes=[xs2])
            S.op("dve", lambda e, hf=hf: e.tensor_reduce(ssq8[:, hf * 4:(hf + 1) * 4], xs2[:].rearrange("p (a b) -> p a b", a=4), axis=AX.X, op=ALU.add),
                 reads=[xs2], writes=[ssq8])
        rsqrt_col(rs8, ssq8, 8, 1.0, EPS)
        ts("dve", rs8[:, 0:4], rs8[:, 0:4], 128.0 ** -0.5, None, ALU.mult, reads=[rs8], writes=[rs8])
        yield

    tix = 0
    screp = xt[:].rearrange("p (a b) -> p a b", a=8)
    for b in range(NBC):
        S.op("pool", lambda e: e.memset(S32[:], 0.0), writes=[S32])
        S.op("pool", lambda e: e.memset(S16[:], 0.0), writes=[S16])
        S.op("pool", lambda e: e.memset(uT[:, :, 0:3], 0.0), writes=[uT])
        for kc in range(8):
            cp("dve" if kc % 2 else "pool", screp[:, kc, :], sc[:, kc, b:b + 1].to_broadcast([128, 128]), reads=[sc], writes=[xt])
        for g4 in range(4):
            st = stage[g4 % 2]
            g0 = g4 * 256
            S.dma(st[:], wada_d[:, :, 2048 + g0:2048 + g0 + 256], writes=[st])
            S.dma(rtmp[:, 0:256], bgate_d[:, g0:g0 + 256].partition_broadcast(128), writes=[rtmp])
            S.dma(rtmp[:, 256:512], gpost_d[:, g0:g0 + 256].partition_broadcast(128), writes=[rtmp])
            P = pab()
            for kc in range(8):
                mm(P[:, 0:256], screp[:, kc, :], st[:, kc, :], start=(kc == 0), stop=(kc == 7), reads=[xt, st], writes=[P])
            tt("dve", GATE[:, g0:g0 + 256], P[:, 0:256], rtmp[:, 0:256], ALU.add, reads=[P, rtmp], writes=[GATE])
            tt("pool", GATE[:, g0:g0 + 256], GATE[:, g0:g0 + 256], rtmp[:, 256:512], ALU.mult, reads=[GATE, rtmp], writes=[GATE])
        for i in range(NT):
            t0 = i * 128
            uc = uT
            if i == 0:
                for _ in pre_gen(b, 0):
                    pass
            for kc in range(8):
                mm(PC[:, 0:8], hT16[:, kc, :], w16[:, kc, O_B:O_B + 8], start=(kc == 0), stop=(kc == 7), reads=[hT16, w16], writes=[PC])
            for kc in range(8):
                mm(PC[:, 128:256], hT16[:, kc, :], w16[:, kc, O_CKV:O_CKV + 128], start=(kc == 0), stop=(kc == 7), reads=[hT16, w16], writes=[PC])
            for kc in range(8):
                mm(PC[:, 8:16], hT32[:, kc, :], widx[:, kc, 640:648], start=(kc == 0), stop=(kc == 7), reads=[hT32, widx], writes=[PC])
            cp("act", ba[:], PC[:, 0:8], reads=[PC], writes=[ba])
            ts("dve", iw[:], PC[:, 8:16], (8.0 * 64.0) ** -0.5, None, ALU.mult, reads=[PC], writes=[iw])
            act(junkA[:, 0:128], PC[:, 128:256], AF.Square, reads=[PC], writes=[junkA, ssq1], accum_out=ssq1[:, 0:1])
            rsqrt_col(rstd1, ssq1, 1, 1.0 / 128, EPS)
            stt(ckvn[:, i, :], PC[:, 128:256], rstd1[:, 0:1], gkvB[:], ALU.mult, ALU.mult, reads=[PC, rstd1, gkvB], writes=[ckvn])
            Pq = pab()
            Pq16 = Pq[:].bitcast(BF16)
            tr(Pq16[:, 0:128], ckvn[:, i, :], ident16[:], reads=[ckvn, ident16], writes=[Pq])
            cp("act", ckvnT[:, t0:t0 + 128], Pq16[:, 0:128], reads=[Pq], writes=[ckvnT])
            P = pab()
            for kc in range(8):
                mm(P[:], hT16[:, kc, :], w16[:, kc, O_ZA:O_ZA + 512], start=(kc == 0), stop=(kc == 7), reads=[hT16, w16], writes=[P])
            act(zas[:], P[:], AF.Silu, reads=[P], writes=[zas])
            tt("pool", zas[:].rearrange("p (h v) -> p h v", h=4), zas[:].rearrange("p (h v) -> p h v", h=4),
               ggdnH[:].unsqueeze(1).to_broadcast([128, 4, 128]), ALU.mult, reads=[zas, ggdnH], writes=[zas])
            P = pab()
            for h in range(4):
                for kc in range(8):
                    mm(P[:, h * 128:(h + 1) * 128], w16[:, kc, O_QB + h * 128:O_QB + (h + 1) * 128], hT16[:, kc, :],
                       start=(kc == 0), stop=(kc == 7), reads=[w16, hT16], writes=[P])
            cp("act", qbT[:], P[:].rearrange("p (a b) -> p a b", a=4), reads=[P], writes=[qbT])
            P = pab()
            for h in range(4):
                for kc in range(8):
                    mm(P[:, h * 128:(h + 1) * 128], w16[:, kc, O_ZB + h * 128:O_ZB + (h + 1) * 128], hT16[:, kc, :],
                       start=(kc == 0), stop=(kc == 7), reads=[w16, hT16], writes=[P])
            act(szbT[:], P[:].rearrange("p (a b) -> p a b", a=4), AF.Silu, reads=[P], writes=[szbT])
            P = pab()
            for c4 in range(4):
                for kc in range(8):
                    mm(P[:, c4 * 128:(c4 + 1) * 128], widx[:, kc, c4 * 128:(c4 + 1) * 128], hT32[:, kc, :],
                       start=(kc == 0), stop=(kc == 7), reads=[widx, hT32], writes=[P])
            cp("act", iqT[:], P[:].rearrange("p (a b) -> p a b", a=4), reads=[P], writes=[iqT])
            P = pab()
            for kc in range(8):
                mm(P[:, 0:128], widx[:, kc, 512:640], hT32[:, kc, :], start=(kc == 0), stop=(kc == 7), reads=[widx, hT32], writes=[P])
            cp("act", ikT[:, t0:t0 + 128], P[:, 0:128], reads=[P], writes=[ikTb])

            def gdn_stream():
                act(beta[:], ba[:, 0:4], AF.Exp, reads=[ba], writes=[beta], scale=-1.0)
                yield
                ts("dve", beta[:], beta[:], 1.0, None, ALU.add, reads=[beta], writes=[beta])
                yield
                S.op("dve", lambda e: e.reciprocal(beta[:], beta[:]), reads=[beta], writes=[beta])
                yield
                ts("dve", nbeta[:], beta[:], -1.0, None, ALU.mult, reads=[beta], writes=[nbeta])
                yield
                tt("dve", tmp4[:], ba[:, 4:8], dtbB[:], ALU.add, reads=[ba, dtbB], writes=[tmp4])
                yield
                act(tmp4[:], tmp4[:], AF.Exp, reads=[tmp4], writes=[tmp4])
                yield
                act(tmp4[:], tmp4[:], AF.Ln, reads=[tmp4], writes=[tmp4], bias=1.0)
                yield
                tt("dve", gcol[:], tmp4[:], negA[:], ALU.mult, reads=[tmp4, negA], writes=[gcol])
                yield
                mm(PC[:, 16:20], Umat[:], gcol[:], reads=[Umat, gcol], writes=[PC])
                yield
                mm(PC[:, 20:24], ones32[:], gcol[:], reads=[ones32, gcol], writes=[PC])
                yield
                cp("dve", gc[:], PC[:, 16:20], reads=[PC], writes=[gc])
                yield
                cp("dve", glB[:], PC[:, 20:24], reads=[PC], writes=[glB])
                yield
                act(egc[:], gc[:], AF.Exp, reads=[gc], writes=[egc])
                yield
                act(egl[:], glB[:], AF.Exp, reads=[glB], writes=[egl])
                yield
                tt("dve", tmp4[:], glB[:], gc[:], ALU.subtract, reads=[glB, gc], writes=[tmp4])
                yield
                act(ekg[:], tmp4[:], AF.Exp, reads=[tmp4], writes=[ekg])
                yield
                tt("dve", cf["kbg"][:], rs8[:, 4:8], beta[:], ALU.mult, reads=[rs8, beta], writes=[cf["kbg"]])
                yield
                tt("dve", cf["kbg"][:], cf["kbg"][:], egc[:], ALU.mult, reads=[cf["kbg"], egc], writes=[cf["kbg"]])
                yield
                tt("dve", cf["kg"][:], rs8[:, 4:8], ekg[:], ALU.mult, reads=[rs8, ekg], writes=[cf["kg"]])
                yield
                tt("dve", cf["qg"][:], rs8[:, 0:4], egc[:], ALU.mult, reads=[rs8, egc], writes=[cf["qg"]])
                yield
                q3 = qkvs[:, 0:512].rearrange("p (h d) -> p h d", h=4)
                yield
                k3 = qkvs[:, 512:1024].rearrange("p (h d) -> p h d", h=4)
                yield
                v3 = qkvs[:, 1024:1536].rearrange("p (h d) -> p h d", h=4)
                yield
                bc = lambda c, lo_=0: c[:, lo_:lo_ + 4].unsqueeze(2).to_broadcast([128, 4, 128])
                yield
                tt("dve", khat[:], k3, bc(rs8, 4), ALU.mult, reads=[qkvs, rs8], writes=[khat])
                yield
                tt("pool", qhat[:], q3, bc(rs8, 0), ALU.mult, reads=[qkvs, rs8], writes=[qhat])
                yield
                tt("dve", qg[:], q3, bc(cf["qg"]), ALU.mult, reads=[qkvs, cf["qg"]], writes=[qg])
                yield
                tt("pool", kbg[:], k3, bc(cf["kbg"]), ALU.mult, reads=[qkvs, cf["kbg"]], writes=[kbg])
                yield
                tt("dve", kg[:], k3, bc(cf["kg"]), ALU.mult, reads=[qkvs, cf["kg"]], writes=[kg])
                yield
                tt("pool", vb[:], v3, bc(beta), ALU.mult, reads=[qkvs, beta], writes=[vb])
                yield "M"
                for src, dst in ((khat, khT), (qhat, qhT), (qg, qgT)):
                    P = pabG()
                    P16 = P[:].bitcast(BF16)
                    for h in range(4):
                        tr(P16[:, h * 128:(h + 1) * 128], src[:, h, :], ident16[:], reads=[src, ident16], writes=[P])
                    cp("act", dst[:], P16[:, 0:512].rearrange("p (a b) -> p a b", a=4), reads=[P], writes=[dst])
                yield
                PD = pabG(); PDT = pabG()
                yield
                for h in range(4):
                    gU = gUb[h % 2]
                    ts("dve" if h % 2 else "pool", gU[:], Umat[:], gcol[:, h:h + 1], None, ALU.mult, reads=[Umat, gcol], writes=[gU])
                    mm(PD[:, h * 128:(h + 1) * 128], gU[:], SLmat[:], start=True, stop=False, reads=[gU, SLmat], writes=[PD])
                    mm(PD[:, h * 128:(h + 1) * 128], ident[:], NEGs[:], start=False, stop=True, reads=[ident, NEGs], writes=[PD])
                    mm(PDT[:, h * 128:(h + 1) * 128], SLmat[:], gU[:], start=True, stop=False, reads=[gU, SLmat], writes=[PDT])
                    mm(PDT[:, h * 128:(h + 1) * 128], ident[:], NEGsT[:], start=False, stop=True, reads=[ident, NEGsT], writes=[PDT])
                yield
                act(Es[:], PD[:].rearrange("p (a b) -> p a b", a=4), AF.Exp, reads=[PD], writes=[Es])
                yield
                act(EsT[:], PDT[:].rearrange("p (a b) -> p a b", a=4), AF.Exp, reads=[PDT], writes=[EsT])
                yield
                if b == 0 and i == 0:
                    dump("Es", Es[:].rearrange("p a b -> p (a b)"), Es)
                    dump("beta4", beta[:], beta); dump("gc4", gc[:], gc); dump("rs8", rs8[:], rs8)
                yield
                P = pabG()
                yield
                for h in range(4):
                    mm(P[:, h * 128:(h + 1) * 128], khT[:, h, :], khT[:, h, :], reads=[khT], writes=[P])
                yield
                X, XT = Xb[0], XTb[0]
                yield
                for h in range(4):
                    stt(X[:, h, :], P[:, h * 128:(h + 1) * 128], nbeta[:, h:h + 1], Es[:, h, :], ALU.mult, ALU.mult,
                        reads=[P, nbeta, Es], writes=[X])
                yield
                if b == 0 and i == 0:
                    dump("X0", X[:].rearrange("p a b -> p (a b)"), X)
                yield
                P = pabG()
                yield
                for h in range(4):
                    mm(P[:, h * 128:(h + 1) * 128], khT[:, h, :], qhT[:, h, :], reads=[khT, qhT], writes=[P])
                yield
                tt("pool", EsT[:], EsT[:], ident16[:].unsqueeze(1).to_broadcast([128, 4, 128]), ALU.add, reads=[EsT, ident16], writes=[EsT])
                yield
                tt("dve", attnT[:], P[:].rearrange("p (a b) -> p a b", a=4), EsT[:], ALU.mult, reads=[P, EsT], writes=[attnT])
                yield
                P = pabG()
                yield
                P16 = P[:].bitcast(BF16)
                yield
                for h in range(4):
                    tr(P16[:, h * 128:(h + 1) * 128], X[:, h, :], ident16[:], reads=[X, ident16], writes=[P])
                yield
                cp("act", XT[:], P16[:, 0:512].rearrange("p (a b) -> p a b", a=4), reads=[P], writes=[XT])
                yield
                bcm = lambda ls: msk[:, ls, :].unsqueeze(1).to_broadcast([128, 4, 128])
                yield
                Tc, TT = Tb[0], TTb[0]
                yield
                tt("pool", Tc[:], X[:], bcm(0), ALU.mult, reads=[X, msk], writes=[Tc])
                yield
                tt("pool", Tc[:], Tc[:], ident16[:].unsqueeze(1).to_broadcast([128, 4, 128]), ALU.add, reads=[Tc, ident16], writes=[Tc])
                yield
                tt("dve", TT[:], XT[:], bcm(7), ALU.mult, reads=[XT, msk], writes=[TT])
                yield
                tt("dve", TT[:], TT[:], ident16[:].unsqueeze(1).to_broadcast([128, 4, 128]), ALU.add, reads=[TT, ident16], writes=[TT])
                yield
                gen = 0
                yield
                for ls in range(1, 7):
                    Tn, TTn = Tb[1 - gen], TTb[1 - gen]
                    P1 = pabG()
                    for h in range(4):
                        mm(P1[:, h * 128:(h + 1) * 128], XT[:, h, :], Tc[:, h, :], reads=[XT, Tc], writes=[P1])
                    tt("dve", Wp[:], P1[:].rearrange("p (a b) -> p a b", a=4), bcm(ls), ALU.mult, reads=[P1, msk], writes=[Wp])
                    if ls < 6:
                        P2 = pabG()
                        for h in range(4):
                            mm(P2[:, h * 128:(h + 1) * 128], TT[:, h, :], Wp[:, h, :], reads=[TT, Wp], writes=[P2])
                        tt("dve", Tn[:], P2[:].rearrange("p (a b) -> p a b", a=4), Tc[:], ALU.add, reads=[P2, Tc], writes=[Tn])
                    P3 = PS_
                    for h in range(4):
                        mm(P3[:, h * 128:(h + 1) * 128], Wp[:, h, :], TT[:, h, :], reads=[Wp, TT], writes=[P3])
                    tt("dve", TTn[:], P3[:].rearrange("p (a b) -> p a b", a=4), TT[:], ALU.add, reads=[P3, TT], writes=[TTn])
                    Tc, TT = Tn, TTn
                    gen = 1 - gen
                    yield
                yield
                if b == 0 and i == 0:
                    dump("TTf", TT[:].rearrange("p a b -> p (a b)"), TT)
                    dump("attnT0", attnT[:].rearrange("p a b -> p (a b)"), attnT)
                yield
                P = pabG()
                yield
                for h in range(4):
                    mm(P[:, h * 128:(h + 1) * 128], kbg[:, h, :], TT[:, h, :], reads=[kbg, TT], writes=[P])
                yield
                ts("dve", negwT[:], P[:].rearrange("p (a b) -> p a b", a=4), -1.0, None, ALU.mult, reads=[P], writes=[negwT])
                yield
                PV = pabG()
                for h in range(4):
                    hs = slice(h * 128, (h + 1) * 128)
                    mm(PV[:, hs], TT[:, h, :], vb[:, h, :], start=True, stop=False, reads=[TT, vb], writes=[PV])
                    mm(PV[:, hs], negwT[:, h, :], S16[:, h, :], start=False, stop=True, reads=[negwT, S16], writes=[PV])
                yield
                cp("act", vnew[:], PV[:].rearrange("p (a b) -> p a b", a=4), reads=[PV], writes=[vnew])
                yield
                if b == 0 and i == 0:
                    dump("vnew0", vnew[:].rearrange("p a b -> p (a b)"), vnew)
                yield
                for h in range(4):
                    hs = slice(h * 128, (h + 1) * 128)
                    mm(PS_[:, hs], qgT[:, h, :], S16[:, h, :], start=True, stop=False, reads=[qgT, S16], writes=[PS_])
                    mm(PS_[:, hs], attnT[:, h, :], vnew[:, h, :], start=False, stop=True, reads=[attnT, vnew], writes=[PS_])
                yield
                PDS = pabG()
                for h in range(4):
                    hs = slice(h * 128, (h + 1) * 128)
                    mm(PDS[:, hs], kg[:, h, :], vnew[:, h, :], reads=[kg, vnew], writes=[PDS])
                yield
                for h in range(4):
                    hs = slice(h * 128, (h + 1) * 128)
                    stt(S32[:, h, :], S32[:, h, :], egl[:, h:h + 1], PDS[:, hs], ALU.mult, ALU.add, reads=[S32, egl, PDS], writes=[S32])
                yield
                cp("pool", S16[:], S32[:], reads=[S32], writes=[S16])
                yield
                act(junkA[:, 0:512], PS_[:], AF.Square, reads=[PS_], writes=[junkA])
                yield
                S.op("dve", lambda e: e.tensor_reduce(osq4[:], junkA[:, 0:512].rearrange("p (a b) -> p a b", a=4), axis=AX.X, op=ALU.add),
                     reads=[junkA], writes=[osq4])
                yield
                rsqrt_col(ors4, osq4, 4, 1.0 / 128, EPS)
                yield
                for h in range(4):
                    hs = slice(h * 128, (h + 1) * 128)
                    stt(og[:, h, :], PS_[:, hs], ors4[:, h:h + 1], G1[:, hs], ALU.mult, ALU.mult, reads=[PS_, ors4, G1], writes=[og])
                yield
                if b == 0 and i <= 1:
                    dump(f"og{i}", og[:].rearrange("p a b -> p (a b)"), og)
                yield
                P = pabG()
                yield
                P16 = P[:].bitcast(BF16)
                yield
                for h in range(4):
                    tr(P16[:, h * 128:(h + 1) * 128], og[:, h, :], ident16[:], reads=[og, ident16], writes=[P])
                yield
                cp("act", ogT[:], P16[:, 0:512].rearrange("p (a b) -> p a b", a=4), reads=[P], writes=[ogT])

                yield
            def dsa_stream():
                n = t0 + 128
                yield
                for s0 in range(0, n, 512):
                    w = min(512, n - s0)
                    for h in range(8):
                        P = pab()
                        pr = slice((h % 2) * 64, (h % 2) * 64 + 64)
                        mm(P[:, 0:w], iqT[pr, h // 2, :], ikT[pr, s0:s0 + w], reads=[iqT, ikTb], writes=[P])
                        if h == 0:
                            ts("dve", scoreF[:, s0:s0 + w], P[:, 0:w], 0.0, iw[:, 0:1], ALU.max, ALU.mult, reads=[P, iw], writes=[score])
                        else:
                            rt = rtmp if h % 2 else rtmp2
                            act(rt[:, 0:w], P[:, 0:w], AF.Relu, reads=[P], writes=[rt])
                            stt(scoreF[:, s0:s0 + w], rt[:, 0:w], iw[:, h:h + 1], scoreF[:, s0:s0 + w], ALU.mult, ALU.add,
                                reads=[rt, iw, score], writes=[score])
                        yield
                yield
                tt("dve", scoreF[:, t0:t0 + 128], scoreF[:, t0:t0 + 128], NEGC[:], ALU.add, reads=[score, NEGC], writes=[score])
                yield
                if b == 0 and i == 2:
                    dump("score2", scoreF[:, 0:384], score)
                yield
                yield "B"
                if i >= 2:
                    S.op("dve", lambda e: e.tensor_reduce(lo[:], scoreF[:, 0:t0], axis=AX.X, op=ALU.min), reads=[score], writes=[lo])
                    S.op("dve", lambda e: e.tensor_reduce(hw0[:], scoreF[:, 0:n], axis=AX.X, op=ALU.max), reads=[score], writes=[hw0])
                    tt("dve", hw0[:], hw0[:], lo[:], ALU.subtract, reads=[hw0, lo], writes=[hw0])
                    ts("dve", hw0[:], hw0[:], 1.0001, 1e-6, ALU.mult, ALU.add, reads=[hw0], writes=[hw0])
                    ts("dve", hwk[:], pow2[:], hw0[:, 0:1], None, ALU.mult, reads=[pow2, hw0], writes=[hwk])
                    ts("dve", nhwk[:], hwk[:], -1.0, None, ALU.mult, reads=[hwk], writes=[nhwk])
                    ts("dve", mid[:], lo[:], hwk[:, 0:1], -1.0, ALU.add, ALU.mult, reads=[lo, hwk], writes=[mid])
                    S.op("pool", lambda e: e.memset(cbc[:], float(n) - 511.5), writes=[cbc])
                    nmc, nmn = mid, mid2
                    for k in range(NBIS):
                        act(mk[:, 0:n], scoreF[:, 0:n], AF.Sign, reads=[score, nmc], writes=[junkD, cntc], bias=nmc[:, 0:1],
                            accum_out=cntc[:, 0:1])
                        act(sgnc[:], cntc[:], AF.Sign, reads=[cntc, cbc], writes=[sgnc], bias=cbc[:, 0:1])
                        if k < NBIS - 1:
                            act(nmn[:], sgnc[:], AF.Identity, reads=[sgnc, nhwk, nmc], writes=[nmn], scale=nhwk[:, k + 1:k + 2], bias=nmc[:, 0:1])
                            nmc, nmn = nmn, nmc
                        yield
                    act(nmn[:], nmc[:], AF.Identity, reads=[nmc, nhwk], writes=[nmn], scale=-1.0, bias=nhwk[:, NBIS:NBIS + 1])
                    act(lo[:], sgnc[:], AF.Identity, reads=[sgnc, hwk, nmn], writes=[lo], scale=hwk[:, NBIS:NBIS + 1], bias=nmn[:, 0:1])
                else:
                    S.op("dve", lambda e: e.memset(lo[:], -1e29), writes=[lo])
                yield
                ts("dve", mk[:, 0:n], scoreF[:, 0:n], lo[:, 0:1], None, ALU.is_ge, reads=[score, lo], writes=[mk])
                yield
                if b == 0 and i == 2:
                    dump("thr2", lo[:], lo)
                yield
                for j0 in range(0, i + 1, 4):
                    nj = min(4, i + 1 - j0)
                    P = pab()
                    P16 = P[:].bitcast(BF16)
                    for jj in range(nj):
                        j = j0 + jj
                        tr(P16[:, jj * 128:(jj + 1) * 128], mk[:, j * 128:(j + 1) * 128], ident16[:], reads=[mk, ident16], writes=[P])
                    ts("dve", negmk[:, j0:j0 + nj, :], P16[:, 0:nj * 128].rearrange("p (a b) -> p a b", a=nj), 30000.0, -30000.0, ALU.mult, ALU.add,
                       reads=[P], writes=[negmk])
                yield
                qb2 = qbT[:].rearrange("p a b -> p (a b)")
                yield
                for j in range(i + 1):
                    P = pab()
                    P3v = P[:].rearrange("p (a b) -> p a b", a=4)
                    near = j >= i - 1
                    mm(P3v, ckvnT[:, j * 128:(j + 1) * 128], qbT[:], start=True, stop=False, reads=[ckvnT, qbT], writes=[P])
                    mm(P3v, ident16[:], negmk[:, j, :].unsqueeze(1).to_broadcast([128, 4, 128]), start=False, stop=not near,
                       reads=[ident16, negmk], writes=[P])
                    if near:
                        mm(P3v, ident16[:], EB[i - j][:], start=False, stop=True, reads=[ident16, EB[i - j]], writes=[P])
                    E = Eb[j % 2]
                    act(E[:], P3v, AF.Exp, reads=[P], writes=[E], scale=128.0 ** -0.5)
                    pm2 = E[:].rearrange("p a b -> p (a b)")
                    mm(PO[:], ckvn[:, j, :], pm2, start=(j == 0), stop=(j == i), reads=[ckvn, E], writes=[PO])
                    mm(PO2[:], ones16[:], pm2, start=(j == 0), stop=(j == i), reads=[ones16, E], writes=[PO2])
                    yield
                yield
                cp("act", obT[:], PO[:].rearrange("p (a b) -> p a b", a=4), reads=[PO], writes=[obT])
                yield
                act(rden[:], PO2[:], AF.Ln, reads=[PO2], writes=[rden])
                yield
                act(rden[:], rden[:], AF.Exp, reads=[rden], writes=[rden], scale=-1.0)
                yield
                tt("pool", rden[:], rden[:], szbT[:].rearrange("p a b -> p (a b)"), ALU.mult, reads=[rden, szbT], writes=[rden])
                yield
                PY = pab()
                for h in range(4):
                    hs = slice(h * 128, (h + 1) * 128)
                    mm(PY[:, hs], wuv16[:, h, :], obT[:, h, :], reads=[wuv16, obT], writes=[PY])
                yield
                tt("dve", ygT[:].rearrange("p a b -> p (a b)"), PY[:], Rg[:], ALU.mult, reads=[PY, Rg], writes=[ygT])
                yield
                if b == 0 and i <= 2:
                    dump(f"yg{i}", ygT[:].rearrange("p a b -> p (a b)"), ygT)
                yield
                yield
            streams = [[gdn_stream(), 3], [dsa_stream(), 1]]
            pre = pre_gen(b, i + 1) if i + 1 < NT else None
            seen = set()
            while streams or pre is not None:
                for ent in list(streams):
                    for _ in range(ent[1]):
                        try:
                            seen.add(next(ent[0]))
                        except StopIteration:
                            streams.remove(ent)
                            break
                if pre is not None and (("M" in seen and "B" in seen) or not streams):
                    for _ in range(1):
                        try:
                            next(pre)
                        except StopIteration:
                            pre = None
                            break
            for nh in range(2):
                for c in range(8):
                    lhs = ogT[:, c, :] if c < 4 else ygT[:, c - 4, :]
                    mm(PT2h[nh][:], lhs, wout16[:, c, nh * 512:(nh + 1) * 512],
                       start=(c == 0), stop=(c == 7), reads=[ogT, ygT, wout16], writes=[PT2h[nh]])
            for nh in range(2):
                act(junkA[:, nh * 512:(nh + 1) * 512], PT2h[nh][:], AF.Square, reads=[PT2h[nh]],
                    writes=[junkA, msq], accum_out=msq[:, nh:nh + 1])
            tt("dve", ssq1[:], msq[:, 0:1], msq[:, 1:2], ALU.add, reads=[msq], writes=[ssq1])
            rsqrt_col(mrs, ssq1, 1, 1.0 / D, EPS)
            for nh in range(2):
                sl = slice(nh * 512, (nh + 1) * 512)
                stt(otmp[:, sl], PT2h[nh][:], mrs[:, 0:1], GATE[:, sl], ALU.mult, ALU.mult, reads=[PT2h[nh], mrs, GATE], writes=[otmp])
            xr = negmk[:].rearrange("p a b -> p (a b)").bitcast(F32)
            S.dma(xr, x_d[b, t0:t0 + 128, :], writes=[negmk])
            tt("pool", otmp[:], otmp[:], xr, ALU.add, reads=[otmp, negmk], writes=[otmp])
            S.dma(out_d[b, t0:t0 + 128, :], otmp[:], reads=[otmp])
            tix += 1
    S.finish("sp")
    return nc, S


def _masks():
    i = np.arange(128)[:, None]; j = np.arange(128)[None, :]
    m = np.zeros((128, 8, 128), np.float32)
    for ls in range(7):
        sz = 1 << ls
        m[:, ls, :] = (((i // sz) % 2 == 1) & ((j // sz) == (i // sz) - 1)).astype(np.float32)
    m[:, 7, :] = m[:, 0, :].T
    return m


def _layout(inputs, core):
    f = lambda a: np.ascontiguousarray(a, dtype=np.float32)
    bs = slice(core * NBC, (core + 1) * NBC)
    kp = lambda w: w.reshape(8, 128, -1).transpose(1, 0, 2)
    w_in = inputs["w_in"][0]
    ik = w_in[:, O_IK:O_IK + 64]
    w_idx = np.concatenate([w_in[:, O_IQ:O_IQ + 512], ik, ik, w_in[:, O_IW:O_IW + 8]], axis=1)
    b_ada = inputs["b_ada"][0]
    return {
        "x": f(inputs["x"][bs]),
        "cT": f(inputs["c"][bs].T.reshape(8, 128, NBC).transpose(1, 0, 2)),
        "w_ada": f(kp(inputs["w_ada"][0])),
        "b_col": f(b_ada.reshape(24, 128).T),
        "b_gate": f(b_ada[2048:3072].reshape(1, 1024)),
        "g_pre": f(inputs["g_pre"][0].reshape(8, 128).T),
        "w_in": f(kp(w_in[:, :NW16])),
        "w_idx": f(kp(w_idx)),
        "conv_w": f(inputs["conv_w"][0].reshape(4, 12, 128).transpose(2, 1, 0)),
        "a_log": f(inputs["a_log"].reshape(1, 4)),
        "dt_bias": f(inputs["dt_bias"].reshape(1, 4)),
        "g_gdn": f(inputs["g_gdn"].reshape(1, 128)),
        "g_kv": f(inputs["g_kv"].reshape(1, 128)),
        "w_uv": f(inputs["w_uv"][0].transpose(1, 0, 2)),
        "rel_bias": f(inputs["rel_bias"].reshape(1, 128)),
        "w_out": f(kp(inputs["w_out"][0])),
        "g_post": f(inputs["g_post"].reshape(1, 1024)),
        "masks": _masks(),
    }


def kernel(**inputs):
    inputs = {k: np.asarray(v) for k, v in inputs.items()}
    nc, _ = build()
    in_maps = [_layout(inputs, c) for c in range(8)]
    res = run_bass_kernel_spmd(nc, in_maps, core_ids=list(range(8)))
    return np.concatenate([r["out"] for r in res.results], axis=0).astype(np.float32)
```

```python
import math
import numpy as np
import concourse.bass as bass
import concourse.mybir as mybir
from concourse.bass_utils import run_bass_kernel_spmd

F32 = mybir.dt.float32
BF16 = mybir.dt.bfloat16
AF = mybir.ActivationFunctionType
ALU = mybir.AluOpType
AX = mybir.AxisListType

D = 1024
L = 2048
NBC = 4
NT = 16
EPS = 1e-6
NEG = -30000.0
NBIS = 14


class Res:
    __slots__ = ("name", "w", "r")

    def __init__(self, name):
        self.name = name
        self.w = None
        self.r = {}


class Buf:
    def __init__(self, t, name):
        self.t = t
        self.r = Res(name)

    def __getitem__(self, k):
        return self.t[k]


class Sched:
    def __init__(self, nc, ndma=8):
        self.nc = nc
        self.e = {"pe": nc.tensor, "act": nc.scalar, "dve": nc.vector, "pool": nc.gpsimd, "sp": nc.sync}
        self.sem = {k: nc.alloc_semaphore("sem_" + k) for k in self.e}
        self.cnt = {k: 0 for k in self.e}
        self.seen = {k: {} for k in self.e}
        self.dsem = [nc.alloc_semaphore(f"dsem{i}") for i in range(ndma)]
        self.dcnt = [0] * ndma
        self.dnext = 0
        self.nwait = 0

    def _wait(self, eng, key, val):
        if self.seen[eng].get(key, 0) >= val:
            return
        self.seen[eng][key] = val
        sem = self.sem[key] if isinstance(key, str) else self.dsem[key[1]]
        self.e[eng].wait_ge(sem, val)
        self.nwait += 1

    def _deps(self, eng, reads, writes):
        need = {}
        for b in reads:
            r = b.r
            if r.w is not None:
                k, v = r.w
                need[k] = max(need.get(k, 0), v)
        for b in writes:
            w = b.r
            if w.w is not None:
                k, v = w.w
                need[k] = max(need.get(k, 0), v)
            for k, v in w.r.items():
                need[k] = max(need.get(k, 0), v)
        for k, v in need.items():
            if eng == "pe" and k == "pe":
                continue
            self._wait(eng, k, v)

    def op(self, eng, fn, reads=(), writes=()):
        self._deps(eng, reads, writes)
        ins = fn(self.e[eng])
        self.cnt[eng] += 1
        ins.then_inc(self.sem[eng], 1)
        v = self.cnt[eng]
        for b in reads:
            b.r.r[eng] = v
        for b in writes:
            b.r.w = (eng, v)
            b.r.r = {}
        return ins

    def dma(self, out, in_, reads=(), writes=(), q="sp", **kw):
        slot = self.dnext
        self.dnext = (self.dnext + 1) % len(self.dsem)
        key = ("d", slot)
        if self.dcnt[slot] > 0:
            self._wait(q, key, self.dcnt[slot])
        self._deps(q, reads, writes)
        self.dcnt[slot] += 16
        self.e[q].dma_start(out=out, in_=in_, **kw).then_inc(self.dsem[slot], 16)
        v = self.dcnt[slot]
        for b in reads:
            b.r.r[key] = v
        for b in writes:
            b.r.w = (key, v)
            b.r.r = {}

    def finish(self, eng="sp"):
        for k in self.cnt:
            if self.cnt[k] > 0 and k != eng:
                self._wait(eng, k, self.cnt[k])
        for i, c in enumerate(self.dcnt):
            if c > 0:
                self._wait(eng, ("d", i), c)


def t5_bucket_np(n):
    n = np.maximum(n, 0)
    nf = np.maximum(n, 1).astype(np.float32)
    large = 16 + (np.log(nf / np.float32(16)) / np.float32(math.log(128 / 16)) * np.float32(16)).astype(np.int32)
    large = np.minimum(large, 31)
    return np.where(n < 16, n, large)


O_QKV, O_ZA, O_B, O_A, O_QB, O_CKV, O_ZB, O_IQ, O_IK, O_IW = 0, 1536, 2048, 2052, 2056, 2568, 2696, 3208, 3720, 3784
NW16 = 3208
NIDX = 648


def build(debug=None):
    nc = bass.Bass("TRN2", target_bir_lowering=False)
    din = lambda n, sh: nc.dram_tensor(n, sh, F32, kind="ExternalInput").ap()
    x_d = din("x", [NBC, L, D])
    cT_d = din("cT", [128, 8, NBC])
    wada_d = din("w_ada", [128, 8, 3072])
    bcol_d = din("b_col", [128, 24])
    bgate_d = din("b_gate", [1, 1024])
    gpre_d = din("g_pre", [128, 8])
    win_d = din("w_in", [128, 8, NW16])
    widx_d = din("w_idx", [128, 8, NIDX])
    conv_d = din("conv_w", [128, 12, 4])
    alog_d = din("a_log", [1, 4])
    dtb_d = din("dt_bias", [1, 4])
    ggdn_d = din("g_gdn", [1, 128])
    gkv_d = din("g_kv", [1, 128])
    wuv_d = din("w_uv", [128, 4, 128])
    rb_d = din("rel_bias", [1, 128])
    wout_d = din("w_out", [128, 8, 1024])
    gpost_d = din("g_post", [1, 1024])
    msk_d = din("masks", [128, 8, 128])
    out_d = nc.dram_tensor("out", [NBC, L, D], F32, kind="ExternalOutput").ap()
    dbg_d = {}
    if debug:
        for n, sh in debug.items():
            dbg_d[n] = nc.dram_tensor("dbg_" + n, list(sh), F32, kind="ExternalOutput").ap()

    S = Sched(nc)
    cnt = [0]

    def sb(shape, dt=F32, name=None):
        cnt[0] += 1
        name = "s_" + (name or f"t{cnt[0]}")
        return Buf(nc.alloc_sbuf_tensor(name, list(shape), dt), name)

    def ps(shape, dt=F32, name=None):
        cnt[0] += 1
        name = "p_" + (name or f"p{cnt[0]}")
        return Buf(nc.alloc_psum_tensor(name, list(shape), dt), name)

    def mm(out, lhsT, rhs, start=True, stop=True, reads=(), writes=()):
        S.op("pe", lambda e: e.matmul(out, lhsT, rhs, start=start, stop=stop), reads, writes)

    def tr(out, in_, ident, reads=(), writes=()):
        S.op("pe", lambda e: e.transpose(out, in_, ident), reads, writes)

    def act(out, in_, func, reads=(), writes=(), **kw):
        S.op("act", lambda e: e.activation(out, in_, func, **kw), reads, writes)

    def tt(eng, out, in0, in1, op, reads=(), writes=()):
        S.op(eng, lambda e: e.tensor_tensor(out, in0, in1, op=op), reads, writes)

    def ts(eng, out, in0, s1, s2, op0, op1=None, reads=(), writes=(), accum_out=None):
        if op1 is None:
            S.op(eng, lambda e: e.tensor_scalar(out, in0, s1, None, op0=op0), reads, writes)
        elif accum_out is not None:
            S.op(eng, lambda e: e.tensor_scalar(out, in0, s1, s2, op0=op0, op1=op1, accum_out=accum_out), reads, writes)
        else:
            S.op(eng, lambda e: e.tensor_scalar(out, in0, s1, s2, op0=op0, op1=op1), reads, writes)

    def stt(out, in0, scalar, in1, op0, op1, reads=(), writes=()):
        S.op("dve", lambda e: e.scalar_tensor_tensor(out, in0, scalar, in1, op0=op0, op1=op1), reads, writes)

    def cp(eng, out, in_, reads=(), writes=()):
        if eng == "act":
            S.op("act", lambda e: e.copy(out, in_), reads, writes)
        else:
            S.op(eng, lambda e: e.tensor_copy(out, in_), reads, writes)

    def dump(name, ap, buf):
        if name in dbg_d:
            S.dma(dbg_d[name], ap, reads=[buf], q="pool")

    big4 = sb([128, 1024], name="big4")
    rtmp2 = sb([128, 512], name="rtmp2")

    def view(buf, ap):
        v = Buf.__new__(Buf)
        v.t = ap
        v.r = buf.r
        return v
    io = view(big4, big4[:, 0:128])
    S.op("pool", lambda e: e.iota(io[:], [[1, 128]], base=0, channel_multiplier=-1,
                                  allow_small_or_imprecise_dtypes=True), writes=[io])
    ident = sb([128, 128], name="ident")
    ident16 = sb([128, 128], BF16, name="ident16")
    Umat = sb([128, 128], name="Umat")
    SLmat = sb([128, 128], name="SLmat")
    NEGs = sb([128, 128], name="NEGs")
    NEGsT = sb([128, 128], name="NEGsT")
    NEGC = sb([128, 128], name="NEGC")
    ones32 = sb([128, 128], name="ones32")
    ones16 = sb([128, 128], BF16, name="ones16")
    mhalf = sb([128, 8], name="mhalf")
    ts("dve", ident[:], io[:], 0.0, None, ALU.is_equal, reads=[io], writes=[ident])
    ts("dve", ident16[:], io[:], 0.0, None, ALU.is_equal, reads=[io], writes=[ident16])
    ts("dve", Umat[:], io[:], 0.0, None, ALU.is_ge, reads=[io], writes=[Umat])
    ts("dve", SLmat[:], io[:], 0.0, None, ALU.is_lt, reads=[io], writes=[SLmat])
    ts("dve", NEGs[:], io[:], 0.0, NEG, ALU.is_ge, ALU.mult, reads=[io], writes=[NEGs])
    ts("dve", NEGsT[:], io[:], 0.0, NEG, ALU.is_le, ALU.mult, reads=[io], writes=[NEGsT])
    ts("dve", NEGC[:], io[:], 0.0, -1e30, ALU.is_gt, ALU.mult, reads=[io], writes=[NEGC])
    S.op("pool", lambda e: e.memset(ones32[:], 1.0), writes=[ones32])
    S.op("pool", lambda e: e.memset(ones16[:], 1.0), writes=[ones16])
    S.op("pool", lambda e: e.memset(mhalf[:], -0.5), writes=[mhalf])
    pow2 = sb([128, NBIS + 1], name="pow2")
    for k in range(NBIS + 1):
        S.op("pool", lambda e, k=k: e.memset(pow2[:, k:k + 1], 0.5 ** (k + 1)), writes=[pow2])

    PT2a = ps([128, 512], name="PT2a")
    PT2b = ps([128, 512], name="PT2b")
    PT2h = [PT2a, PT2b]
    rotg = [0]

    def pabG():
        rotg[0] ^= 1
        return PT2a if rotg[0] else PT2b
    PA = ps([128, 512], name="PA")
    PB = ps([128, 512], name="PB")
    PC = ps([128, 512], name="PC")
    PO = ps([128, 512], name="PO")
    PO2 = ps([128, 512], name="PO2")
    PS_ = ps([128, 512], name="PS_")
    rot = [0]

    def pab():
        rot[0] ^= 1
        return PA if rot[0] else PB

    stage = [sb([128, 8, 256], name="stage0"), sb([128, 8, 256], name="stage1")]
    w16 = sb([128, 8, NW16], BF16, name="w16")
    widx = sb([128, 8, NIDX], name="widx")
    wout16 = sb([128, 8, 1024], BF16, name="wout16")
    wuv16 = sb([128, 4, 128], BF16, name="wuv16")
    S.dma(widx[:], widx_d, writes=[widx])
    si = 0
    for c0 in range(0, NW16, 256):
        w = min(256, NW16 - c0)
        st = stage[si % 2]; si += 1
        S.dma(st[:, :, 0:w], win_d[:, :, c0:c0 + w], writes=[st])
        cp("dve" if si % 2 else "pool", w16[:, :, c0:c0 + w], st[:, :, 0:w], reads=[st], writes=[w16])
    for c0 in range(0, 1024, 256):
        st = stage[si % 2]; si += 1
        S.dma(st[:], wout_d[:, :, c0:c0 + 256], writes=[st])
        cp("dve" if si % 2 else "pool", wout16[:, :, c0:c0 + 256], st[:], reads=[st], writes=[wout16])
    st = stage[si % 2]; si += 1
    S.dma(st[:, 0:4, 0:128], wuv_d, writes=[st])
    cp("dve", wuv16[:], st[:, 0:4, 0:128], reads=[st], writes=[wuv16])

    st = stage[si % 2]; si += 1
    S.dma(st[:, :, 0:128], msk_d, writes=[st])
    msk = sb([128, 8, 128], BF16, name="msk")
    cp("dve", msk[:], st[:, :, 0:128], reads=[st], writes=[msk])
    convw = sb([128, 12, 4], name="convw")
    S.dma(convw[:], conv_d, writes=[convw])
    diagw = sb([128, 12, 4, 128], BF16, name="diagw")
    for ch in range(12):
        for j in range(4):
            ts("dve" if (ch + j) % 2 else "pool", diagw[:, ch, j, :], ident[:], convw[:, ch, j:j + 1], None, ALU.mult,
               reads=[ident, convw], writes=[diagw])

    def bcast_row(src, n, name):
        t = sb([128, n], name=name)
        S.dma(t[:], src.partition_broadcast(128), writes=[t])
        return t
    alogB = bcast_row(alog_d, 4, "alogB")
    dtbB = bcast_row(dtb_d, 4, "dtbB")
    ggdnB = bcast_row(ggdn_d, 128, "ggdnB")
    gkvB = bcast_row(gkv_d, 128, "gkvB")
    rbB = bcast_row(rb_d, 128, "rbB")
    negA = sb([128, 4], name="negA")
    act(negA[:], alogB[:], AF.Exp, reads=[alogB], writes=[negA])
    ts("dve", negA[:], negA[:], -1.0, None, ALU.mult, reads=[negA], writes=[negA])
    ggdnH = ggdnB

    cT = sb([128, 8, NBC], name="cT")
    S.dma(cT[:], cT_d, writes=[cT])
    sc = sb([128, 8, NBC], name="sc")
    act(sc[:], cT[:], AF.Silu, reads=[cT], writes=[sc])
    bcol = sb([128, 24], name="bcol")
    S.dma(bcol[:], bcol_d, writes=[bcol])
    gpre = sb([128, 8], name="gpre")
    S.dma(gpre[:], gpre_d, writes=[gpre])
    shiftc = sb([128, 8, NBC], name="shiftc")
    Gc = sb([128, 8, NBC], name="Gc")
    GATE = sb([128, 1024], name="GATE")
    for n8 in range(8):
        st = stage[si % 2]; si += 1
        S.dma(st[:], wada_d[:, :, n8 * 256:(n8 + 1) * 256], writes=[st])
        for q2 in range(2):
            dch = (n8 % 4) * 2 + q2
            for kc in range(8):
                mm(PC[:, q2 * 4:q2 * 4 + 4], st[:, kc, q2 * 128:(q2 + 1) * 128], sc[:, kc, :],
                   start=(kc == 0), stop=(kc == 7), reads=[st, sc], writes=[PC])
            dst = shiftc if n8 < 4 else Gc
            ts("dve", dst[:, dch, :], PC[:, q2 * 4:q2 * 4 + 4], bcol[:, n8 * 2 + q2:n8 * 2 + q2 + 1], None, ALU.add,
               reads=[PC, bcol], writes=[dst])
    ts("dve", Gc[:], Gc[:], 1.0, None, ALU.add, reads=[Gc], writes=[Gc])
    tt("dve", Gc[:], Gc[:], gpre[:].unsqueeze(2).to_broadcast([128, 8, NBC]), ALU.mult, reads=[Gc, gpre], writes=[Gc])

    bk = t5_bucket_np(np.arange(256))
    lo_b = [int(np.argmax(bk >= b)) if (bk >= b).any() else 100000 for b in range(32)]
    rb3 = rbB[:].rearrange("p (b h) -> p b h", h=4)
    dlt = sb([128, 32, 4], name="dlt")
    cp("dve", dlt[:, 0:1, :], rb3[:, 0:1, :], reads=[rbB], writes=[dlt])
    tt("dve", dlt[:, 1:32, :], rb3[:, 1:32, :], rb3[:, 0:31, :], ALU.subtract, reads=[rbB], writes=[dlt])
    EB = [sb([128, 4, 128], BF16, name=f"EB{t}") for t in range(2)]
    EBf = view(rtmp2, rtmp2[:, 0:128])
    rtmp = sb([128, 512], name="rtmp")
    gUb = [sb([128, 128], name=f"gU{i}") for i in range(2)]
    distT = gUb[0]
    tmpb = gUb[1]
    for typ in range(2):
        ts("dve", distT[:], io[:], float(128 * typ), None, ALU.add, reads=[io], writes=[distT])
        for h in range(4):
            acc = EBf
            ts("dve", acc[:], ones32[:], dlt[:, 0, h:h + 1], rb3[:, 31, h:h + 1], ALU.mult, ALU.subtract,
               reads=[ones32, dlt, rbB], writes=[acc])
            for bb in range(1, 32):
                if lo_b[bb] > 255:
                    continue
                ts("dve", tmpb[:], distT[:], float(lo_b[bb]) - 0.5, dlt[:, bb, h:h + 1], ALU.is_ge, ALU.mult,
                   reads=[distT, dlt], writes=[tmpb])
                tt("dve", acc[:], acc[:], tmpb[:], ALU.add, reads=[acc, tmpb], writes=[acc])
            ts("dve", acc[:], acc[:], 128.0 ** 0.5, None, ALU.mult, reads=[acc], writes=[acc])
            cp("dve", EB[typ][:, h, :], acc[:], reads=[acc], writes=[EB[typ]])

    xt = sb([128, 1024], name="xt")
    junkA = big4; xs = big4; otmp = big4
    hT32 = sb([128, 8, 128], name="hT32")
    hT16 = sb([128, 8, 128], BF16, name="hT16")
    uT = sb([128, 12, 131], BF16, name="uT")
    qkvs = sb([128, 1536], BF16, name="qkvs")
    col = lambda n, name: sb([128, n], name=name)
    ssq1 = col(1, "ssq1"); rstd1 = col(1, "rstd1"); ssqP2 = col(2, "ssqP2"); ssqP = col(1, "ssqP"); rstdP = col(1, "rstdP")
    xs2 = sb([128, 512], name="xs2")
    ssq8 = col(8, "ssq8"); rs8 = col(8, "rs8")
    ba = col(8, "ba"); beta = col(4, "beta"); gcol = col(4, "gcol"); gc = col(4, "gc"); glB = col(4, "glB")
    egc = col(4, "egc"); ekg = col(4, "ekg"); egl = col(4, "egl"); tmp4 = col(4, "tmp4"); nbeta = col(4, "nbeta")
    cf = {n: col(4, "cf_" + n) for n in ("kbg", "kg", "qg")}
    khat = sb([128, 4, 128], BF16, name="khat"); qhat = sb([128, 4, 128], BF16, name="qhat")
    qg = sb([128, 4, 128], BF16, name="qg"); kbg = sb([128, 4, 128], BF16, name="kbg")
    kg = sb([128, 4, 128], BF16, name="kg"); vb = sb([128, 4, 128], BF16, name="vb")
    khT = sb([128, 4, 128], BF16, name="khT"); qhT = sb([128, 4, 128], BF16, name="qhT"); qgT = sb([128, 4, 128], BF16, name="qgT")
    Es = sb([128, 4, 128], BF16, name="Es"); EsT = sb([128, 4, 128], BF16, name="EsT")
    Xb = [sb([128, 4, 128], BF16, name="X0")]
    XTb = [sb([128, 4, 128], BF16, name="XT0")]
    Tb = [sb([128, 4, 128], BF16, name=f"T{i}") for i in range(2)]
    TTb = [sb([128, 4, 128], BF16, name=f"TT{i}") for i in range(2)]
    Wp = khat
    attnT = sb([128, 4, 128], BF16, name="attnT")
    negwT = qhat
    vnew = qg
    S32 = sb([128, 4, 128], name="S32"); S16 = sb([128, 4, 128], BF16, name="S16")
    zas = sb([128, 512], BF16, name="zas"); G1 = zas
    osq4 = col(4, "osq4"); ors4 = col(4, "ors4")
    og = sb([128, 4, 128], BF16, name="og"); ogT = sb([128, 4, 128], BF16, name="ogT")
    qbT = sb([128, 4, 128], BF16, name="qbT"); szbT = sb([128, 4, 128], BF16, name="szbT")
    iqT = sb([128, 4, 128], name="iqT"); iw = col(8, "iw")
    ckvn = sb([128, NT, 128], BF16, name="ckvn"); ckvnT = sb([128, L], BF16, name="ckvnT")
    ikTb = stage[1]
    ikT = ikTb[:].rearrange("p a b -> p (a b)")
    score = stage[0]
    scoreF = score[:].rearrange("p a b -> p (a b)")
    mk = sb([128, L], BF16, name="mk")
    junkD = mk
    junkDF = mk
    lo = col(1, "lo"); hw0 = col(1, "hw0"); hwk = col(NBIS + 1, "hwk"); nhwk = col(NBIS + 1, "nhwk"); mid = col(1, "mid"); mid2 = col(1, "mid2"); sgnc = col(1, "sgnc"); cbc = col(1, "cbc"); cntc = col(1, "cntc"); tstep = col(1, "tstep")
    Eb = [sb([128, 4, 128], BF16, name=f"Eb{i}") for i in range(2)]
    negmk = sb([128, NT, 128], BF16, name="negmk"); mkT = negmk
    obT = sb([128, 4, 128], BF16, name="obT")
    rden = rtmp; Rg = rden
    ygT = sb([128, 4, 128], BF16, name="ygT")
    msq = col(2, "msq"); mrs = col(1, "mrs")

    def rsqrt_col(dst, src, n, scale, eps):
        ts("dve", dst[:, 0:n], src[:, 0:n], scale, eps, ALU.mult, ALU.add, reads=[src], writes=[dst])
        tt("pool", dst[:, 0:n], dst[:, 0:n], mhalf[:, 0:n], ALU.pow, reads=[dst, mhalf], writes=[dst])

    def pre_gen(b, i):
        t0 = i * 128
        uc = uT
        S.dma(xt[:], x_d[b, t0:t0 + 128, :], writes=[xt])
        yield
        for hf in range(2):
            act(xs2[:], xt[:, hf * 512:(hf + 1) * 512], AF.Square, reads=[xt], writes=[xs2, ssqP2], accum_out=ssqP2[:, hf:hf + 1])
        tt("pool", ssqP[:], ssqP2[:, 0:1], ssqP2[:, 1:2], ALU.add, reads=[ssqP2], writes=[ssqP])
        rsqrt_col(rstdP, ssqP, 1, 1.0 / D, EPS)
        yield
        for hf in range(2):
            ts("dve", xs2[:], xt[:, hf * 512:(hf + 1) * 512], rstdP[:, 0:1], None, ALU.mult, reads=[xt, rstdP], writes=[xs2])
            for c4 in range(4):
                tr(PC[:, c4 * 128:(c4 + 1) * 128], xs2[:, c4 * 128:(c4 + 1) * 128], ident[:], reads=[xs2, ident], writes=[PC])
            for c4 in range(4):
                c = hf * 4 + c4
                ts("dve", hT32[:, c, :], PC[:, c4 * 128:(c4 + 1) * 128], Gc[:, c, b:b + 1], shiftc[:, c, b:b + 1], ALU.mult, ALU.add,
                   reads=[PC, Gc, shiftc], writes=[hT32])
            yield
        cp("pool", hT16[:], hT32[:], reads=[hT32], writes=[hT16])
        if b == 0 and i == 0:
            dump("hT", hT32[:], hT32)
        yield
        for g3 in range(3):
            for q4 in range(4):
                ch = g3 * 4 + q4
                for kc in range(8):
                    mm(PC[:, q4 * 128:(q4 + 1) * 128], w16[:, kc, O_QKV + ch * 128:O_QKV + (ch + 1) * 128], hT16[:, kc, :],
                       start=(kc == 0), stop=(kc == 7), reads=[w16, hT16], writes=[PC])
            cp("dve", uc[:, g3 * 4:(g3 + 1) * 4, 3:131], PC[:].rearrange("p (a b) -> p a b", a=4), reads=[PC], writes=[uc])
            yield
        for g3 in range(3):
            for q4 in range(4):
                ch = g3 * 4 + q4
                for j in range(4):
                    mm(PC[:, q4 * 128:(q4 + 1) * 128], uc[:, ch, j:j + 128], diagw[:, ch, j, :],
                       start=(j == 0), stop=(j == 3), reads=[uc, diagw], writes=[PC])
            act(qkvs[:, g3 * 512:(g3 + 1) * 512], PC[:], AF.Silu, reads=[PC], writes=[qkvs])
            yield
        cp("pool", uc[:, :, 0:3], uc[:, :, 128:131], reads=[uc], writes=[uc])
        if b == 0 and i == 0:
            dump("qkvs", qkvs[:], qkvs)
        for hf in range(2):
            tt("dve", xs2[:], qkvs[:, hf * 512:(hf + 1) * 512], qkvs[:, hf * 512:(hf + 1) * 512], ALU.mult, reads=[qkvs], writes=[xs2])
            S.op("dve", lambda e, hf=hf: e.tensor_reduce(ssq8[:, hf * 4:(hf + 1) * 4], xs2[:].rearrange("p (a b) -> p a b", a=4), axis=AX.X, op=ALU.add),
                 reads=[xs2], writes=[ssq8])
        rsqrt_col(rs8, ssq8, 8, 1.0, EPS)
        ts("dve", rs8[:, 0:4], rs8[:, 0:4], 128.0 ** -0.5, None, ALU.mult, reads=[rs8], writes=[rs8])
        yield
        for c4 in range(4):
            for kc in range(8):
                mm(PC[:, c4 * 128:(c4 + 1) * 128], widx[:, kc, c4 * 128:(c4 + 1) * 128], hT32[:, kc, :],
                   start=(kc == 0), stop=(kc == 7), reads=[widx, hT32], writes=[PC])
            if c4 % 2:
                yield
        cp("dve", iqT[:], PC[:].rearrange("p (a b) -> p a b", a=4), reads=[PC], writes=[iqT])
        yield
        for kc in range(8):
            mm(PC[:, 0:128], widx[:, kc, 512:640], hT32[:, kc, :], start=(kc == 0), stop=(kc == 7), reads=[widx, hT32], writes=[PC])
        cp("dve", ikT[:, t0:t0 + 128], PC[:, 0:128], reads=[PC], writes=[ikTb])
        for kc in range(8):
            mm(PC[:, 0:8], hT32[:, kc, :], widx[:, kc, 640:648], start=(kc == 0), stop=(kc == 7), reads=[hT32, widx], writes=[PC])
        ts("dve", iw[:], PC[:, 0:8], (8.0 * 64.0) ** -0.5, None, ALU.mult, reads=[PC], writes=[iw])
        yield

    tix = 0
    screp = xt[:].rearrange("p (a b) -> p a b", a=8)
    for b in range(NBC):
        S.op("pool", lambda e: e.memset(S32[:], 0.0), writes=[S32])
        S.op("pool", lambda e: e.memset(S16[:], 0.0), writes=[S16])
        S.op("pool", lambda e: e.memset(uT[:, :, 0:3], 0.0), writes=[uT])
        for kc in range(8):
            cp("dve" if kc % 2 else "pool", screp[:, kc, :], sc[:, kc, b:b + 1].to_broadcast([128, 128]), reads=[sc], writes=[xt])
        for g4 in range(4):
            st = stage[g4 % 2]
            g0 = g4 * 256
            S.dma(st[:], wada_d[:, :, 2048 + g0:2048 + g0 + 256], writes=[st])
            S.dma(rtmp[:, 0:256], bgate_d[:, g0:g0 + 256].partition_broadcast(128), writes=[rtmp])
            S.dma(rtmp[:, 256:512], gpost_d[:, g0:g0 + 256].partition_broadcast(128), writes=[rtmp])
            P = pab()
            for kc in range(8):
                mm(P[:, 0:256], screp[:, kc, :], st[:, kc, :], start=(kc == 0), stop=(kc == 7), reads=[xt, st], writes=[P])
            tt("dve", GATE[:, g0:g0 + 256], P[:, 0:256], rtmp[:, 0:256], ALU.add, reads=[P, rtmp], writes=[GATE])
            tt("pool", GATE[:, g0:g0 + 256], GATE[:, g0:g0 + 256], rtmp[:, 256:512], ALU.mult, reads=[GATE, rtmp], writes=[GATE])
        for i in range(NT):
            t0 = i * 128
            uc = uT
            if i == 0:
                for _ in pre_gen(b, 0):
                    pass
            for kc in range(8):
                mm(PC[:, 0:8], hT16[:, kc, :], w16[:, kc, O_B:O_B + 8], start=(kc == 0), stop=(kc == 7), reads=[hT16, w16], writes=[PC])
            for kc in range(8):
                mm(PC[:, 128:256], hT16[:, kc, :], w16[:, kc, O_CKV:O_CKV + 128], start=(kc == 0), stop=(kc == 7), reads=[hT16, w16], writes=[PC])
            cp("act", ba[:], PC[:, 0:8], reads=[PC], writes=[ba])
            act(junkA[:, 0:128], PC[:, 128:256], AF.Square, reads=[PC], writes=[junkA, ssq1], accum_out=ssq1[:, 0:1])
            rsqrt_col(rstd1, ssq1, 1, 1.0 / 128, EPS)
            stt(ckvn[:, i, :], PC[:, 128:256], rstd1[:, 0:1], gkvB[:], ALU.mult, ALU.mult, reads=[PC, rstd1, gkvB], writes=[ckvn])
            Pq = pab()
            Pq16 = Pq[:].bitcast(BF16)
            tr(Pq16[:, 0:128], ckvn[:, i, :], ident16[:], reads=[ckvn, ident16], writes=[Pq])
            cp("act", ckvnT[:, t0:t0 + 128], Pq16[:, 0:128], reads=[Pq], writes=[ckvnT])
            P = pab()
            for kc in range(8):
                mm(P[:], hT16[:, kc, :], w16[:, kc, O_ZA:O_ZA + 512], start=(kc == 0), stop=(kc == 7), reads=[hT16, w16], writes=[P])
            act(zas[:], P[:], AF.Silu, reads=[P], writes=[zas])
            tt("pool", zas[:].rearrange("p (h v) -> p h v", h=4), zas[:].rearrange("p (h v) -> p h v", h=4),
               ggdnH[:].unsqueeze(1).to_broadcast([128, 4, 128]), ALU.mult, reads=[zas, ggdnH], writes=[zas])
            P = pab()
            for h in range(4):
                for kc in range(8):
                    mm(P[:, h * 128:(h + 1) * 128], w16[:, kc, O_QB + h * 128:O_QB + (h + 1) * 128], hT16[:, kc, :],
                       start=(kc == 0), stop=(kc == 7), reads=[w16, hT16], writes=[P])
            cp("act", qbT[:], P[:].rearrange("p (a b) -> p a b", a=4), reads=[P], writes=[qbT])
            P = pab()
            for h in range(4):
                for kc in range(8):
                    mm(P[:, h * 128:(h + 1) * 128], w16[:, kc, O_ZB + h * 128:O_ZB + (h + 1) * 128], hT16[:, kc, :],
                       start=(kc == 0), stop=(kc == 7), reads=[w16, hT16], writes=[P])
            act(szbT[:], P[:].rearrange("p (a b) -> p a b", a=4), AF.Silu, reads=[P], writes=[szbT])
            def gdn_stream():
                act(beta[:], ba[:, 0:4], AF.Exp, reads=[ba], writes=[beta], scale=-1.0)
                yield
                ts("dve", beta[:], beta[:], 1.0, None, ALU.add, reads=[beta], writes=[beta])
                yield
                S.op("dve", lambda e: e.reciprocal(beta[:], beta[:]), reads=[beta], writes=[beta])
                yield
                ts("dve", nbeta[:], beta[:], -1.0, None, ALU.mult, reads=[beta], writes=[nbeta])
                yield
                tt("dve", tmp4[:], ba[:, 4:8], dtbB[:], ALU.add, reads=[ba, dtbB], writes=[tmp4])
                yield
                act(tmp4[:], tmp4[:], AF.Exp, reads=[tmp4], writes=[tmp4])
                yield
                act(tmp4[:], tmp4[:], AF.Ln, reads=[tmp4], writes=[tmp4], bias=1.0)
                yield
                tt("dve", gcol[:], tmp4[:], negA[:], ALU.mult, reads=[tmp4, negA], writes=[gcol])
                yield
                mm(PC[:, 16:20], Umat[:], gcol[:], reads=[Umat, gcol], writes=[PC])
                yield
                mm(PC[:, 20:24], ones32[:], gcol[:], reads=[ones32, gcol], writes=[PC])
                yield
                cp("dve", gc[:], PC[:, 16:20], reads=[PC], writes=[gc])
                yield
                cp("dve", glB[:], PC[:, 20:24], reads=[PC], writes=[glB])
                yield
                act(egc[:], gc[:], AF.Exp, reads=[gc], writes=[egc])
                yield
                act(egl[:], glB[:], AF.Exp, reads=[glB], writes=[egl])
                yield
                tt("dve", tmp4[:], glB[:], gc[:], ALU.subtract, reads=[glB, gc], writes=[tmp4])
                yield
                act(ekg[:], tmp4[:], AF.Exp, reads=[tmp4], writes=[ekg])
                yield
                tt("dve", cf["kbg"][:], rs8[:, 4:8], beta[:], ALU.mult, reads=[rs8, beta], writes=[cf["kbg"]])
                yield
                tt("dve", cf["kbg"][:], cf["kbg"][:], egc[:], ALU.mult, reads=[cf["kbg"], egc], writes=[cf["kbg"]])
                yield
                tt("dve", cf["kg"][:], rs8[:, 4:8], ekg[:], ALU.mult, reads=[rs8, ekg], writes=[cf["kg"]])
                yield
                tt("dve", cf["qg"][:], rs8[:, 0:4], egc[:], ALU.mult, reads=[rs8, egc], writes=[cf["qg"]])
                yield
                q3 = qkvs[:, 0:512].rearrange("p (h d) -> p h d", h=4)
                yield
                k3 = qkvs[:, 512:1024].rearrange("p (h d) -> p h d", h=4)
                yield
                v3 = qkvs[:, 1024:1536].rearrange("p (h d) -> p h d", h=4)
                yield
                bc = lambda c, lo_=0: c[:, lo_:lo_ + 4].unsqueeze(2).to_broadcast([128, 4, 128])
                yield
                tt("dve", khat[:], k3, bc(rs8, 4), ALU.mult, reads=[qkvs, rs8], writes=[khat])
                yield
                tt("pool", qhat[:], q3, bc(rs8, 0), ALU.mult, reads=[qkvs, rs8], writes=[qhat])
                yield
                tt("dve", qg[:], q3, bc(cf["qg"]), ALU.mult, reads=[qkvs, cf["qg"]], writes=[qg])
                yield
                tt("pool", kbg[:], k3, bc(cf["kbg"]), ALU.mult, reads=[qkvs, cf["kbg"]], writes=[kbg])
                yield
                tt("dve", kg[:], k3, bc(cf["kg"]), ALU.mult, reads=[qkvs, cf["kg"]], writes=[kg])
                yield
                tt("pool", vb[:], v3, bc(beta), ALU.mult, reads=[qkvs, beta], writes=[vb])
                yield "M"
                for src, dst in ((khat, khT), (qhat, qhT), (qg, qgT)):
                    P = pabG()
                    P16 = P[:].bitcast(BF16)
                    for h in range(4):
                        tr(P16[:, h * 128:(h + 1) * 128], src[:, h, :], ident16[:], reads=[src, ident16], writes=[P])
                    cp("act", dst[:], P16[:, 0:512].rearrange("p (a b) -> p a b", a=4), reads=[P], writes=[dst])
                yield
                PD = pabG(); PDT = pabG()
                yield
                for h in range(4):
                    gU = gUb[h % 2]
                    ts("dve" if h % 2 else "pool", gU[:], Umat[:], gcol[:, h:h + 1], None, ALU.mult, reads=[Umat, gcol], writes=[gU])
                    mm(PD[:, h * 128:(h + 1) * 128], gU[:], SLmat[:], start=True, stop=False, reads=[gU, SLmat], writes=[PD])
                    mm(PD[:, h * 128:(h + 1) * 128], ident[:], NEGs[:], start=False, stop=True, reads=[ident, NEGs], writes=[PD])
                    mm(PDT[:, h * 128:(h + 1) * 128], SLmat[:], gU[:], start=True, stop=False, reads=[gU, SLmat], writes=[PDT])
                    mm(PDT[:, h * 128:(h + 1) * 128], ident[:], NEGsT[:], start=False, stop=True, reads=[ident, NEGsT], writes=[PDT])
                yield
                act(Es[:], PD[:].rearrange("p (a b) -> p a b", a=4), AF.Exp, reads=[PD], writes=[Es])
                yield
                act(EsT[:], PDT[:].rearrange("p (a b) -> p a b", a=4), AF.Exp, reads=[PDT], writes=[EsT])
                yield
                if b == 0 and i == 0:
                    dump("Es", Es[:].rearrange("p a b -> p (a b)"), Es)
                    dump("beta4", beta[:], beta); dump("gc4", gc[:], gc); dump("rs8", rs8[:], rs8)
                yield
                P = pabG()
                yield
                for h in range(4):
                    mm(P[:, h * 128:(h + 1) * 128], khT[:, h, :], khT[:, h, :], reads=[khT], writes=[P])
                yield
                X, XT = Xb[0], XTb[0]
                yield
                for h in range(4):
                    stt(X[:, h, :], P[:, h * 128:(h + 1) * 128], nbeta[:, h:h + 1], Es[:, h, :], ALU.mult, ALU.mult,
                        reads=[P, nbeta, Es], writes=[X])
                yield
                if b == 0 and i == 0:
                    dump("X0", X[:].rearrange("p a b -> p (a b)"), X)
                yield
                P = pabG()
                yield
                for h in range(4):
                    mm(P[:, h * 128:(h + 1) * 128], khT[:, h, :], qhT[:, h, :], reads=[khT, qhT], writes=[P])
                yield
                tt("pool", EsT[:], EsT[:], ident16[:].unsqueeze(1).to_broadcast([128, 4, 128]), ALU.add, reads=[EsT, ident16], writes=[EsT])
                yield
                tt("dve", attnT[:], P[:].rearrange("p (a b) -> p a b", a=4), EsT[:], ALU.mult, reads=[P, EsT], writes=[attnT])
                yield
                P = pabG()
                yield
                P16 = P[:].bitcast(BF16)
                yield
                for h in range(4):
                    tr(P16[:, h * 128:(h + 1) * 128], X[:, h, :], ident16[:], reads=[X, ident16], writes=[P])
                yield
                cp("act", XT[:], P16[:, 0:512].rearrange("p (a b) -> p a b", a=4), reads=[P], writes=[XT])
                yield
                bcm = lambda ls: msk[:, ls, :].unsqueeze(1).to_broadcast([128, 4, 128])
                yield
                Tc, TT = Tb[0], TTb[0]
                yield
                tt("pool", Tc[:], X[:], bcm(0), ALU.mult, reads=[X, msk], writes=[Tc])
                yield
                tt("pool", Tc[:], Tc[:], ident16[:].unsqueeze(1).to_broadcast([128, 4, 128]), ALU.add, reads=[Tc, ident16], writes=[Tc])
                yield
                tt("dve", TT[:], XT[:], bcm(7), ALU.mult, reads=[XT, msk], writes=[TT])
                yield
                tt("dve", TT[:], TT[:], ident16[:].unsqueeze(1).to_broadcast([128, 4, 128]), ALU.add, reads=[TT, ident16], writes=[TT])
                yield
                gen = 0
                yield
                for ls in range(1, 7):
                    Tn, TTn = Tb[1 - gen], TTb[1 - gen]
                    P1 = pabG()
                    for h in range(4):
                        mm(P1[:, h * 128:(h + 1) * 128], XT[:, h, :], Tc[:, h, :], reads=[XT, Tc], writes=[P1])
                    tt("dve", Wp[:], P1[:].rearrange("p (a b) -> p a b", a=4), bcm(ls), ALU.mult, reads=[P1, msk], writes=[Wp])
                    if ls < 6:
                        P2 = pabG()
                        for h in range(4):
                            mm(P2[:, h * 128:(h + 1) * 128], TT[:, h, :], Wp[:, h, :], reads=[TT, Wp], writes=[P2])
                        tt("dve", Tn[:], P2[:].rearrange("p (a b) -> p a b", a=4), Tc[:], ALU.add, reads=[P2, Tc], writes=[Tn])
                    P3 = PS_
                    for h in range(4):
                        mm(P3[:, h * 128:(h + 1) * 128], Wp[:, h, :], TT[:, h, :], reads=[Wp, TT], writes=[P3])
                    tt("dve", TTn[:], P3[:].rearrange("p (a b) -> p a b", a=4), TT[:], ALU.add, reads=[P3, TT], writes=[TTn])
                    Tc, TT = Tn, TTn
                    gen = 1 - gen
                    yield
                yield
                if b == 0 and i == 0:
                    dump("TTf", TT[:].rearrange("p a b -> p (a b)"), TT)
                    dump("attnT0", attnT[:].rearrange("p a b -> p (a b)"), attnT)
                yield
                P = pabG()
                yield
                for h in range(4):
                    mm(P[:, h * 128:(h + 1) * 128], kbg[:, h, :], TT[:, h, :], reads=[kbg, TT], writes=[P])
                yield
                ts("dve", negwT[:], P[:].rearrange("p (a b) -> p a b", a=4), -1.0, None, ALU.mult, reads=[P], writes=[negwT])
                yield
                PV = pabG()
                for h in range(4):
                    hs = slice(h * 128, (h + 1) * 128)
                    mm(PV[:, hs], TT[:, h, :], vb[:, h, :], start=True, stop=False, reads=[TT, vb], writes=[PV])
                    mm(PV[:, hs], negwT[:, h, :], S16[:, h, :], start=False, stop=True, reads=[negwT, S16], writes=[PV])
                yield
                cp("act", vnew[:], PV[:].rearrange("p (a b) -> p a b", a=4), reads=[PV], writes=[vnew])
                yield
                if b == 0 and i == 0:
                    dump("vnew0", vnew[:].rearrange("p a b -> p (a b)"), vnew)
                yield
                for h in range(4):
                    hs = slice(h * 128, (h + 1) * 128)
                    mm(PS_[:, hs], qgT[:, h, :], S16[:, h, :], start=True, stop=False, reads=[qgT, S16], writes=[PS_])
                    mm(PS_[:, hs], attnT[:, h, :], vnew[:, h, :], start=False, stop=True, reads=[attnT, vnew], writes=[PS_])
                yield
                PDS = pabG()
                for h in range(4):
                    hs = slice(h * 128, (h + 1) * 128)
                    mm(PDS[:, hs], kg[:, h, :], vnew[:, h, :], reads=[kg, vnew], writes=[PDS])
                yield
                for h in range(4):
                    hs = slice(h * 128, (h + 1) * 128)
                    stt(S32[:, h, :], S32[:, h, :], egl[:, h:h + 1], PDS[:, hs], ALU.mult, ALU.add, reads=[S32, egl, PDS], writes=[S32])
                yield
                cp("pool", S16[:], S32[:], reads=[S32], writes=[S16])
                yield
                act(junkA[:, 0:512], PS_[:], AF.Square, reads=[PS_], writes=[junkA])
                yield
                S.op("dve", lambda e: e.tensor_reduce(osq4[:], junkA[:, 0:512].rearrange("p (a b) -> p a b", a=4), axis=AX.X, op=ALU.add),
                     reads=[junkA], writes=[osq4])
                yield
                rsqrt_col(ors4, osq4, 4, 1.0 / 128, EPS)
                yield
                for h in range(4):
                    hs = slice(h * 128, (h + 1) * 128)
                    stt(og[:, h, :], PS_[:, hs], ors4[:, h:h + 1], G1[:, hs], ALU.mult, ALU.mult, reads=[PS_, ors4, G1], writes=[og])
                yield
                if b == 0 and i <= 1:
                    dump(f"og{i}", og[:].rearrange("p a b -> p (a b)"), og)
                yield
                P = pabG()
                yield
                P16 = P[:].bitcast(BF16)
                yield
                for h in range(4):
                    tr(P16[:, h * 128:(h + 1) * 128], og[:, h, :], ident16[:], reads=[og, ident16], writes=[P])
                yield
                cp("act", ogT[:], P16[:, 0:512].rearrange("p (a b) -> p a b", a=4), reads=[P], writes=[ogT])

                yield
            def dsa_stream():
                n = t0 + 128
                yield
                for s0 in range(0, n, 512):
                    w = min(512, n - s0)
                    for h in range(8):
                        P = pab()
                        pr = slice((h % 2) * 64, (h % 2) * 64 + 64)
                        mm(P[:, 0:w], iqT[pr, h // 2, :], ikT[pr, s0:s0 + w], reads=[iqT, ikTb], writes=[P])
                        if h == 0:
                            ts("dve", scoreF[:, s0:s0 + w], P[:, 0:w], 0.0, iw[:, 0:1], ALU.max, ALU.mult, reads=[P, iw], writes=[score])
                        else:
                            rt = rtmp if h % 2 else rtmp2
                            act(rt[:, 0:w], P[:, 0:w], AF.Relu, reads=[P], writes=[rt])
                            stt(scoreF[:, s0:s0 + w], rt[:, 0:w], iw[:, h:h + 1], scoreF[:, s0:s0 + w], ALU.mult, ALU.add,
                                reads=[rt, iw, score], writes=[score])
                        yield
                yield
                tt("dve", scoreF[:, t0:t0 + 128], scoreF[:, t0:t0 + 128], NEGC[:], ALU.add, reads=[score, NEGC], writes=[score])
                yield
                if b == 0 and i == 2:
                    dump("score2", scoreF[:, 0:384], score)
                yield
                yield "B"
                if i >= 2:
                    S.op("dve", lambda e: e.tensor_reduce(lo[:], scoreF[:, 0:t0], axis=AX.X, op=ALU.min), reads=[score], writes=[lo])
                    S.op("dve", lambda e: e.tensor_reduce(hw0[:], scoreF[:, 0:n], axis=AX.X, op=ALU.max), reads=[score], writes=[hw0])
                    tt("dve", hw0[:], hw0[:], lo[:], ALU.subtract, reads=[hw0, lo], writes=[hw0])
                    ts("dve", hw0[:], hw0[:], 1.0001, 1e-6, ALU.mult, ALU.add, reads=[hw0], writes=[hw0])
                    ts("dve", hwk[:], pow2[:], hw0[:, 0:1], None, ALU.mult, reads=[pow2, hw0], writes=[hwk])
                    ts("dve", nhwk[:], hwk[:], -1.0, None, ALU.mult, reads=[hwk], writes=[nhwk])
                    ts("dve", mid[:], lo[:], hwk[:, 0:1], -1.0, ALU.add, ALU.mult, reads=[lo, hwk], writes=[mid])
                    S.op("pool", lambda e: e.memset(cbc[:], float(n) - 511.5), writes=[cbc])
                    nmc, nmn = mid, mid2
                    for k in range(NBIS):
                        act(mk[:, 0:n], scoreF[:, 0:n], AF.Sign, reads=[score, nmc], writes=[junkD, cntc], bias=nmc[:, 0:1],
                            accum_out=cntc[:, 0:1])
                        act(sgnc[:], cntc[:], AF.Sign, reads=[cntc, cbc], writes=[sgnc], bias=cbc[:, 0:1])
                        if k < NBIS - 1:
                            act(nmn[:], sgnc[:], AF.Identity, reads=[sgnc, nhwk, nmc], writes=[nmn], scale=nhwk[:, k + 1:k + 2], bias=nmc[:, 0:1])
                            nmc, nmn = nmn, nmc
                        yield
                    act(nmn[:], nmc[:], AF.Identity, reads=[nmc, nhwk], writes=[nmn], scale=-1.0, bias=nhwk[:, NBIS:NBIS + 1])
                    act(lo[:], sgnc[:], AF.Identity, reads=[sgnc, hwk, nmn], writes=[lo], scale=hwk[:, NBIS:NBIS + 1], bias=nmn[:, 0:1])
                else:
                    S.op("dve", lambda e: e.memset(lo[:], -1e29), writes=[lo])
                yield
                ts("dve", mk[:, 0:n], scoreF[:, 0:n], lo[:, 0:1], None, ALU.is_ge, reads=[score, lo], writes=[mk])
                yield
                if b == 0 and i == 2:
                    dump("thr2", lo[:], lo)
                yield
                for j0 in range(0, i + 1, 4):
                    nj = min(4, i + 1 - j0)
                    P = pab()
                    P16 = P[:].bitcast(BF16)
                    for jj in range(nj):
                        j = j0 + jj
                        tr(P16[:, jj * 128:(jj + 1) * 128], mk[:, j * 128:(j + 1) * 128], ident16[:], reads=[mk, ident16], writes=[P])
                    ts("dve", negmk[:, j0:j0 + nj, :], P16[:, 0:nj * 128].rearrange("p (a b) -> p a b", a=nj), 30000.0, -30000.0, ALU.mult, ALU.add,
                       reads=[P], writes=[negmk])
                yield
                qb2 = qbT[:].rearrange("p a b -> p (a b)")
                yield
                for j in range(i + 1):
                    P = pab()
                    P3v = P[:].rearrange("p (a b) -> p a b", a=4)
                    near = j >= i - 1
                    mm(P3v, ckvnT[:, j * 128:(j + 1) * 128], qbT[:], start=True, stop=False, reads=[ckvnT, qbT], writes=[P])
                    mm(P3v, ident16[:], negmk[:, j, :].unsqueeze(1).to_broadcast([128, 4, 128]), start=False, stop=not near,
                       reads=[ident16, negmk], writes=[P])
                    if near:
                        mm(P3v, ident16[:], EB[i - j][:], start=False, stop=True, reads=[ident16, EB[i - j]], writes=[P])
                    E = Eb[j % 2]
                    act(E[:], P3v, AF.Exp, reads=[P], writes=[E], scale=128.0 ** -0.5)
                    pm2 = E[:].rearrange("p a b -> p (a b)")
                    mm(PO[:], ckvn[:, j, :], pm2, start=(j == 0), stop=(j == i), reads=[ckvn, E], writes=[PO])
                    mm(PO2[:], ones16[:], pm2, start=(j == 0), stop=(j == i), reads=[ones16, E], writes=[PO2])
                    yield
                yield
                cp("act", obT[:], PO[:].rearrange("p (a b) -> p a b", a=4), reads=[PO], writes=[obT])
                yield
                act(rden[:], PO2[:], AF.Ln, reads=[PO2], writes=[rden])
                yield
                act(rden[:], rden[:], AF.Exp, reads=[rden], writes=[rden], scale=-1.0)
                yield
                tt("pool", rden[:], rden[:], szbT[:].rearrange("p a b -> p (a b)"), ALU.mult, reads=[rden, szbT], writes=[rden])
                yield
                PY = pab()
                for h in range(4):
                    hs = slice(h * 128, (h + 1) * 128)
                    mm(PY[:, hs], wuv16[:, h, :], obT[:, h, :], reads=[wuv16, obT], writes=[PY])
                yield
                tt("dve", ygT[:].rearrange("p a b -> p (a b)"), PY[:], Rg[:], ALU.mult, reads=[PY, Rg], writes=[ygT])
                yield
                if b == 0 and i <= 2:
                    dump(f"yg{i}", ygT[:].rearrange("p a b -> p (a b)"), ygT)
                yield
                yield
            streams = [[gdn_stream(), 3], [dsa_stream(), 1]]
            pre = pre_gen(b, i + 1) if i + 1 < NT else None
            seen = set()
            while streams or pre is not None:
                for ent in list(streams):
                    for _ in range(ent[1]):
                        try:
                            seen.add(next(ent[0]))
                        except StopIteration:
                            streams.remove(ent)
                            break
                if pre is not None and (("M" in seen and "B" in seen) or not streams):
                    for _ in range(1):
                        try:
                            next(pre)
                        except StopIteration:
                            pre = None
                            break
            for nh in range(2):
                for c in range(8):
                    lhs = ogT[:, c, :] if c < 4 else ygT[:, c - 4, :]
                    mm(PT2h[nh][:], lhs, wout16[:, c, nh * 512:(nh + 1) * 512],
                       start=(c == 0), stop=(c == 7), reads=[ogT, ygT, wout16], writes=[PT2h[nh]])
            for nh in range(2):
                act(junkA[:, nh * 512:(nh + 1) * 512], PT2h[nh][:], AF.Square, reads=[PT2h[nh]],
                    writes=[junkA, msq], accum_out=msq[:, nh:nh + 1])
            tt("dve", ssq1[:], msq[:, 0:1], msq[:, 1:2], ALU.add, reads=[msq], writes=[ssq1])
            rsqrt_col(mrs, ssq1, 1, 1.0 / D, EPS)
            for nh in range(2):
                sl = slice(nh * 512, (nh + 1) * 512)
                stt(otmp[:, sl], PT2h[nh][:], mrs[:, 0:1], GATE[:, sl], ALU.mult, ALU.mult, reads=[PT2h[nh], mrs, GATE], writes=[otmp])
            xr = negmk[:].rearrange("p a b -> p (a b)").bitcast(F32)
            S.dma(xr, x_d[b, t0:t0 + 128, :], writes=[negmk])
            tt("pool", otmp[:], otmp[:], xr, ALU.add, reads=[otmp, negmk], writes=[otmp])
            S.dma(out_d[b, t0:t0 + 128, :], otmp[:], reads=[otmp])
            tix += 1
    S.finish("sp")
    return nc, S


def _masks():
    i = np.arange(128)[:, None]; j = np.arange(128)[None, :]
    m = np.zeros((128, 8, 128), np.float32)
    for ls in range(7):
        sz = 1 << ls
        m[:, ls, :] = (((i // sz) % 2 == 1) & ((j // sz) == (i // sz) - 1)).astype(np.float32)
    m[:, 7, :] = m[:, 0, :].T
    return m


def _layout(inputs, core):
    f = lambda a: np.ascontiguousarray(a, dtype=np.float32)
    bs = slice(core * NBC, (core + 1) * NBC)
    kp = lambda w: w.reshape(8, 128, -1).transpose(1, 0, 2)
    w_in = inputs["w_in"][0]
    ik = w_in[:, O_IK:O_IK + 64]
    w_idx = np.concatenate([w_in[:, O_IQ:O_IQ + 512], ik, ik, w_in[:, O_IW:O_IW + 8]], axis=1)
    b_ada = inputs["b_ada"][0]
    return {
        "x": f(inputs["x"][bs]),
        "cT": f(inputs["c"][bs].T.reshape(8, 128, NBC).transpose(1, 0, 2)),
        "w_ada": f(kp(inputs["w_ada"][0])),
        "b_col": f(b_ada.reshape(24, 128).T),
        "b_gate": f(b_ada[2048:3072].reshape(1, 1024)),
        "g_pre": f(inputs["g_pre"][0].reshape(8, 128).T),
        "w_in": f(kp(w_in[:, :NW16])),
        "w_idx": f(kp(w_idx)),
        "conv_w": f(inputs["conv_w"][0].reshape(4, 12, 128).transpose(2, 1, 0)),
        "a_log": f(inputs["a_log"].reshape(1, 4)),
        "dt_bias": f(inputs["dt_bias"].reshape(1, 4)),
        "g_gdn": f(inputs["g_gdn"].reshape(1, 128)),
        "g_kv": f(inputs["g_kv"].reshape(1, 128)),
        "w_uv": f(inputs["w_uv"][0].transpose(1, 0, 2)),
        "rel_bias": f(inputs["rel_bias"].reshape(1, 128)),
        "w_out": f(kp(inputs["w_out"][0])),
        "g_post": f(inputs["g_post"].reshape(1, 1024)),
        "masks": _masks(),
    }


def kernel(**inputs):
    inputs = {k: np.asarray(v) for k, v in inputs.items()}
    nc, _ = build()
    in_maps = [_layout(inputs, c) for c in range(8)]
    res = run_bass_kernel_spmd(nc, in_maps, core_ids=list(range(8)))
    return np.concatenate([r["out"] for r in res.results], axis=0).astype(np.float32)
```

```python
import math
import numpy as np
import concourse.bass as bass
import concourse.mybir as mybir
from concourse.bass_utils import run_bass_kernel_spmd

F32 = mybir.dt.float32
BF16 = mybir.dt.bfloat16
AF = mybir.ActivationFunctionType
ALU = mybir.AluOpType
AX = mybir.AxisListType

D = 1024
L = 2048
NBC = 4
NT = 16
EPS = 1e-6
NEG = -30000.0
NBIS = 14


class Res:
    __slots__ = ("name", "w", "r")

    def __init__(self, name):
        self.name = name
        self.w = None
        self.r = {}


class Buf:
    def __init__(self, t, name):
        self.t = t
        self.r = Res(name)

    def __getitem__(self, k):
        return self.t[k]


class Sched:
    def __init__(self, nc, ndma=8):
        self.nc = nc
        self.e = {"pe": nc.tensor, "act": nc.scalar, "dve": nc.vector, "pool": nc.gpsimd, "sp": nc.sync}
        self.sem = {k: nc.alloc_semaphore("sem_" + k) for k in self.e}
        self.cnt = {k: 0 for k in self.e}
        self.seen = {k: {} for k in self.e}
        self.dsem = [nc.alloc_semaphore(f"dsem{i}") for i in range(ndma)]
        self.dcnt = [0] * ndma
        self.dnext = 0
        self.nwait = 0

    def _wait(self, eng, key, val):
        if self.seen[eng].get(key, 0) >= val:
            return
        self.seen[eng][key] = val
        sem = self.sem[key] if isinstance(key, str) else self.dsem[key[1]]
        self.e[eng].wait_ge(sem, val)
        self.nwait += 1

    def _deps(self, eng, reads, writes):
        need = {}
        for b in reads:
            r = b.r
            if r.w is not None:
                k, v = r.w
                need[k] = max(need.get(k, 0), v)
        for b in writes:
            w = b.r
            if w.w is not None:
                k, v = w.w
                need[k] = max(need.get(k, 0), v)
            for k, v in w.r.items():
                need[k] = max(need.get(k, 0), v)
        for k, v in need.items():
            if eng == "pe" and k == "pe":
                continue
            self._wait(eng, k, v)

    def op(self, eng, fn, reads=(), writes=()):
        self._deps(eng, reads, writes)
        ins = fn(self.e[eng])
        self.cnt[eng] += 1
        ins.then_inc(self.sem[eng], 1)
        v = self.cnt[eng]
        for b in reads:
            b.r.r[eng] = v
        for b in writes:
            b.r.w = (eng, v)
            b.r.r = {}
        return ins

    def dma(self, out, in_, reads=(), writes=(), q="sp", **kw):
        slot = self.dnext
        self.dnext = (self.dnext + 1) % len(self.dsem)
        key = ("d", slot)
        if self.dcnt[slot] > 0:
            self._wait(q, key, self.dcnt[slot])
        self._deps(q, reads, writes)
        self.dcnt[slot] += 16
        self.e[q].dma_start(out=out, in_=in_, **kw).then_inc(self.dsem[slot], 16)
        v = self.dcnt[slot]
        for b in reads:
            b.r.r[key] = v
        for b in writes:
            b.r.w = (key, v)
            b.r.r = {}

    def finish(self, eng="sp"):
        for k in self.cnt:
            if self.cnt[k] > 0 and k != eng:
                self._wait(eng, k, self.cnt[k])
        for i, c in enumerate(self.dcnt):
            if c > 0:
                self._wait(eng, ("d", i), c)


def t5_bucket_np(n):
    n = np.maximum(n, 0)
    nf = np.maximum(n, 1).astype(np.float32)
    large = 16 + (np.log(nf / np.float32(16)) / np.float32(math.log(128 / 16)) * np.float32(16)).astype(np.int32)
    large = np.minimum(large, 31)
    return np.where(n < 16, n, large)


O_QKV, O_ZA, O_B, O_A, O_QB, O_CKV, O_ZB, O_IQ, O_IK, O_IW = 0, 1536, 2048, 2052, 2056, 2568, 2696, 3208, 3720, 3784
NW16 = 3208
NIDX = 648


def build(debug=None):
    nc = bass.Bass("TRN2", target_bir_lowering=False)
    din = lambda n, sh: nc.dram_tensor(n, sh, F32, kind="ExternalInput").ap()
    x_d = din("x", [NBC, L, D])
    cT_d = din("cT", [128, 8, NBC])
    wada_d = din("w_ada", [128, 8, 3072])
    bcol_d = din("b_col", [128, 24])
    bgate_d = din("b_gate", [1, 1024])
    gpre_d = din("g_pre", [128, 8])
    win_d = din("w_in", [128, 8, NW16])
    widx_d = din("w_idx", [128, 8, NIDX])
    conv_d = din("conv_w", [128, 12, 4])
    alog_d = din("a_log", [1, 4])
    dtb_d = din("dt_bias", [1, 4])
    ggdn_d = din("g_gdn", [1, 128])
    gkv_d = din("g_kv", [1, 128])
    wuv_d = din("w_uv", [128, 4, 128])
    rb_d = din("rel_bias", [1, 128])
    wout_d = din("w_out", [128, 8, 1024])
    gpost_d = din("g_post", [1, 1024])
    msk_d = din("masks", [128, 8, 128])
    out_d = nc.dram_tensor("out", [NBC, L, D], F32, kind="ExternalOutput").ap()
    dbg_d = {}
    if debug:
        for n, sh in debug.items():
            dbg_d[n] = nc.dram_tensor("dbg_" + n, list(sh), F32, kind="ExternalOutput").ap()

    S = Sched(nc)
    cnt = [0]

    def sb(shape, dt=F32, name=None):
        cnt[0] += 1
        name = "s_" + (name or f"t{cnt[0]}")
        return Buf(nc.alloc_sbuf_tensor(name, list(shape), dt), name)

    def ps(shape, dt=F32, name=None):
        cnt[0] += 1
        name = "p_" + (name or f"p{cnt[0]}")
        return Buf(nc.alloc_psum_tensor(name, list(shape), dt), name)

    def mm(out, lhsT, rhs, start=True, stop=True, reads=(), writes=()):
        S.op("pe", lambda e: e.matmul(out, lhsT, rhs, start=start, stop=stop), reads, writes)

    def tr(out, in_, ident, reads=(), writes=()):
        S.op("pe", lambda e: e.transpose(out, in_, ident), reads, writes)

    def act(out, in_, func, reads=(), writes=(), **kw):
        S.op("act", lambda e: e.activation(out, in_, func, **kw), reads, writes)

    def tt(eng, out, in0, in1, op, reads=(), writes=()):
        S.op(eng, lambda e: e.tensor_tensor(out, in0, in1, op=op), reads, writes)

    def ts(eng, out, in0, s1, s2, op0, op1=None, reads=(), writes=(), accum_out=None):
        if op1 is None:
            S.op(eng, lambda e: e.tensor_scalar(out, in0, s1, None, op0=op0), reads, writes)
        elif accum_out is not None:
            S.op(eng, lambda e: e.tensor_scalar(out, in0, s1, s2, op0=op0, op1=op1, accum_out=accum_out), reads, writes)
        else:
            S.op(eng, lambda e: e.tensor_scalar(out, in0, s1, s2, op0=op0, op1=op1), reads, writes)

    def stt(out, in0, scalar, in1, op0, op1, reads=(), writes=()):
        S.op("dve", lambda e: e.scalar_tensor_tensor(out, in0, scalar, in1, op0=op0, op1=op1), reads, writes)

    def cp(eng, out, in_, reads=(), writes=()):
        if eng == "act":
            S.op("act", lambda e: e.copy(out, in_), reads, writes)
        else:
            S.op(eng, lambda e: e.tensor_copy(out, in_), reads, writes)

    def dump(name, ap, buf):
        if name in dbg_d:
            S.dma(dbg_d[name], ap, reads=[buf], q="pool")

    big4 = sb([128, 1024], name="big4")
    rtmp2 = sb([128, 512], name="rtmp2")

    def view(buf, ap):
        v = Buf.__new__(Buf)
        v.t = ap
        v.r = buf.r
        return v
    io = view(big4, big4[:, 0:128])
    S.op("pool", lambda e: e.iota(io[:], [[1, 128]], base=0, channel_multiplier=-1,
                                  allow_small_or_imprecise_dtypes=True), writes=[io])
    ident = sb([128, 128], name="ident")
    ident16 = sb([128, 128], BF16, name="ident16")
    Umat = sb([128, 128], name="Umat")
    SLmat = sb([128, 128], name="SLmat")
    NEGs = sb([128, 128], name="NEGs")
    NEGsT = sb([128, 128], name="NEGsT")
    NEGC = sb([128, 128], name="NEGC")
    ones32 = sb([128, 128], name="ones32")
    ones16 = sb([128, 128], BF16, name="ones16")
    mhalf = sb([128, 8], name="mhalf")
    ts("dve", ident[:], io[:], 0.0, None, ALU.is_equal, reads=[io], writes=[ident])
    ts("dve", ident16[:], io[:], 0.0, None, ALU.is_equal, reads=[io], writes=[ident16])
    ts("dve", Umat[:], io[:], 0.0, None, ALU.is_ge, reads=[io], writes=[Umat])
    ts("dve", SLmat[:], io[:], 0.0, None, ALU.is_lt, reads=[io], writes=[SLmat])
    ts("dve", NEGs[:], io[:], 0.0, NEG, ALU.is_ge, ALU.mult, reads=[io], writes=[NEGs])
    ts("dve", NEGsT[:], io[:], 0.0, NEG, ALU.is_le, ALU.mult, reads=[io], writes=[NEGsT])
    ts("dve", NEGC[:], io[:], 0.0, -1e30, ALU.is_gt, ALU.mult, reads=[io], writes=[NEGC])
    S.op("pool", lambda e: e.memset(ones32[:], 1.0), writes=[ones32])
    S.op("pool", lambda e: e.memset(ones16[:], 1.0), writes=[ones16])
    S.op("pool", lambda e: e.memset(mhalf[:], -0.5), writes=[mhalf])
    pow2 = sb([128, NBIS + 1], name="pow2")
    for k in range(NBIS + 1):
        S.op("pool", lambda e, k=k: e.memset(pow2[:, k:k + 1], 0.5 ** (k + 1)), writes=[pow2])

    PT2a = ps([128, 512], name="PT2a")
    PT2b = ps([128, 512], name="PT2b")
    PT2h = [PT2a, PT2b]
    rotg = [0]

    def pabG():
        rotg[0] ^= 1
        return PT2a if rotg[0] else PT2b
    PA = ps([128, 512], name="PA")
    PB = ps([128, 512], name="PB")
    PC = ps([128, 512], name="PC")
    PO = ps([128, 512], name="PO")
    PO2 = ps([128, 512], name="PO2")
    PS_ = ps([128, 512], name="PS_")
    rot = [0]

    def pab():
        rot[0] ^= 1
        return PA if rot[0] else PB

    stage = [sb([128, 8, 256], name="stage0"), sb([128, 8, 256], name="stage1")]
    w16 = sb([128, 8, NW16], BF16, name="w16")
    widx = sb([128, 8, NIDX], name="widx")
    wout16 = sb([128, 8, 1024], BF16, name="wout16")
    wuv16 = sb([128, 4, 128], BF16, name="wuv16")
    S.dma(widx[:], widx_d, writes=[widx])
    si = 0
    for c0 in range(0, NW16, 256):
        w = min(256, NW16 - c0)
        st = stage[si % 2]; si += 1
        S.dma(st[:, :, 0:w], win_d[:, :, c0:c0 + w], writes=[st])
        cp("dve" if si % 2 else "pool", w16[:, :, c0:c0 + w], st[:, :, 0:w], reads=[st], writes=[w16])
    for c0 in range(0, 1024, 256):
        st = stage[si % 2]; si += 1
        S.dma(st[:], wout_d[:, :, c0:c0 + 256], writes=[st])
        cp("dve" if si % 2 else "pool", wout16[:, :, c0:c0 + 256], st[:], reads=[st], writes=[wout16])
    st = stage[si % 2]; si += 1
    S.dma(st[:, 0:4, 0:128], wuv_d, writes=[st])
    cp("dve", wuv16[:], st[:, 0:4, 0:128], reads=[st], writes=[wuv16])

    st = stage[si % 2]; si += 1
    S.dma(st[:, :, 0:128], msk_d, writes=[st])
    msk = sb([128, 8, 128], BF16, name="msk")
    cp("dve", msk[:], st[:, :, 0:128], reads=[st], writes=[msk])
    convw = sb([128, 12, 4], name="convw")
    S.dma(convw[:], conv_d, writes=[convw])
    diagw = sb([128, 12, 4, 128], BF16, name="diagw")
    for ch in range(12):
        for j in range(4):
            ts("dve" if (ch + j) % 2 else "pool", diagw[:, ch, j, :], ident[:], convw[:, ch, j:j + 1], None, ALU.mult,
               reads=[ident, convw], writes=[diagw])

    def bcast_row(src, n, name):
        t = sb([128, n], name=name)
        S.dma(t[:], src.partition_broadcast(128), writes=[t])
        return t
    alogB = bcast_row(alog_d, 4, "alogB")
    dtbB = bcast_row(dtb_d, 4, "dtbB")
    ggdnB = bcast_row(ggdn_d, 128, "ggdnB")
    gkvB = bcast_row(gkv_d, 128, "gkvB")
    rbB = bcast_row(rb_d, 128, "rbB")
    negA = sb([128, 4], name="negA")
    act(negA[:], alogB[:], AF.Exp, reads=[alogB], writes=[negA])
    ts("dve", negA[:], negA[:], -1.0, None, ALU.mult, reads=[negA], writes=[negA])
    ggdnH = ggdnB

    cT = sb([128, 8, NBC], name="cT")
    S.dma(cT[:], cT_d, writes=[cT])
    sc = sb([128, 8, NBC], name="sc")
    act(sc[:], cT[:], AF.Silu, reads=[cT], writes=[sc])
    bcol = sb([128, 24], name="bcol")
    S.dma(bcol[:], bcol_d, writes=[bcol])
    gpre = sb([128, 8], name="gpre")
    S.dma(gpre[:], gpre_d, writes=[gpre])
    shiftc = sb([128, 8, NBC], name="shiftc")
    Gc = sb([128, 8, NBC], name="Gc")
    GATE = sb([128, 1024], name="GATE")
    for n8 in range(8):
        st = stage[si % 2]; si += 1
        S.dma(st[:], wada_d[:, :, n8 * 256:(n8 + 1) * 256], writes=[st])
        for q2 in range(2):
            dch = (n8 % 4) * 2 + q2
            for kc in range(8):
                mm(PC[:, q2 * 4:q2 * 4 + 4], st[:, kc, q2 * 128:(q2 + 1) * 128], sc[:, kc, :],
                   start=(kc == 0), stop=(kc == 7), reads=[st, sc], writes=[PC])
            dst = shiftc if n8 < 4 else Gc
            ts("dve", dst[:, dch, :], PC[:, q2 * 4:q2 * 4 + 4], bcol[:, n8 * 2 + q2:n8 * 2 + q2 + 1], None, ALU.add,
               reads=[PC, bcol], writes=[dst])
    ts("dve", Gc[:], Gc[:], 1.0, None, ALU.add, reads=[Gc], writes=[Gc])
    tt("dve", Gc[:], Gc[:], gpre[:].unsqueeze(2).to_broadcast([128, 8, NBC]), ALU.mult, reads=[Gc, gpre], writes=[Gc])

    bk = t5_bucket_np(np.arange(256))
    lo_b = [int(np.argmax(bk >= b)) if (bk >= b).any() else 100000 for b in range(32)]
    rb3 = rbB[:].rearrange("p (b h) -> p b h", h=4)
    dlt = sb([128, 32, 4], name="dlt")
    cp("dve", dlt[:, 0:1, :], rb3[:, 0:1, :], reads=[rbB], writes=[dlt])
    tt("dve", dlt[:, 1:32, :], rb3[:, 1:32, :], rb3[:, 0:31, :], ALU.subtract, reads=[rbB], writes=[dlt])
    EB = [sb([128, 4, 128], BF16, name=f"EB{t}") for t in range(2)]
    EBf = view(rtmp2, rtmp2[:, 0:128])
    rtmp = sb([128, 512], name="rtmp")
    gUb = [sb([128, 128], name=f"gU{i}") for i in range(2)]
    distT = gUb[0]
    tmpb = gUb[1]
    for typ in range(2):
        ts("dve", distT[:], io[:], float(128 * typ), None, ALU.add, reads=[io], writes=[distT])
        for h in range(4):
            acc = EBf
            ts("dve", acc[:], ones32[:], dlt[:, 0, h:h + 1], rb3[:, 31, h:h + 1], ALU.mult, ALU.subtract,
               reads=[ones32, dlt, rbB], writes=[acc])
            for bb in range(1, 32):
                if lo_b[bb] > 255:
                    continue
                ts("dve", tmpb[:], distT[:], float(lo_b[bb]) - 0.5, dlt[:, bb, h:h + 1], ALU.is_ge, ALU.mult,
                   reads=[distT, dlt], writes=[tmpb])
                tt("dve", acc[:], acc[:], tmpb[:], ALU.add, reads=[acc, tmpb], writes=[acc])
            ts("dve", acc[:], acc[:], 128.0 ** 0.5, None, ALU.mult, reads=[acc], writes=[acc])
            cp("dve", EB[typ][:, h, :], acc[:], reads=[acc], writes=[EB[typ]])

    xt = sb([128, 1024], name="xt")
    junkA = big4; xs = big4; otmp = big4
    hT32 = sb([128, 8, 128], name="hT32")
    hT16 = sb([128, 8, 128], BF16, name="hT16")
    uT = sb([128, 12, 131], BF16, name="uT")
    qkvs = sb([128, 1536], BF16, name="qkvs")
    col = lambda n, name: sb([128, n], name=name)
    ssq1 = col(1, "ssq1"); rstd1 = col(1, "rstd1"); ssqP2 = col(2, "ssqP2"); ssqP = col(1, "ssqP"); rstdP = col(1, "rstdP")
    xs2 = sb([128, 512], name="xs2")
    ssq8 = col(8, "ssq8"); rs8 = col(8, "rs8")
    ba = col(8, "ba"); beta = col(4, "beta"); gcol = col(4, "gcol"); gc = col(4, "gc"); glB = col(4, "glB")
    egc = col(4, "egc"); ekg = col(4, "ekg"); egl = col(4, "egl"); tmp4 = col(4, "tmp4"); nbeta = col(4, "nbeta")
    cf = {n: col(4, "cf_" + n) for n in ("kbg", "kg", "qg")}
    khat = sb([128, 4, 128], BF16, name="khat"); qhat = sb([128, 4, 128], BF16, name="qhat")
    qg = sb([128, 4, 128], BF16, name="qg"); kbg = sb([128, 4, 128], BF16, name="kbg")
    kg = sb([128, 4, 128], BF16, name="kg"); vb = sb([128, 4, 128], BF16, name="vb")
    khT = sb([128, 4, 128], BF16, name="khT"); qhT = sb([128, 4, 128], BF16, name="qhT"); qgT = sb([128, 4, 128], BF16, name="qgT")
    Es = sb([128, 4, 128], BF16, name="Es"); EsT = sb([128, 4, 128], BF16, name="EsT")
    Xb = [sb([128, 4, 128], BF16, name="X0")]
    XTb = [sb([128, 4, 128], BF16, name="XT0")]
    Tb = [sb([128, 4, 128], BF16, name=f"T{i}") for i in range(2)]
    TTb = [sb([128, 4, 128], BF16, name=f"TT{i}") for i in range(2)]
    Wp = khat
    attnT = sb([128, 4, 128], BF16, name="attnT")
    negwT = qhat
    vnew = qg
    S32 = sb([128, 4, 128], name="S32"); S16 = sb([128, 4, 128], BF16, name="S16")
    zas = sb([128, 512], BF16, name="zas"); G1 = zas
    osq4 = col(4, "osq4"); ors4 = col(4, "ors4")
    og = sb([128, 4, 128], BF16, name="og"); ogT = sb([128, 4, 128], BF16, name="ogT")
    qbT = sb([128, 4, 128], BF16, name="qbT"); szbT = sb([128, 4, 128], BF16, name="szbT")
    iqT = sb([128, 4, 128], name="iqT"); iw = col(8, "iw")
    ckvn = sb([128, NT, 128], BF16, name="ckvn"); ckvnT = sb([128, L], BF16, name="ckvnT")
    ikTb = stage[1]
    ikT = ikTb[:].rearrange("p a b -> p (a b)")
    score = stage[0]
    scoreF = score[:].rearrange("p a b -> p (a b)")
    mk = sb([128, L], BF16, name="mk")
    junkD = mk
    junkDF = mk
    lo = col(1, "lo"); hw0 = col(1, "hw0"); hwk = col(NBIS + 1, "hwk"); nhwk = col(NBIS + 1, "nhwk"); mid = col(1, "mid"); mid2 = col(1, "mid2"); sgnc = col(1, "sgnc"); cbc = col(1, "cbc"); cntc = col(1, "cntc"); tstep = col(1, "tstep")
    Eb = [sb([128, 4, 128], BF16, name=f"Eb{i}") for i in range(2)]
    negmk = sb([128, NT, 128], BF16, name="negmk"); mkT = negmk
    obT = sb([128, 4, 128], BF16, name="obT")
    rden = rtmp; Rg = rden
    ygT = sb([128, 4, 128], BF16, name="ygT")
    msq = col(2, "msq"); mrs = col(1, "mrs")

    def rsqrt_col(dst, src, n, scale, eps):
        ts("dve", dst[:, 0:n], src[:, 0:n], scale, eps, ALU.mult, ALU.add, reads=[src], writes=[dst])
        tt("pool", dst[:, 0:n], dst[:, 0:n], mhalf[:, 0:n], ALU.pow, reads=[dst, mhalf], writes=[dst])

    def pre_gen(b, i):
        t0 = i * 128
        uc = uT
        S.dma(xt[:], x_d[b, t0:t0 + 128, :], writes=[xt])
        yield
        for hf in range(2):
            act(xs2[:], xt[:, hf * 512:(hf + 1) * 512], AF.Square, reads=[xt], writes=[xs2, ssqP2], accum_out=ssqP2[:, hf:hf + 1])
        tt("pool", ssqP[:], ssqP2[:, 0:1], ssqP2[:, 1:2], ALU.add, reads=[ssqP2], writes=[ssqP])
        rsqrt_col(rstdP, ssqP, 1, 1.0 / D, EPS)
        yield
        for hf in range(2):
            ts("dve", xs2[:], xt[:, hf * 512:(hf + 1) * 512], rstdP[:, 0:1], None, ALU.mult, reads=[xt, rstdP], writes=[xs2])
            for c4 in range(4):
                tr(PC[:, c4 * 128:(c4 + 1) * 128], xs2[:, c4 * 128:(c4 + 1) * 128], ident[:], reads=[xs2, ident], writes=[PC])
            for c4 in range(4):
                c = hf * 4 + c4
                ts("dve", hT32[:, c, :], PC[:, c4 * 128:(c4 + 1) * 128], Gc[:, c, b:b + 1], shiftc[:, c, b:b + 1], ALU.mult, ALU.add,
                   reads=[PC, Gc, shiftc], writes=[hT32])
            yield
        cp("pool", hT16[:], hT32[:], reads=[hT32], writes=[hT16])
        if b == 0 and i == 0:
            dump("hT", hT32[:], hT32)
        yield
        for g3 in range(3):
            for q4 in range(4):
                ch = g3 * 4 + q4
                for kc in range(8):
                    mm(PC[:, q4 * 128:(q4 + 1) * 128], w16[:, kc, O_QKV + ch * 128:O_QKV + (ch + 1) * 128], hT16[:, kc, :],
                       start=(kc == 0), stop=(kc == 7), reads=[w16, hT16], writes=[PC])
            cp("dve", uc[:, g3 * 4:(g3 + 1) * 4, 3:131], PC[:].rearrange("p (a b) -> p a b", a=4), reads=[PC], writes=[uc])
            yield
        for g3 in range(3):
            for q4 in range(4):
                ch = g3 * 4 + q4
                for j in range(4):
                    mm(PC[:, q4 * 128:(q4 + 1) * 128], uc[:, ch, j:j + 128], diagw[:, ch, j, :],
                       start=(j == 0), stop=(j == 3), reads=[uc, diagw], writes=[PC])
            act(qkvs[:, g3 * 512:(g3 + 1) * 512], PC[:], AF.Silu, reads=[PC], writes=[qkvs])
            yield
        cp("pool", uc[:, :, 0:3], uc[:, :, 128:131], reads=[uc], writes=[uc])
        if b == 0 and i == 0:
            dump("qkvs", qkvs[:], qkvs)
        for hf in range(2):
            tt("dve", xs2[:], qkvs[:, hf * 512:(hf + 1) * 512], qkvs[:, hf * 512:(hf + 1) * 512], ALU.mult, reads=[qkvs], writes=[xs2])
            S.op("dve", lambda e, hf=hf: e.tensor_reduce(ssq8[:, hf * 4:(hf + 1) * 4], xs2[:].rearrange("p (a b) -> p a b", a=4), axis=AX.X, op=ALU.add),
                 reads=[xs2], writes=[ssq8])
        rsqrt_col(rs8, ssq8, 8, 1.0, EPS)
        ts("dve", rs8[:, 0:4], rs8[:, 0:4], 128.0 ** -0.5, None, ALU.mult, reads=[rs8], writes=[rs8])
        yield
        for c4 in range(4):
            for kc in range(8):
                mm(PC[:, c4 * 128:(c4 + 1) * 128], widx[:, kc, c4 * 128:(c4 + 1) * 128], hT32[:, kc, :],
                   start=(kc == 0), stop=(kc == 7), reads=[widx, hT32], writes=[PC])
            if c4 % 2:
                yield
        cp("dve", iqT[:], PC[:].rearrange("p (a b) -> p a b", a=4), reads=[PC], writes=[iqT])
        yield
        for kc in range(8):
            mm(PC[:, 0:128], widx[:, kc, 512:640], hT32[:, kc, :], start=(kc == 0), stop=(kc == 7), reads=[widx, hT32], writes=[PC])
        cp("dve", ikT[:, t0:t0 + 128], PC[:, 0:128], reads=[PC], writes=[ikTb])
        for kc in range(8):
            mm(PC[:, 0:8], hT32[:, kc, :], widx[:, kc, 640:648], start=(kc == 0), stop=(kc == 7), reads=[hT32, widx], writes=[PC])
        ts("dve", iw[:], PC[:, 0:8], (8.0 * 64.0) ** -0.5, None, ALU.mult, reads=[PC], writes=[iw])
        yield
        for kc in range(8):
            mm(PC[:, 16:24], hT16[:, kc, :], w16[:, kc, O_B:O_B + 8], start=(kc == 0), stop=(kc == 7), reads=[hT16, w16], writes=[PC])
        for kc in range(8):
            mm(PC[:, 128:256], hT16[:, kc, :], w16[:, kc, O_CKV:O_CKV + 128], start=(kc == 0), stop=(kc == 7), reads=[hT16, w16], writes=[PC])
        cp("dve", ba[:], PC[:, 16:24], reads=[PC], writes=[ba])
        act(xs2[:, 0:128], PC[:, 128:256], AF.Square, reads=[PC], writes=[xs2, ssqP], accum_out=ssqP[:, 0:1])
        rsqrt_col(rstdP, ssqP, 1, 1.0 / 128, EPS)
        stt(ckvn[:, i, :], PC[:, 128:256], rstdP[:, 0:1], gkvB[:], ALU.mult, ALU.mult, reads=[PC, rstdP, gkvB], writes=[ckvn])
        yield
        PC16 = PC[:].bitcast(BF16)
        tr(PC16[:, 512:640], ckvn[:, i, :], ident16[:], reads=[ckvn, ident16], writes=[PC])
        cp("dve", ckvnT[:, t0:t0 + 128], PC16[:, 512:640], reads=[PC], writes=[ckvnT])
        yield

    tix = 0
    screp = xt[:].rearrange("p (a b) -> p a b", a=8)
    for b in range(NBC):
        S.op("pool", lambda e: e.memset(S32[:], 0.0), writes=[S32])
        S.op("pool", lambda e: e.memset(S16[:], 0.0), writes=[S16])
        S.op("pool", lambda e: e.memset(uT[:, :, 0:3], 0.0), writes=[uT])
        for kc in range(8):
            cp("dve" if kc % 2 else "pool", screp[:, kc, :], sc[:, kc, b:b + 1].to_broadcast([128, 128]), reads=[sc], writes=[xt])
        for g4 in range(4):
            st = stage[g4 % 2]
            g0 = g4 * 256
            S.dma(st[:], wada_d[:, :, 2048 + g0:2048 + g0 + 256], writes=[st])
            S.dma(rtmp[:, 0:256], bgate_d[:, g0:g0 + 256].partition_broadcast(128), writes=[rtmp])
            S.dma(rtmp[:, 256:512], gpost_d[:, g0:g0 + 256].partition_broadcast(128), writes=[rtmp])
            P = pab()
            for kc in range(8):
                mm(P[:, 0:256], screp[:, kc, :], st[:, kc, :], start=(kc == 0), stop=(kc == 7), reads=[xt, st], writes=[P])
            tt("dve", GATE[:, g0:g0 + 256], P[:, 0:256], rtmp[:, 0:256], ALU.add, reads=[P, rtmp], writes=[GATE])
            tt("pool", GATE[:, g0:g0 + 256], GATE[:, g0:g0 + 256], rtmp[:, 256:512], ALU.mult, reads=[GATE, rtmp], writes=[GATE])
        for i in range(NT):
            t0 = i * 128
            uc = uT
            if i == 0:
                for _ in pre_gen(b, 0):
                    pass
            P = pab()
            for kc in range(8):
                mm(P[:], hT16[:, kc, :], w16[:, kc, O_ZA:O_ZA + 512], start=(kc == 0), stop=(kc == 7), reads=[hT16, w16], writes=[P])
            act(zas[:], P[:], AF.Silu, reads=[P], writes=[zas])
            tt("pool", zas[:].rearrange("p (h v) -> p h v", h=4), zas[:].rearrange("p (h v) -> p h v", h=4),
               ggdnH[:].unsqueeze(1).to_broadcast([128, 4, 128]), ALU.mult, reads=[zas, ggdnH], writes=[zas])
            def gdn_stream():
                act(beta[:], ba[:, 0:4], AF.Exp, reads=[ba], writes=[beta], scale=-1.0)
                yield
                ts("dve", beta[:], beta[:], 1.0, None, ALU.add, reads=[beta], writes=[beta])
                yield
                S.op("dve", lambda e: e.reciprocal(beta[:], beta[:]), reads=[beta], writes=[beta])
                yield
                ts("dve", nbeta[:], beta[:], -1.0, None, ALU.mult, reads=[beta], writes=[nbeta])
                yield
                tt("dve", tmp4[:], ba[:, 4:8], dtbB[:], ALU.add, reads=[ba, dtbB], writes=[tmp4])
                yield
                act(tmp4[:], tmp4[:], AF.Exp, reads=[tmp4], writes=[tmp4])
                yield
                act(tmp4[:], tmp4[:], AF.Ln, reads=[tmp4], writes=[tmp4], bias=1.0)
                yield
                tt("dve", gcol[:], tmp4[:], negA[:], ALU.mult, reads=[tmp4, negA], writes=[gcol])
                yield
                mm(PC[:, 16:20], Umat[:], gcol[:], reads=[Umat, gcol], writes=[PC])
                yield
                mm(PC[:, 20:24], ones32[:], gcol[:], reads=[ones32, gcol], writes=[PC])
                yield
                cp("dve", gc[:], PC[:, 16:20], reads=[PC], writes=[gc])
                yield
                cp("dve", glB[:], PC[:, 20:24], reads=[PC], writes=[glB])
                yield
                act(egc[:], gc[:], AF.Exp, reads=[gc], writes=[egc])
                yield
                act(egl[:], glB[:], AF.Exp, reads=[glB], writes=[egl])
                yield
                tt("dve", tmp4[:], glB[:], gc[:], ALU.subtract, reads=[glB, gc], writes=[tmp4])
                yield
                act(ekg[:], tmp4[:], AF.Exp, reads=[tmp4], writes=[ekg])
                yield
                tt("dve", cf["kbg"][:], rs8[:, 4:8], beta[:], ALU.mult, reads=[rs8, beta], writes=[cf["kbg"]])
                yield
                tt("dve", cf["kbg"][:], cf["kbg"][:], egc[:], ALU.mult, reads=[cf["kbg"], egc], writes=[cf["kbg"]])
                yield
                tt("dve", cf["kg"][:], rs8[:, 4:8], ekg[:], ALU.mult, reads=[rs8, ekg], writes=[cf["kg"]])
                yield
                tt("dve", cf["qg"][:], rs8[:, 0:4], egc[:], ALU.mult, reads=[rs8, egc], writes=[cf["qg"]])
                yield
                q3 = qkvs[:, 0:512].rearrange("p (h d) -> p h d", h=4)
                yield
                k3 = qkvs[:, 512:1024].rearrange("p (h d) -> p h d", h=4)
                yield
                v3 = qkvs[:, 1024:1536].rearrange("p (h d) -> p h d", h=4)
                yield
                bc = lambda c, lo_=0: c[:, lo_:lo_ + 4].unsqueeze(2).to_broadcast([128, 4, 128])
                yield
                tt("dve", khat[:], k3, bc(rs8, 4), ALU.mult, reads=[qkvs, rs8], writes=[khat])
                yield
                tt("pool", qhat[:], q3, bc(rs8, 0), ALU.mult, reads=[qkvs, rs8], writes=[qhat])
                yield
                tt("dve", qg[:], q3, bc(cf["qg"]), ALU.mult, reads=[qkvs, cf["qg"]], writes=[qg])
                yield
                tt("pool", kbg[:], k3, bc(cf["kbg"]), ALU.mult, reads=[qkvs, cf["kbg"]], writes=[kbg])
                yield
                tt("dve", kg[:], k3, bc(cf["kg"]), ALU.mult, reads=[qkvs, cf["kg"]], writes=[kg])
                yield
                tt("pool", vb[:], v3, bc(beta), ALU.mult, reads=[qkvs, beta], writes=[vb])
                yield "M"
                for src, dst in ((khat, khT), (qhat, qhT), (qg, qgT)):
                    P = pabG()
                    P16 = P[:].bitcast(BF16)
                    for h in range(4):
                        tr(P16[:, h * 128:(h + 1) * 128], src[:, h, :], ident16[:], reads=[src, ident16], writes=[P])
                    cp("act", dst[:], P16[:, 0:512].rearrange("p (a b) -> p a b", a=4), reads=[P], writes=[dst])
                yield
                PD = pabG(); PDT = pabG()
                yield
                for h in range(4):
                    gU = gUb[h % 2]
                    ts("dve" if h % 2 else "pool", gU[:], Umat[:], gcol[:, h:h + 1], None, ALU.mult, reads=[Umat, gcol], writes=[gU])
                    mm(PD[:, h * 128:(h + 1) * 128], gU[:], SLmat[:], start=True, stop=False, reads=[gU, SLmat], writes=[PD])
                    mm(PD[:, h * 128:(h + 1) * 128], ident[:], NEGs[:], start=False, stop=True, reads=[ident, NEGs], writes=[PD])
                    mm(PDT[:, h * 128:(h + 1) * 128], SLmat[:], gU[:], start=True, stop=False, reads=[gU, SLmat], writes=[PDT])
                    mm(PDT[:, h * 128:(h + 1) * 128], ident[:], NEGsT[:], start=False, stop=True, reads=[ident, NEGsT], writes=[PDT])
                yield
                act(Es[:], PD[:].rearrange("p (a b) -> p a b", a=4), AF.Exp, reads=[PD], writes=[Es])
                yield
                act(EsT[:], PDT[:].rearrange("p (a b) -> p a b", a=4), AF.Exp, reads=[PDT], writes=[EsT])
                yield
                if b == 0 and i == 0:
                    dump("Es", Es[:].rearrange("p a b -> p (a b)"), Es)
                    dump("beta4", beta[:], beta); dump("gc4", gc[:], gc); dump("rs8", rs8[:], rs8)
                yield
                P = pabG()
                yield
                for h in range(4):
                    mm(P[:, h * 128:(h + 1) * 128], khT[:, h, :], khT[:, h, :], reads=[khT], writes=[P])
                yield
                X, XT = Xb[0], XTb[0]
                yield
                for h in range(4):
                    stt(X[:, h, :], P[:, h * 128:(h + 1) * 128], nbeta[:, h:h + 1], Es[:, h, :], ALU.mult, ALU.mult,
                        reads=[P, nbeta, Es], writes=[X])
                yield
                if b == 0 and i == 0:
                    dump("X0", X[:].rearrange("p a b -> p (a b)"), X)
                yield
                P = pabG()
                yield
                for h in range(4):
                    mm(P[:, h * 128:(h + 1) * 128], khT[:, h, :], qhT[:, h, :], reads=[khT, qhT], writes=[P])
                yield
                tt("pool", EsT[:], EsT[:], ident16[:].unsqueeze(1).to_broadcast([128, 4, 128]), ALU.add, reads=[EsT, ident16], writes=[EsT])
                yield
                tt("dve", attnT[:], P[:].rearrange("p (a b) -> p a b", a=4), EsT[:], ALU.mult, reads=[P, EsT], writes=[attnT])
                yield
                P = pabG()
                yield
                P16 = P[:].bitcast(BF16)
                yield
                for h in range(4):
                    tr(P16[:, h * 128:(h + 1) * 128], X[:, h, :], ident16[:], reads=[X, ident16], writes=[P])
                yield
                cp("act", XT[:], P16[:, 0:512].rearrange("p (a b) -> p a b", a=4), reads=[P], writes=[XT])
                yield
                bcm = lambda ls: msk[:, ls, :].unsqueeze(1).to_broadcast([128, 4, 128])
                yield
                Tc, TT = Tb[0], TTb[0]
                yield
                tt("pool", Tc[:], X[:], bcm(0), ALU.mult, reads=[X, msk], writes=[Tc])
                yield
                tt("pool", Tc[:], Tc[:], ident16[:].unsqueeze(1).to_broadcast([128, 4, 128]), ALU.add, reads=[Tc, ident16], writes=[Tc])
                yield
                tt("dve", TT[:], XT[:], bcm(7), ALU.mult, reads=[XT, msk], writes=[TT])
                yield
                tt("dve", TT[:], TT[:], ident16[:].unsqueeze(1).to_broadcast([128, 4, 128]), ALU.add, reads=[TT, ident16], writes=[TT])
                yield
                gen = 0
                yield
                for ls in range(1, 7):
                    Tn, TTn = Tb[1 - gen], TTb[1 - gen]
                    P1 = pabG()
                    for h in range(4):
                        mm(P1[:, h * 128:(h + 1) * 128], XT[:, h, :], Tc[:, h, :], reads=[XT, Tc], writes=[P1])
                    tt("dve", Wp[:], P1[:].rearrange("p (a b) -> p a b", a=4), bcm(ls), ALU.mult, reads=[P1, msk], writes=[Wp])
                    if ls < 6:
                        P2 = pabG()
                        for h in range(4):
                            mm(P2[:, h * 128:(h + 1) * 128], TT[:, h, :], Wp[:, h, :], reads=[TT, Wp], writes=[P2])
                        tt("dve", Tn[:], P2[:].rearrange("p (a b) -> p a b", a=4), Tc[:], ALU.add, reads=[P2, Tc], writes=[Tn])
                    P3 = PS_
                    for h in range(4):
                        mm(P3[:, h * 128:(h + 1) * 128], Wp[:, h, :], TT[:, h, :], reads=[Wp, TT], writes=[P3])
                    tt("dve", TTn[:], P3[:].rearrange("p (a b) -> p a b", a=4), TT[:], ALU.add, reads=[P3, TT], writes=[TTn])
                    Tc, TT = Tn, TTn
                    gen = 1 - gen
                    yield
                yield
                if b == 0 and i == 0:
                    dump("TTf", TT[:].rearrange("p a b -> p (a b)"), TT)
                    dump("attnT0", attnT[:].rearrange("p a b -> p (a b)"), attnT)
                yield
                P = pabG()
                yield
                for h in range(4):
                    mm(P[:, h * 128:(h + 1) * 128], kbg[:, h, :], TT[:, h, :], reads=[kbg, TT], writes=[P])
                yield
                ts("dve", negwT[:], P[:].rearrange("p (a b) -> p a b", a=4), -1.0, None, ALU.mult, reads=[P], writes=[negwT])
                yield
                PV = pabG()
                for h in range(4):
                    hs = slice(h * 128, (h + 1) * 128)
                    mm(PV[:, hs], TT[:, h, :], vb[:, h, :], start=True, stop=False, reads=[TT, vb], writes=[PV])
                    mm(PV[:, hs], negwT[:, h, :], S16[:, h, :], start=False, stop=True, reads=[negwT, S16], writes=[PV])
                yield
                cp("act", vnew[:], PV[:].rearrange("p (a b) -> p a b", a=4), reads=[PV], writes=[vnew])
                yield
                if b == 0 and i == 0:
                    dump("vnew0", vnew[:].rearrange("p a b -> p (a b)"), vnew)
                yield
                for h in range(4):
                    hs = slice(h * 128, (h + 1) * 128)
                    mm(PS_[:, hs], qgT[:, h, :], S16[:, h, :], start=True, stop=False, reads=[qgT, S16], writes=[PS_])
                    mm(PS_[:, hs], attnT[:, h, :], vnew[:, h, :], start=False, stop=True, reads=[attnT, vnew], writes=[PS_])
                yield
                PDS = pabG()
                for h in range(4):
                    hs = slice(h * 128, (h + 1) * 128)
                    mm(PDS[:, hs], kg[:, h, :], vnew[:, h, :], reads=[kg, vnew], writes=[PDS])
                yield
                for h in range(4):
                    hs = slice(h * 128, (h + 1) * 128)
                    stt(S32[:, h, :], S32[:, h, :], egl[:, h:h + 1], PDS[:, hs], ALU.mult, ALU.add, reads=[S32, egl, PDS], writes=[S32])
                yield
                cp("pool", S16[:], S32[:], reads=[S32], writes=[S16])
                yield
                act(junkA[:, 0:512], PS_[:], AF.Square, reads=[PS_], writes=[junkA])
                yield
                S.op("dve", lambda e: e.tensor_reduce(osq4[:], junkA[:, 0:512].rearrange("p (a b) -> p a b", a=4), axis=AX.X, op=ALU.add),
                     reads=[junkA], writes=[osq4])
                yield
                rsqrt_col(ors4, osq4, 4, 1.0 / 128, EPS)
                yield
                for h in range(4):
                    hs = slice(h * 128, (h + 1) * 128)
                    stt(og[:, h, :], PS_[:, hs], ors4[:, h:h + 1], G1[:, hs], ALU.mult, ALU.mult, reads=[PS_, ors4, G1], writes=[og])
                yield
                if b == 0 and i <= 1:
                    dump(f"og{i}", og[:].rearrange("p a b -> p (a b)"), og)
                yield
                P = pabG()
                yield
                P16 = P[:].bitcast(BF16)
                yield
                for h in range(4):
                    tr(P16[:, h * 128:(h + 1) * 128], og[:, h, :], ident16[:], reads=[og, ident16], writes=[P])
                yield
                cp("act", ogT[:], P16[:, 0:512].rearrange("p (a b) -> p a b", a=4), reads=[P], writes=[ogT])

                yield
            def dsa_stream():
                n = t0 + 128
                yield
                for s0 in range(0, n, 512):
                    w = min(512, n - s0)
                    for h in range(8):
                        P = pab()
                        pr = slice((h % 2) * 64, (h % 2) * 64 + 64)
                        mm(P[:, 0:w], iqT[pr, h // 2, :], ikT[pr, s0:s0 + w], reads=[iqT, ikTb], writes=[P])
                        if h == 0:
                            ts("dve", scoreF[:, s0:s0 + w], P[:, 0:w], 0.0, iw[:, 0:1], ALU.max, ALU.mult, reads=[P, iw], writes=[score])
                        else:
                            rt = rtmp if h % 2 else rtmp2
                            act(rt[:, 0:w], P[:, 0:w], AF.Relu, reads=[P], writes=[rt])
                            stt(scoreF[:, s0:s0 + w], rt[:, 0:w], iw[:, h:h + 1], scoreF[:, s0:s0 + w], ALU.mult, ALU.add,
                                reads=[rt, iw, score], writes=[score])
                        yield
                yield
                tt("dve", scoreF[:, t0:t0 + 128], scoreF[:, t0:t0 + 128], NEGC[:], ALU.add, reads=[score, NEGC], writes=[score])
                yield
                if b == 0 and i == 2:
                    dump("score2", scoreF[:, 0:384], score)
                yield
                yield "B"
                P = pab()
                for h in range(4):
                    for kc in range(8):
                        mm(P[:, h * 128:(h + 1) * 128], w16[:, kc, O_QB + h * 128:O_QB + (h + 1) * 128], hT16[:, kc, :],
                           start=(kc == 0), stop=(kc == 7), reads=[w16, hT16], writes=[P])
                cp("act", qbT[:], P[:].rearrange("p (a b) -> p a b", a=4), reads=[P], writes=[qbT])
                yield
                P = pab()
                for h in range(4):
                    for kc in range(8):
                        mm(P[:, h * 128:(h + 1) * 128], w16[:, kc, O_ZB + h * 128:O_ZB + (h + 1) * 128], hT16[:, kc, :],
                           start=(kc == 0), stop=(kc == 7), reads=[w16, hT16], writes=[P])
                act(szbT[:], P[:].rearrange("p (a b) -> p a b", a=4), AF.Silu, reads=[P], writes=[szbT])
                yield
                if i >= 2:
                    S.op("dve", lambda e: e.tensor_reduce(lo[:], scoreF[:, 0:t0], axis=AX.X, op=ALU.min), reads=[score], writes=[lo])
                    S.op("dve", lambda e: e.tensor_reduce(hw0[:], scoreF[:, 0:n], axis=AX.X, op=ALU.max), reads=[score], writes=[hw0])
                    tt("dve", hw0[:], hw0[:], lo[:], ALU.subtract, reads=[hw0, lo], writes=[hw0])
                    ts("dve", hw0[:], hw0[:], 1.0001, 1e-6, ALU.mult, ALU.add, reads=[hw0], writes=[hw0])
                    ts("dve", hwk[:], pow2[:], hw0[:, 0:1], None, ALU.mult, reads=[pow2, hw0], writes=[hwk])
                    ts("dve", nhwk[:], hwk[:], -1.0, None, ALU.mult, reads=[hwk], writes=[nhwk])
                    ts("dve", mid[:], lo[:], hwk[:, 0:1], -1.0, ALU.add, ALU.mult, reads=[lo, hwk], writes=[mid])
                    S.op("pool", lambda e: e.memset(cbc[:], float(n) - 511.5), writes=[cbc])
                    nmc, nmn = mid, mid2
                    for k in range(NBIS):
                        act(mk[:, 0:n], scoreF[:, 0:n], AF.Sign, reads=[score, nmc], writes=[junkD, cntc], bias=nmc[:, 0:1],
                            accum_out=cntc[:, 0:1])
                        act(sgnc[:], cntc[:], AF.Sign, reads=[cntc, cbc], writes=[sgnc], bias=cbc[:, 0:1])
                        if k < NBIS - 1:
                            act(nmn[:], sgnc[:], AF.Identity, reads=[sgnc, nhwk, nmc], writes=[nmn], scale=nhwk[:, k + 1:k + 2], bias=nmc[:, 0:1])
                            nmc, nmn = nmn, nmc
                        yield
                    act(nmn[:], nmc[:], AF.Identity, reads=[nmc, nhwk], writes=[nmn], scale=-1.0, bias=nhwk[:, NBIS:NBIS + 1])
                    act(lo[:], sgnc[:], AF.Identity, reads=[sgnc, hwk, nmn], writes=[lo], scale=hwk[:, NBIS:NBIS + 1], bias=nmn[:, 0:1])
                else:
                    S.op("dve", lambda e: e.memset(lo[:], -1e29), writes=[lo])
                yield
                ts("dve", mk[:, 0:n], scoreF[:, 0:n], lo[:, 0:1], None, ALU.is_ge, reads=[score, lo], writes=[mk])
                yield
                if b == 0 and i == 2:
                    dump("thr2", lo[:], lo)
                yield
                for j0 in range(0, i + 1, 4):
                    nj = min(4, i + 1 - j0)
                    P = pab()
                    P16 = P[:].bitcast(BF16)
                    for jj in range(nj):
                        j = j0 + jj
                        tr(P16[:, jj * 128:(jj + 1) * 128], mk[:, j * 128:(j + 1) * 128], ident16[:], reads=[mk, ident16], writes=[P])
                    ts("dve", negmk[:, j0:j0 + nj, :], P16[:, 0:nj * 128].rearrange("p (a b) -> p a b", a=nj), 30000.0, -30000.0, ALU.mult, ALU.add,
                       reads=[P], writes=[negmk])
                yield
                qb2 = qbT[:].rearrange("p a b -> p (a b)")
                yield
                for j in range(i + 1):
                    P = pab()
                    P3v = P[:].rearrange("p (a b) -> p a b", a=4)
                    near = j >= i - 1
                    mm(P3v, ckvnT[:, j * 128:(j + 1) * 128], qbT[:], start=True, stop=False, reads=[ckvnT, qbT], writes=[P])
                    mm(P3v, ident16[:], negmk[:, j, :].unsqueeze(1).to_broadcast([128, 4, 128]), start=False, stop=not near,
                       reads=[ident16, negmk], writes=[P])
                    if near:
                        mm(P3v, ident16[:], EB[i - j][:], start=False, stop=True, reads=[ident16, EB[i - j]], writes=[P])
                    E = Eb[j % 2]
                    act(E[:], P3v, AF.Exp, reads=[P], writes=[E], scale=128.0 ** -0.5)
                    pm2 = E[:].rearrange("p a b -> p (a b)")
                    mm(PO[:], ckvn[:, j, :], pm2, start=(j == 0), stop=(j == i), reads=[ckvn, E], writes=[PO])
                    mm(PO2[:], ones16[:], pm2, start=(j == 0), stop=(j == i), reads=[ones16, E], writes=[PO2])
                    yield
                yield
                cp("act", obT[:], PO[:].rearrange("p (a b) -> p a b", a=4), reads=[PO], writes=[obT])
                yield
                act(rden[:], PO2[:], AF.Ln, reads=[PO2], writes=[rden])
                yield
                act(rden[:], rden[:], AF.Exp, reads=[rden], writes=[rden], scale=-1.0)
                yield
                tt("pool", rden[:], rden[:], szbT[:].rearrange("p a b -> p (a b)"), ALU.mult, reads=[rden, szbT], writes=[rden])
                yield
                PY = pab()
                for h in range(4):
                    hs = slice(h * 128, (h + 1) * 128)
                    mm(PY[:, hs], wuv16[:, h, :], obT[:, h, :], reads=[wuv16, obT], writes=[PY])
                yield
                tt("dve", ygT[:].rearrange("p a b -> p (a b)"), PY[:], Rg[:], ALU.mult, reads=[PY, Rg], writes=[ygT])
                yield
                if b == 0 and i <= 2:
                    dump(f"yg{i}", ygT[:].rearrange("p a b -> p (a b)"), ygT)
                yield
                yield
            streams = [[gdn_stream(), 3], [dsa_stream(), 1]]
            pre = pre_gen(b, i + 1) if i + 1 < NT else None
            seen = set()
            while streams or pre is not None:
                for ent in list(streams):
                    for _ in range(ent[1]):
                        try:
                            seen.add(next(ent[0]))
                        except StopIteration:
                            streams.remove(ent)
                            break
                if pre is not None and (("M" in seen and "B" in seen) or not streams):
                    for _ in range(1):
                        try:
                            next(pre)
                        except StopIteration:
                            pre = None
                            break
            for nh in range(2):
                for c in range(8):
                    lhs = ogT[:, c, :] if c < 4 else ygT[:, c - 4, :]
                    mm(PT2h[nh][:], lhs, wout16[:, c, nh * 512:(nh + 1) * 512],
                       start=(c == 0), stop=(c == 7), reads=[ogT, ygT, wout16], writes=[PT2h[nh]])
            for nh in range(2):
                act(junkA[:, nh * 512:(nh + 1) * 512], PT2h[nh][:], AF.Square, reads=[PT2h[nh]],
                    writes=[junkA, msq], accum_out=msq[:, nh:nh + 1])
            tt("dve", ssq1[:], msq[:, 0:1], msq[:, 1:2], ALU.add, reads=[msq], writes=[ssq1])
            rsqrt_col(mrs, ssq1, 1, 1.0 / D, EPS)
            for nh in range(2):
                sl = slice(nh * 512, (nh + 1) * 512)
                stt(otmp[:, sl], PT2h[nh][:], mrs[:, 0:1], GATE[:, sl], ALU.mult, ALU.mult, reads=[PT2h[nh], mrs, GATE], writes=[otmp])
            xr = negmk[:].rearrange("p a b -> p (a b)").bitcast(F32)
            S.dma(xr, x_d[b, t0:t0 + 128, :], writes=[negmk])
            tt("pool", otmp[:], otmp[:], xr, ALU.add, reads=[otmp, negmk], writes=[otmp])
            S.dma(out_d[b, t0:t0 + 128, :], otmp[:], reads=[otmp])
            tix += 1
    S.finish("sp")
    return nc, S


def _masks():
    i = np.arange(128)[:, None]; j = np.arange(128)[None, :]
    m = np.zeros((128, 8, 128), np.float32)
    for ls in range(7):
        sz = 1 << ls
        m[:, ls, :] = (((i // sz) % 2 == 1) & ((j // sz) == (i // sz) - 1)).astype(np.float32)
    m[:, 7, :] = m[:, 0, :].T
    return m


def _layout(inputs, core):
    f = lambda a: np.ascontiguousarray(a, dtype=np.float32)
    bs = slice(core * NBC, (core + 1) * NBC)
    kp = lambda w: w.reshape(8, 128, -1).transpose(1, 0, 2)
    w_in = inputs["w_in"][0]
    ik = w_in[:, O_IK:O_IK + 64]
    w_idx = np.concatenate([w_in[:, O_IQ:O_IQ + 512], ik, ik, w_in[:, O_IW:O_IW + 8]], axis=1)
    b_ada = inputs["b_ada"][0]
    return {
        "x": f(inputs["x"][bs]),
        "cT": f(inputs["c"][bs].T.reshape(8, 128, NBC).transpose(1, 0, 2)),
        "w_ada": f(kp(inputs["w_ada"][0])),
        "b_col": f(b_ada.reshape(24, 128).T),
        "b_gate": f(b_ada[2048:3072].reshape(1, 1024)),
        "g_pre": f(inputs["g_pre"][0].reshape(8, 128).T),
        "w_in": f(kp(w_in[:, :NW16])),
        "w_idx": f(kp(w_idx)),
        "conv_w": f(inputs["conv_w"][0].reshape(4, 12, 128).transpose(2, 1, 0)),
        "a_log": f(inputs["a_log"].reshape(1, 4)),
        "dt_bias": f(inputs["dt_bias"].reshape(1, 4)),
        "g_gdn": f(inputs["g_gdn"].reshape(1, 128)),
        "g_kv": f(inputs["g_kv"].reshape(1, 128)),
        "w_uv": f(inputs["w_uv"][0].transpose(1, 0, 2)),
        "rel_bias": f(inputs["rel_bias"].reshape(1, 128)),
        "w_out": f(kp(inputs["w_out"][0])),
        "g_post": f(inputs["g_post"].reshape(1, 1024)),
        "masks": _masks(),
    }


def kernel(**inputs):
    inputs = {k: np.asarray(v) for k, v in inputs.items()}
    nc, _ = build()
    in_maps = [_layout(inputs, c) for c in range(8)]
    res = run_bass_kernel_spmd(nc, in_maps, core_ids=list(range(8)))
    return np.concatenate([r["out"] for r in res.results], axis=0).astype(np.float32)
```

```python
import math
import numpy as np
import concourse.bass as bass
import concourse.mybir as mybir
from concourse.bass_utils import run_bass_kernel_spmd

F32 = mybir.dt.float32
BF16 = mybir.dt.bfloat16
AF = mybir.ActivationFunctionType
ALU = mybir.AluOpType
AX = mybir.AxisListType

D = 1024
L = 2048
NBC = 4
NT = 16
EPS = 1e-6
NEG = -30000.0
NBIS = 13


class Res:
    __slots__ = ("name", "w", "r")

    def __init__(self, name):
        self.name = name
        self.w = None
        self.r = {}


class Buf:
    def __init__(self, t, name):
        self.t = t
        self.r = Res(name)

    def __getitem__(self, k):
        return self.t[k]


class Sched:
    def __init__(self, nc, ndma=8):
        self.nc = nc
        self.e = {"pe": nc.tensor, "act": nc.scalar, "dve": nc.vector, "pool": nc.gpsimd, "sp": nc.sync}
        self.sem = {k: nc.alloc_semaphore("sem_" + k) for k in self.e}
        self.cnt = {k: 0 for k in self.e}
        self.seen = {k: {} for k in self.e}
        self.dsem = [nc.alloc_semaphore(f"dsem{i}") for i in range(ndma)]
        self.dcnt = [0] * ndma
        self.dnext = 0
        self.nwait = 0

    def _wait(self, eng, key, val):
        if self.seen[eng].get(key, 0) >= val:
            return
        self.seen[eng][key] = val
        sem = self.sem[key] if isinstance(key, str) else self.dsem[key[1]]
        self.e[eng].wait_ge(sem, val)
        self.nwait += 1

    def _deps(self, eng, reads, writes):
        need = {}
        for b in reads:
            r = b.r
            if r.w is not None:
                k, v = r.w
                need[k] = max(need.get(k, 0), v)
        for b in writes:
            w = b.r
            if w.w is not None:
                k, v = w.w
                need[k] = max(need.get(k, 0), v)
            for k, v in w.r.items():
                need[k] = max(need.get(k, 0), v)
        for k, v in need.items():
            if eng == "pe" and k == "pe":
                continue
            self._wait(eng, k, v)

    def op(self, eng, fn, reads=(), writes=()):
        self._deps(eng, reads, writes)
        ins = fn(self.e[eng])
        self.cnt[eng] += 1
        ins.then_inc(self.sem[eng], 1)
        v = self.cnt[eng]
        for b in reads:
            b.r.r[eng] = v
        for b in writes:
            b.r.w = (eng, v)
            b.r.r = {}
        return ins

    def dma(self, out, in_, reads=(), writes=(), q="sp", **kw):
        slot = self.dnext
        self.dnext = (self.dnext + 1) % len(self.dsem)
        key = ("d", slot)
        if self.dcnt[slot] > 0:
            self._wait(q, key, self.dcnt[slot])
        self._deps(q, reads, writes)
        self.dcnt[slot] += 16
        self.e[q].dma_start(out=out, in_=in_, **kw).then_inc(self.dsem[slot], 16)
        v = self.dcnt[slot]
        for b in reads:
            b.r.r[key] = v
        for b in writes:
            b.r.w = (key, v)
            b.r.r = {}

    def finish(self, eng="sp"):
        for k in self.cnt:
            if self.cnt[k] > 0 and k != eng:
                self._wait(eng, k, self.cnt[k])
        for i, c in enumerate(self.dcnt):
            if c > 0:
                self._wait(eng, ("d", i), c)


def t5_bucket_np(n):
    n = np.maximum(n, 0)
    nf = np.maximum(n, 1).astype(np.float32)
    large = 16 + (np.log(nf / np.float32(16)) / np.float32(math.log(128 / 16)) * np.float32(16)).astype(np.int32)
    large = np.minimum(large, 31)
    return np.where(n < 16, n, large)


O_QKV, O_ZA, O_B, O_A, O_QB, O_CKV, O_ZB, O_IQ, O_IK, O_IW = 0, 1536, 2048, 2052, 2056, 2568, 2696, 3208, 3720, 3784
NW16 = 3208
NIDX = 648


def build(debug=None):
    nc = bass.Bass("TRN2", target_bir_lowering=False)
    din = lambda n, sh: nc.dram_tensor(n, sh, F32, kind="ExternalInput").ap()
    x_d = din("x", [NBC, L, D])
    cT_d = din("cT", [128, 8, NBC])
    wada_d = din("w_ada", [128, 8, 3072])
    bcol_d = din("b_col", [128, 24])
    bgate_d = din("b_gate", [1, 1024])
    gpre_d = din("g_pre", [128, 8])
    win_d = din("w_in", [128, 8, NW16])
    widx_d = din("w_idx", [128, 8, NIDX])
    conv_d = din("conv_w", [128, 12, 4])
    alog_d = din("a_log", [1, 4])
    dtb_d = din("dt_bias", [1, 4])
    ggdn_d = din("g_gdn", [1, 128])
    gkv_d = din("g_kv", [1, 128])
    wuv_d = din("w_uv", [128, 4, 128])
    rb_d = din("rel_bias", [1, 128])
    wout_d = din("w_out", [128, 8, 1024])
    gpost_d = din("g_post", [1, 1024])
    msk_d = din("masks", [128, 8, 128])
    out_d = nc.dram_tensor("out", [NBC, L, D], F32, kind="ExternalOutput").ap()
    dbg_d = {}
    if debug:
        for n, sh in debug.items():
            dbg_d[n] = nc.dram_tensor("dbg_" + n, list(sh), F32, kind="ExternalOutput").ap()

    S = Sched(nc)
    cnt = [0]

    def sb(shape, dt=F32, name=None):
        cnt[0] += 1
        name = "s_" + (name or f"t{cnt[0]}")
        return Buf(nc.alloc_sbuf_tensor(name, list(shape), dt), name)

    def ps(shape, dt=F32, name=None):
        cnt[0] += 1
        name = "p_" + (name or f"p{cnt[0]}")
        return Buf(nc.alloc_psum_tensor(name, list(shape), dt), name)

    def mm(out, lhsT, rhs, start=True, stop=True, reads=(), writes=()):
        S.op("pe", lambda e: e.matmul(out, lhsT, rhs, start=start, stop=stop), reads, writes)

    def tr(out, in_, ident, reads=(), writes=()):
        S.op("pe", lambda e: e.transpose(out, in_, ident), reads, writes)

    def act(out, in_, func, reads=(), writes=(), **kw):
        S.op("act", lambda e: e.activation(out, in_, func, **kw), reads, writes)

    def tt(eng, out, in0, in1, op, reads=(), writes=()):
        S.op(eng, lambda e: e.tensor_tensor(out, in0, in1, op=op), reads, writes)

    def ts(eng, out, in0, s1, s2, op0, op1=None, reads=(), writes=(), accum_out=None):
        if op1 is None:
            S.op(eng, lambda e: e.tensor_scalar(out, in0, s1, None, op0=op0), reads, writes)
        elif accum_out is not None:
            S.op(eng, lambda e: e.tensor_scalar(out, in0, s1, s2, op0=op0, op1=op1, accum_out=accum_out), reads, writes)
        else:
            S.op(eng, lambda e: e.tensor_scalar(out, in0, s1, s2, op0=op0, op1=op1), reads, writes)

    def stt(out, in0, scalar, in1, op0, op1, reads=(), writes=()):
        S.op("dve", lambda e: e.scalar_tensor_tensor(out, in0, scalar, in1, op0=op0, op1=op1), reads, writes)

    def cp(eng, out, in_, reads=(), writes=()):
        if eng == "act":
            S.op("act", lambda e: e.copy(out, in_), reads, writes)
        else:
            S.op(eng, lambda e: e.tensor_copy(out, in_), reads, writes)

    def dump(name, ap, buf):
        if name in dbg_d:
            S.dma(dbg_d[name], ap, reads=[buf], q="pool")

    big4 = sb([128, 1024], name="big4")
    rtmp2 = sb([128, 512], name="rtmp2")

    def view(buf, ap):
        v = Buf.__new__(Buf)
        v.t = ap
        v.r = buf.r
        return v
    io = view(big4, big4[:, 0:128])
    S.op("pool", lambda e: e.iota(io[:], [[1, 128]], base=0, channel_multiplier=-1,
                                  allow_small_or_imprecise_dtypes=True), writes=[io])
    ident = sb([128, 128], name="ident")
    ident16 = sb([128, 128], BF16, name="ident16")
    Umat = sb([128, 128], name="Umat")
    SLmat = sb([128, 128], name="SLmat")
    NEGs = sb([128, 128], name="NEGs")
    NEGsT = sb([128, 128], name="NEGsT")
    NEGC = sb([128, 128], name="NEGC")
    ones32 = sb([128, 128], name="ones32")
    ones16 = sb([128, 128], BF16, name="ones16")
    mhalf = sb([128, 8], name="mhalf")
    ts("dve", ident[:], io[:], 0.0, None, ALU.is_equal, reads=[io], writes=[ident])
    ts("dve", ident16[:], io[:], 0.0, None, ALU.is_equal, reads=[io], writes=[ident16])
    ts("dve", Umat[:], io[:], 0.0, None, ALU.is_ge, reads=[io], writes=[Umat])
    ts("dve", SLmat[:], io[:], 0.0, None, ALU.is_lt, reads=[io], writes=[SLmat])
    ts("dve", NEGs[:], io[:], 0.0, NEG, ALU.is_ge, ALU.mult, reads=[io], writes=[NEGs])
    ts("dve", NEGsT[:], io[:], 0.0, NEG, ALU.is_le, ALU.mult, reads=[io], writes=[NEGsT])
    ts("dve", NEGC[:], io[:], 0.0, -1e30, ALU.is_gt, ALU.mult, reads=[io], writes=[NEGC])
    S.op("pool", lambda e: e.memset(ones32[:], 1.0), writes=[ones32])
    S.op("pool", lambda e: e.memset(ones16[:], 1.0), writes=[ones16])
    S.op("pool", lambda e: e.memset(mhalf[:], -0.5), writes=[mhalf])
    pow2 = sb([128, NBIS + 1], name="pow2")
    for k in range(NBIS + 1):
        S.op("pool", lambda e, k=k: e.memset(pow2[:, k:k + 1], 0.5 ** (k + 1)), writes=[pow2])

    PT2a = ps([128, 512], name="PT2a")
    PT2b = ps([128, 512], name="PT2b")
    PT2h = [PT2a, PT2b]
    rotg = [0]

    def pabG():
        rotg[0] ^= 1
        return PT2a if rotg[0] else PT2b
    PA = ps([128, 512], name="PA")
    PB = ps([128, 512], name="PB")
    PC = ps([128, 512], name="PC")
    PO = ps([128, 512], name="PO")
    PO2 = ps([128, 512], name="PO2")
    PS_ = ps([128, 512], name="PS_")
    rot = [0]

    def pab():
        rot[0] ^= 1
        return PA if rot[0] else PB

    stage = [sb([128, 8, 256], name="stage0"), sb([128, 8, 256], name="stage1")]
    w16 = sb([128, 8, NW16], BF16, name="w16")
    widx = sb([128, 8, NIDX], name="widx")
    wout16 = sb([128, 8, 1024], BF16, name="wout16")
    wuv16 = sb([128, 4, 128], BF16, name="wuv16")
    S.dma(widx[:], widx_d, writes=[widx])
    si = 0
    for c0 in range(0, NW16, 256):
        w = min(256, NW16 - c0)
        st = stage[si % 2]; si += 1
        S.dma(st[:, :, 0:w], win_d[:, :, c0:c0 + w], writes=[st])
        cp("dve" if si % 2 else "pool", w16[:, :, c0:c0 + w], st[:, :, 0:w], reads=[st], writes=[w16])
    for c0 in range(0, 1024, 256):
        st = stage[si % 2]; si += 1
        S.dma(st[:], wout_d[:, :, c0:c0 + 256], writes=[st])
        cp("dve" if si % 2 else "pool", wout16[:, :, c0:c0 + 256], st[:], reads=[st], writes=[wout16])
    st = stage[si % 2]; si += 1
    S.dma(st[:, 0:4, 0:128], wuv_d, writes=[st])
    cp("dve", wuv16[:], st[:, 0:4, 0:128], reads=[st], writes=[wuv16])

    st = stage[si % 2]; si += 1
    S.dma(st[:, :, 0:128], msk_d, writes=[st])
    msk = sb([128, 8, 128], BF16, name="msk")
    cp("dve", msk[:], st[:, :, 0:128], reads=[st], writes=[msk])
    convw = sb([128, 12, 4], name="convw")
    S.dma(convw[:], conv_d, writes=[convw])
    diagw = sb([128, 12, 4, 128], BF16, name="diagw")
    for ch in range(12):
        for j in range(4):
            ts("dve" if (ch + j) % 2 else "pool", diagw[:, ch, j, :], ident[:], convw[:, ch, j:j + 1], None, ALU.mult,
               reads=[ident, convw], writes=[diagw])

    def bcast_row(src, n, name):
        t = sb([128, n], name=name)
        S.dma(t[:], src.partition_broadcast(128), writes=[t])
        return t
    alogB = bcast_row(alog_d, 4, "alogB")
    dtbB = bcast_row(dtb_d, 4, "dtbB")
    ggdnB = bcast_row(ggdn_d, 128, "ggdnB")
    gkvB = bcast_row(gkv_d, 128, "gkvB")
    rbB = bcast_row(rb_d, 128, "rbB")
    negA = sb([128, 4], name="negA")
    act(negA[:], alogB[:], AF.Exp, reads=[alogB], writes=[negA])
    ts("dve", negA[:], negA[:], -1.0, None, ALU.mult, reads=[negA], writes=[negA])
    ggdnH = ggdnB

    cT = sb([128, 8, NBC], name="cT")
    S.dma(cT[:], cT_d, writes=[cT])
    sc = sb([128, 8, NBC], name="sc")
    act(sc[:], cT[:], AF.Silu, reads=[cT], writes=[sc])
    bcol = sb([128, 24], name="bcol")
    S.dma(bcol[:], bcol_d, writes=[bcol])
    gpre = sb([128, 8], name="gpre")
    S.dma(gpre[:], gpre_d, writes=[gpre])
    shiftc = sb([128, 8, NBC], name="shiftc")
    Gc = sb([128, 8, NBC], name="Gc")
    GATE = sb([128, 1024], name="GATE")
    for n8 in range(8):
        st = stage[si % 2]; si += 1
        S.dma(st[:], wada_d[:, :, n8 * 256:(n8 + 1) * 256], writes=[st])
        for q2 in range(2):
            dch = (n8 % 4) * 2 + q2
            for kc in range(8):
                mm(PC[:, q2 * 4:q2 * 4 + 4], st[:, kc, q2 * 128:(q2 + 1) * 128], sc[:, kc, :],
                   start=(kc == 0), stop=(kc == 7), reads=[st, sc], writes=[PC])
            dst = shiftc if n8 < 4 else Gc
            ts("dve", dst[:, dch, :], PC[:, q2 * 4:q2 * 4 + 4], bcol[:, n8 * 2 + q2:n8 * 2 + q2 + 1], None, ALU.add,
               reads=[PC, bcol], writes=[dst])
    ts("dve", Gc[:], Gc[:], 1.0, None, ALU.add, reads=[Gc], writes=[Gc])
    tt("dve", Gc[:], Gc[:], gpre[:].unsqueeze(2).to_broadcast([128, 8, NBC]), ALU.mult, reads=[Gc, gpre], writes=[Gc])

    bk = t5_bucket_np(np.arange(256))
    lo_b = [int(np.argmax(bk >= b)) if (bk >= b).any() else 100000 for b in range(32)]
    rb3 = rbB[:].rearrange("p (b h) -> p b h", h=4)
    dlt = sb([128, 32, 4], name="dlt")
    cp("dve", dlt[:, 0:1, :], rb3[:, 0:1, :], reads=[rbB], writes=[dlt])
    tt("dve", dlt[:, 1:32, :], rb3[:, 1:32, :], rb3[:, 0:31, :], ALU.subtract, reads=[rbB], writes=[dlt])
    EB = [sb([128, 4, 128], BF16, name=f"EB{t}") for t in range(2)]
    EBf = view(rtmp2, rtmp2[:, 0:128])
    rtmp = sb([128, 512], name="rtmp")
    gUb = [sb([128, 128], name=f"gU{i}") for i in range(2)]
    distT = gUb[0]
    tmpb = gUb[1]
    for typ in range(2):
        ts("dve", distT[:], io[:], float(128 * typ), None, ALU.add, reads=[io], writes=[distT])
        for h in range(4):
            acc = EBf
            ts("dve", acc[:], ones32[:], dlt[:, 0, h:h + 1], rb3[:, 31, h:h + 1], ALU.mult, ALU.subtract,
               reads=[ones32, dlt, rbB], writes=[acc])
            for bb in range(1, 32):
                if lo_b[bb] > 255:
                    continue
                ts("dve", tmpb[:], distT[:], float(lo_b[bb]) - 0.5, dlt[:, bb, h:h + 1], ALU.is_ge, ALU.mult,
                   reads=[distT, dlt], writes=[tmpb])
                tt("dve", acc[:], acc[:], tmpb[:], ALU.add, reads=[acc, tmpb], writes=[acc])
            ts("dve", acc[:], acc[:], 128.0 ** 0.5, None, ALU.mult, reads=[acc], writes=[acc])
            cp("dve", EB[typ][:, h, :], acc[:], reads=[acc], writes=[EB[typ]])

    xt = sb([128, 1024], name="xt")
    junkA = big4; xs = big4; otmp = big4
    hT32 = sb([128, 8, 128], name="hT32")
    hT16 = sb([128, 8, 128], BF16, name="hT16")
    uT = sb([128, 12, 131], BF16, name="uT")
    qkvs = sb([128, 1536], BF16, name="qkvs")
    col = lambda n, name: sb([128, n], name=name)
    ssq1 = col(1, "ssq1"); rstd1 = col(1, "rstd1"); ssqP2 = col(2, "ssqP2"); ssqP = col(1, "ssqP"); rstdP = col(1, "rstdP")
    xs2 = sb([128, 512], name="xs2")
    ssq8 = col(8, "ssq8"); rs8 = col(8, "rs8")
    ba = col(8, "ba"); beta = col(4, "beta"); gcol = col(4, "gcol"); gc = col(4, "gc"); glB = col(4, "glB")
    egc = col(4, "egc"); ekg = col(4, "ekg"); egl = col(4, "egl"); tmp4 = col(4, "tmp4"); nbeta = col(4, "nbeta")
    cf = {n: col(4, "cf_" + n) for n in ("kbg", "kg", "qg")}
    khat = sb([128, 4, 128], BF16, name="khat"); qhat = sb([128, 4, 128], BF16, name="qhat")
    qg = sb([128, 4, 128], BF16, name="qg"); kbg = sb([128, 4, 128], BF16, name="kbg")
    kg = sb([128, 4, 128], BF16, name="kg"); vb = sb([128, 4, 128], BF16, name="vb")
    khT = sb([128, 4, 128], BF16, name="khT"); qhT = sb([128, 4, 128], BF16, name="qhT"); qgT = sb([128, 4, 128], BF16, name="qgT")
    Es = sb([128, 4, 128], BF16, name="Es"); EsT = sb([128, 4, 128], BF16, name="EsT")
    Xb = [sb([128, 4, 128], BF16, name="X0")]
    XTb = [sb([128, 4, 128], BF16, name="XT0")]
    Tb = [sb([128, 4, 128], BF16, name=f"T{i}") for i in range(2)]
    TTb = [sb([128, 4, 128], BF16, name=f"TT{i}") for i in range(2)]
    Wp = khat
    attnT = sb([128, 4, 128], BF16, name="attnT")
    negwT = qhat
    vnew = qg
    S32 = sb([128, 4, 128], name="S32"); S16 = sb([128, 4, 128], BF16, name="S16")
    zas = sb([128, 512], BF16, name="zas"); G1 = zas
    osq4 = col(4, "osq4"); ors4 = col(4, "ors4")
    og = sb([128, 4, 128], BF16, name="og"); ogT = sb([128, 4, 128], BF16, name="ogT")
    qbT = sb([128, 4, 128], BF16, name="qbT"); szbT = sb([128, 4, 128], BF16, name="szbT")
    iqT = sb([128, 4, 128], name="iqT"); iw = col(8, "iw")
    ckvn = sb([128, NT, 128], BF16, name="ckvn"); ckvnT = sb([128, L], BF16, name="ckvnT")
    ikTb = stage[1]
    ikT = ikTb[:].rearrange("p a b -> p (a b)")
    score = stage[0]
    scoreF = score[:].rearrange("p a b -> p (a b)")
    mk = sb([128, L], BF16, name="mk")
    junkD = mk
    junkDF = mk
    lo = col(1, "lo"); hw0 = col(1, "hw0"); hwk = col(NBIS + 1, "hwk"); nhwk = col(NBIS + 1, "nhwk"); mid = col(1, "mid"); mid2 = col(1, "mid2"); sgnc = col(1, "sgnc"); cbc = col(1, "cbc"); cntc = col(1, "cntc"); tstep = col(1, "tstep")
    Eb = [sb([128, 4, 128], BF16, name=f"Eb{i}") for i in range(2)]
    negmk = sb([128, NT, 128], BF16, name="negmk"); mkT = negmk
    obT = sb([128, 4, 128], BF16, name="obT")
    rden = rtmp; Rg = rden
    ygT = sb([128, 4, 128], BF16, name="ygT")
    msq = col(2, "msq"); mrs = col(1, "mrs")

    def rsqrt_col(dst, src, n, scale, eps):
        ts("dve", dst[:, 0:n], src[:, 0:n], scale, eps, ALU.mult, ALU.add, reads=[src], writes=[dst])
        tt("pool", dst[:, 0:n], dst[:, 0:n], mhalf[:, 0:n], ALU.pow, reads=[dst, mhalf], writes=[dst])

    def pre_gen(b, i):
        t0 = i * 128
        uc = uT
        S.dma(xt[:], x_d[b, t0:t0 + 128, :], writes=[xt])
        yield
        for hf in range(2):
            act(xs2[:], xt[:, hf * 512:(hf + 1) * 512], AF.Square, reads=[xt], writes=[xs2, ssqP2], accum_out=ssqP2[:, hf:hf + 1])
        tt("pool", ssqP[:], ssqP2[:, 0:1], ssqP2[:, 1:2], ALU.add, reads=[ssqP2], writes=[ssqP])
        rsqrt_col(rstdP, ssqP, 1, 1.0 / D, EPS)
        yield
        for hf in range(2):
            ts("dve", xs2[:], xt[:, hf * 512:(hf + 1) * 512], rstdP[:, 0:1], None, ALU.mult, reads=[xt, rstdP], writes=[xs2])
            for c4 in range(4):
                tr(PC[:, c4 * 128:(c4 + 1) * 128], xs2[:, c4 * 128:(c4 + 1) * 128], ident[:], reads=[xs2, ident], writes=[PC])
            for c4 in range(4):
                c = hf * 4 + c4
                ts("dve", hT32[:, c, :], PC[:, c4 * 128:(c4 + 1) * 128], Gc[:, c, b:b + 1], shiftc[:, c, b:b + 1], ALU.mult, ALU.add,
                   reads=[PC, Gc, shiftc], writes=[hT32])
            yield
        cp("pool", hT16[:], hT32[:], reads=[hT32], writes=[hT16])
        if b == 0 and i == 0:
            dump("hT", hT32[:], hT32)
        yield
        for g3 in range(3):
            for q4 in range(4):
                ch = g3 * 4 + q4
                for kc in range(8):
                    mm(PC[:, q4 * 128:(q4 + 1) * 128], w16[:, kc, O_QKV + ch * 128:O_QKV + (ch + 1) * 128], hT16[:, kc, :],
                       start=(kc == 0), stop=(kc == 7), reads=[w16, hT16], writes=[PC])
            cp("dve", uc[:, g3 * 4:(g3 + 1) * 4, 3:131], PC[:].rearrange("p (a b) -> p a b", a=4), reads=[PC], writes=[uc])
            yield
        for g3 in range(3):
            for q4 in range(4):
                ch = g3 * 4 + q4
                for j in range(4):
                    mm(PC[:, q4 * 128:(q4 + 1) * 128], uc[:, ch, j:j + 128], diagw[:, ch, j, :],
                       start=(j == 0), stop=(j == 3), reads=[uc, diagw], writes=[PC])
            act(qkvs[:, g3 * 512:(g3 + 1) * 512], PC[:], AF.Silu, reads=[PC], writes=[qkvs])
            yield
        cp("pool", uc[:, :, 0:3], uc[:, :, 128:131], reads=[uc], writes=[uc])
        if b == 0 and i == 0:
            dump("qkvs", qkvs[:], qkvs)
        for hf in range(2):
            tt("dve", xs2[:], qkvs[:, hf * 512:(hf + 1) * 512], qkvs[:, hf * 512:(hf + 1) * 512], ALU.mult, reads=[qkvs], writes=[xs2])
            S.op("dve", lambda e, hf=hf: e.tensor_reduce(ssq8[:, hf * 4:(hf + 1) * 4], xs2[:].rearrange("p (a b) -> p a b", a=4), axis=AX.X, op=ALU.add),
                 reads=[xs2], writes=[ssq8])
        rsqrt_col(rs8, ssq8, 8, 1.0, EPS)
        ts("dve", rs8[:, 0:4], rs8[:, 0:4], 128.0 ** -0.5, None, ALU.mult, reads=[rs8], writes=[rs8])
        yield
        for c4 in range(4):
            for kc in range(8):
                mm(PC[:, c4 * 128:(c4 + 1) * 128], widx[:, kc, c4 * 128:(c4 + 1) * 128], hT32[:, kc, :],
                   start=(kc == 0), stop=(kc == 7), reads=[widx, hT32], writes=[PC])
            if c4 % 2:
                yield
        cp("dve", iqT[:], PC[:].rearrange("p (a b) -> p a b", a=4), reads=[PC], writes=[iqT])
        yield
        for kc in range(8):
            mm(PC[:, 0:128], widx[:, kc, 512:640], hT32[:, kc, :], start=(kc == 0), stop=(kc == 7), reads=[widx, hT32], writes=[PC])
        cp("dve", ikT[:, t0:t0 + 128], PC[:, 0:128], reads=[PC], writes=[ikTb])
        for kc in range(8):
            mm(PC[:, 0:8], hT32[:, kc, :], widx[:, kc, 640:648], start=(kc == 0), stop=(kc == 7), reads=[hT32, widx], writes=[PC])
        ts("dve", iw[:], PC[:, 0:8], (8.0 * 64.0) ** -0.5, None, ALU.mult, reads=[PC], writes=[iw])
        yield
        for kc in range(8):
            mm(PC[:, 16:24], hT16[:, kc, :], w16[:, kc, O_B:O_B + 8], start=(kc == 0), stop=(kc == 7), reads=[hT16, w16], writes=[PC])
        for kc in range(8):
            mm(PC[:, 128:256], hT16[:, kc, :], w16[:, kc, O_CKV:O_CKV + 128], start=(kc == 0), stop=(kc == 7), reads=[hT16, w16], writes=[PC])
        cp("dve", ba[:], PC[:, 16:24], reads=[PC], writes=[ba])
        act(xs2[:, 0:128], PC[:, 128:256], AF.Square, reads=[PC], writes=[xs2, ssqP], accum_out=ssqP[:, 0:1])
        rsqrt_col(rstdP, ssqP, 1, 1.0 / 128, EPS)
        stt(ckvn[:, i, :], PC[:, 128:256], rstdP[:, 0:1], gkvB[:], ALU.mult, ALU.mult, reads=[PC, rstdP, gkvB], writes=[ckvn])
        yield
        PC16 = PC[:].bitcast(BF16)
        tr(PC16[:, 512:640], ckvn[:, i, :], ident16[:], reads=[ckvn, ident16], writes=[PC])
        cp("dve", ckvnT[:, t0:t0 + 128], PC16[:, 512:640], reads=[PC], writes=[ckvnT])
        yield

    tix = 0
    screp = xt[:].rearrange("p (a b) -> p a b", a=8)
    for b in range(NBC):
        S.op("pool", lambda e: e.memset(S32[:], 0.0), writes=[S32])
        S.op("pool", lambda e: e.memset(S16[:], 0.0), writes=[S16])
        S.op("pool", lambda e: e.memset(uT[:, :, 0:3], 0.0), writes=[uT])
        for kc in range(8):
            cp("dve" if kc % 2 else "pool", screp[:, kc, :], sc[:, kc, b:b + 1].to_broadcast([128, 128]), reads=[sc], writes=[xt])
        for g4 in range(4):
            st = stage[g4 % 2]
            g0 = g4 * 256
            S.dma(st[:], wada_d[:, :, 2048 + g0:2048 + g0 + 256], writes=[st])
            S.dma(rtmp[:, 0:256], bgate_d[:, g0:g0 + 256].partition_broadcast(128), writes=[rtmp])
            S.dma(rtmp[:, 256:512], gpost_d[:, g0:g0 + 256].partition_broadcast(128), writes=[rtmp])
            P = pab()
            for kc in range(8):
                mm(P[:, 0:256], screp[:, kc, :], st[:, kc, :], start=(kc == 0), stop=(kc == 7), reads=[xt, st], writes=[P])
            tt("dve", GATE[:, g0:g0 + 256], P[:, 0:256], rtmp[:, 0:256], ALU.add, reads=[P, rtmp], writes=[GATE])
            tt("pool", GATE[:, g0:g0 + 256], GATE[:, g0:g0 + 256], rtmp[:, 256:512], ALU.mult, reads=[GATE, rtmp], writes=[GATE])
        for i in range(NT):
            t0 = i * 128
            uc = uT
            if i == 0:
                for _ in pre_gen(b, 0):
                    pass
            P = pab()
            for kc in range(8):
                mm(P[:], hT16[:, kc, :], w16[:, kc, O_ZA:O_ZA + 512], start=(kc == 0), stop=(kc == 7), reads=[hT16, w16], writes=[P])
            act(zas[:], P[:], AF.Silu, reads=[P], writes=[zas])
            tt("pool", zas[:].rearrange("p (h v) -> p h v", h=4), zas[:].rearrange("p (h v) -> p h v", h=4),
               ggdnH[:].unsqueeze(1).to_broadcast([128, 4, 128]), ALU.mult, reads=[zas, ggdnH], writes=[zas])
            def gdn_stream():
                act(beta[:], ba[:, 0:4], AF.Exp, reads=[ba], writes=[beta], scale=-1.0)
                yield
                ts("dve", beta[:], beta[:], 1.0, None, ALU.add, reads=[beta], writes=[beta])
                yield
                S.op("dve", lambda e: e.reciprocal(beta[:], beta[:]), reads=[beta], writes=[beta])
                yield
                ts("dve", nbeta[:], beta[:], -1.0, None, ALU.mult, reads=[beta], writes=[nbeta])
                yield
                tt("dve", tmp4[:], ba[:, 4:8], dtbB[:], ALU.add, reads=[ba, dtbB], writes=[tmp4])
                yield
                act(tmp4[:], tmp4[:], AF.Exp, reads=[tmp4], writes=[tmp4])
                yield
                act(tmp4[:], tmp4[:], AF.Ln, reads=[tmp4], writes=[tmp4], bias=1.0)
                yield
                tt("dve", gcol[:], tmp4[:], negA[:], ALU.mult, reads=[tmp4, negA], writes=[gcol])
                yield
                mm(PC[:, 16:20], Umat[:], gcol[:], reads=[Umat, gcol], writes=[PC])
                yield
                mm(PC[:, 20:24], ones32[:], gcol[:], reads=[ones32, gcol], writes=[PC])
                yield
                cp("dve", gc[:], PC[:, 16:20], reads=[PC], writes=[gc])
                yield
                cp("dve", glB[:], PC[:, 20:24], reads=[PC], writes=[glB])
                yield
                act(egc[:], gc[:], AF.Exp, reads=[gc], writes=[egc])
                yield
                act(egl[:], glB[:], AF.Exp, reads=[glB], writes=[egl])
                yield
                tt("dve", tmp4[:], glB[:], gc[:], ALU.subtract, reads=[glB, gc], writes=[tmp4])
                yield
                act(ekg[:], tmp4[:], AF.Exp, reads=[tmp4], writes=[ekg])
                yield
                tt("dve", cf["kbg"][:], rs8[:, 4:8], beta[:], ALU.mult, reads=[rs8, beta], writes=[cf["kbg"]])
                yield
                tt("dve", cf["kbg"][:], cf["kbg"][:], egc[:], ALU.mult, reads=[cf["kbg"], egc], writes=[cf["kbg"]])
                yield
                tt("dve", cf["kg"][:], rs8[:, 4:8], ekg[:], ALU.mult, reads=[rs8, ekg], writes=[cf["kg"]])
                yield
                tt("dve", cf["qg"][:], rs8[:, 0:4], egc[:], ALU.mult, reads=[rs8, egc], writes=[cf["qg"]])
                yield
                q3 = qkvs[:, 0:512].rearrange("p (h d) -> p h d", h=4)
                yield
                k3 = qkvs[:, 512:1024].rearrange("p (h d) -> p h d", h=4)
                yield
                v3 = qkvs[:, 1024:1536].rearrange("p (h d) -> p h d", h=4)
                yield
                bc = lambda c, lo_=0: c[:, lo_:lo_ + 4].unsqueeze(2).to_broadcast([128, 4, 128])
                yield
                tt("dve", khat[:], k3, bc(rs8, 4), ALU.mult, reads=[qkvs, rs8], writes=[khat])
                yield
                tt("pool", qhat[:], q3, bc(rs8, 0), ALU.mult, reads=[qkvs, rs8], writes=[qhat])
                yield
                tt("dve", qg[:], q3, bc(cf["qg"]), ALU.mult, reads=[qkvs, cf["qg"]], writes=[qg])
                yield
                tt("pool", kbg[:], k3, bc(cf["kbg"]), ALU.mult, reads=[qkvs, cf["kbg"]], writes=[kbg])
                yield
                tt("dve", kg[:], k3, bc(cf["kg"]), ALU.mult, reads=[qkvs, cf["kg"]], writes=[kg])
                yield
                tt("pool", vb[:], v3, bc(beta), ALU.mult, reads=[qkvs, beta], writes=[vb])
                yield "M"
                for src, dst in ((khat, khT), (qhat, qhT), (qg, qgT)):
                    P = pabG()
                    P16 = P[:].bitcast(BF16)
                    for h in range(4):
                        tr(P16[:, h * 128:(h + 1) * 128], src[:, h, :], ident16[:], reads=[src, ident16], writes=[P])
                    cp("act", dst[:], P16[:, 0:512].rearrange("p (a b) -> p a b", a=4), reads=[P], writes=[dst])
                yield
                PD = pabG(); PDT = pabG()
                yield
                for h in range(4):
                    gU = gUb[h % 2]
                    ts("dve" if h % 2 else "pool", gU[:], Umat[:], gcol[:, h:h + 1], None, ALU.mult, reads=[Umat, gcol], writes=[gU])
                    mm(PD[:, h * 128:(h + 1) * 128], gU[:], SLmat[:], start=True, stop=False, reads=[gU, SLmat], writes=[PD])
                    mm(PD[:, h * 128:(h + 1) * 128], ident[:], NEGs[:], start=False, stop=True, reads=[ident, NEGs], writes=[PD])
                    mm(PDT[:, h * 128:(h + 1) * 128], SLmat[:], gU[:], start=True, stop=False, reads=[gU, SLmat], writes=[PDT])
                    mm(PDT[:, h * 128:(h + 1) * 128], ident[:], NEGsT[:], start=False, stop=True, reads=[ident, NEGsT], writes=[PDT])
                yield
                act(Es[:], PD[:].rearrange("p (a b) -> p a b", a=4), AF.Exp, reads=[PD], writes=[Es])
                yield
                act(EsT[:], PDT[:].rearrange("p (a b) -> p a b", a=4), AF.Exp, reads=[PDT], writes=[EsT])
                yield
                if b == 0 and i == 0:
                    dump("Es", Es[:].rearrange("p a b -> p (a b)"), Es)
                    dump("beta4", beta[:], beta); dump("gc4", gc[:], gc); dump("rs8", rs8[:], rs8)
                yield
                P = pabG()
                yield
                for h in range(4):
                    mm(P[:, h * 128:(h + 1) * 128], khT[:, h, :], khT[:, h, :], reads=[khT], writes=[P])
                yield
                X, XT = Xb[0], XTb[0]
                yield
                for h in range(4):
                    stt(X[:, h, :], P[:, h * 128:(h + 1) * 128], nbeta[:, h:h + 1], Es[:, h, :], ALU.mult, ALU.mult,
                        reads=[P, nbeta, Es], writes=[X])
                yield
                if b == 0 and i == 0:
                    dump("X0", X[:].rearrange("p a b -> p (a b)"), X)
                yield
                P = pabG()
                yield
                for h in range(4):
                    mm(P[:, h * 128:(h + 1) * 128], khT[:, h, :], qhT[:, h, :], reads=[khT, qhT], writes=[P])
                yield
                tt("pool", EsT[:], EsT[:], ident16[:].unsqueeze(1).to_broadcast([128, 4, 128]), ALU.add, reads=[EsT, ident16], writes=[EsT])
                yield
                tt("dve", attnT[:], P[:].rearrange("p (a b) -> p a b", a=4), EsT[:], ALU.mult, reads=[P, EsT], writes=[attnT])
                yield
                P = pabG()
                yield
                P16 = P[:].bitcast(BF16)
                yield
                for h in range(4):
                    tr(P16[:, h * 128:(h + 1) * 128], X[:, h, :], ident16[:], reads=[X, ident16], writes=[P])
                yield
                cp("act", XT[:], P16[:, 0:512].rearrange("p (a b) -> p a b", a=4), reads=[P], writes=[XT])
                yield
                bcm = lambda ls: msk[:, ls, :].unsqueeze(1).to_broadcast([128, 4, 128])
                yield
                Tc, TT = Tb[0], TTb[0]
                yield
                tt("pool", Tc[:], X[:], bcm(0), ALU.mult, reads=[X, msk], writes=[Tc])
                yield
                tt("pool", Tc[:], Tc[:], ident16[:].unsqueeze(1).to_broadcast([128, 4, 128]), ALU.add, reads=[Tc, ident16], writes=[Tc])
                yield
                tt("dve", TT[:], XT[:], bcm(7), ALU.mult, reads=[XT, msk], writes=[TT])
                yield
                tt("dve", TT[:], TT[:], ident16[:].unsqueeze(1).to_broadcast([128, 4, 128]), ALU.add, reads=[TT, ident16], writes=[TT])
                yield
                gen = 0
                yield
                for ls in range(1, 7):
                    Tn, TTn = Tb[1 - gen], TTb[1 - gen]
                    P1 = pabG()
                    for h in range(4):
                        mm(P1[:, h * 128:(h + 1) * 128], XT[:, h, :], Tc[:, h, :], reads=[XT, Tc], writes=[P1])
                    tt("dve", Wp[:], P1[:].rearrange("p (a b) -> p a b", a=4), bcm(ls), ALU.mult, reads=[P1, msk], writes=[Wp])
                    if ls < 6:
                        P2 = pabG()
                        for h in range(4):
                            mm(P2[:, h * 128:(h + 1) * 128], TT[:, h, :], Wp[:, h, :], reads=[TT, Wp], writes=[P2])
                        tt("dve", Tn[:], P2[:].rearrange("p (a b) -> p a b", a=4), Tc[:], ALU.add, reads=[P2, Tc], writes=[Tn])
                    P3 = PS_
                    for h in range(4):
                        mm(P3[:, h * 128:(h + 1) * 128], Wp[:, h, :], TT[:, h, :], reads=[Wp, TT], writes=[P3])
                    tt("dve", TTn[:], P3[:].rearrange("p (a b) -> p a b", a=4), TT[:], ALU.add, reads=[P3, TT], writes=[TTn])
                    Tc, TT = Tn, TTn
                    gen = 1 - gen
                    yield
                yield
                if b == 0 and i == 0:
                    dump("TTf", TT[:].rearrange("p a b -> p (a b)"), TT)
                    dump("attnT0", attnT[:].rearrange("p a b -> p (a b)"), attnT)
                yield
                P = pabG()
                yield
                for h in range(4):
                    mm(P[:, h * 128:(h + 1) * 128], kbg[:, h, :], TT[:, h, :], reads=[kbg, TT], writes=[P])
                yield
                ts("dve", negwT[:], P[:].rearrange("p (a b) -> p a b", a=4), -1.0, None, ALU.mult, reads=[P], writes=[negwT])
                yield
                PV = pabG()
                for h in range(4):
                    hs = slice(h * 128, (h + 1) * 128)
                    mm(PV[:, hs], TT[:, h, :], vb[:, h, :], start=True, stop=False, reads=[TT, vb], writes=[PV])
                    mm(PV[:, hs], negwT[:, h, :], S16[:, h, :], start=False, stop=True, reads=[negwT, S16], writes=[PV])
                yield
                cp("act", vnew[:], PV[:].rearrange("p (a b) -> p a b", a=4), reads=[PV], writes=[vnew])
                yield
                if b == 0 and i == 0:
                    dump("vnew0", vnew[:].rearrange("p a b -> p (a b)"), vnew)
                yield
                for h in range(4):
                    hs = slice(h * 128, (h + 1) * 128)
                    mm(PS_[:, hs], qgT[:, h, :], S16[:, h, :], start=True, stop=False, reads=[qgT, S16], writes=[PS_])
                    mm(PS_[:, hs], attnT[:, h, :], vnew[:, h, :], start=False, stop=True, reads=[attnT, vnew], writes=[PS_])
                yield
                PDS = pabG()
                for h in range(4):
                    hs = slice(h * 128, (h + 1) * 128)
                    mm(PDS[:, hs], kg[:, h, :], vnew[:, h, :], reads=[kg, vnew], writes=[PDS])
                yield
                for h in range(4):
                    hs = slice(h * 128, (h + 1) * 128)
                    stt(S32[:, h, :], S32[:, h, :], egl[:, h:h + 1], PDS[:, hs], ALU.mult, ALU.add, reads=[S32, egl, PDS], writes=[S32])
                yield
                cp("pool", S16[:], S32[:], reads=[S32], writes=[S16])
                yield
                act(junkA[:, 0:512], PS_[:], AF.Square, reads=[PS_], writes=[junkA])
                yield
                S.op("dve", lambda e: e.tensor_reduce(osq4[:], junkA[:, 0:512].rearrange("p (a b) -> p a b", a=4), axis=AX.X, op=ALU.add),
                     reads=[junkA], writes=[osq4])
                yield
                rsqrt_col(ors4, osq4, 4, 1.0 / 128, EPS)
                yield
                for h in range(4):
                    hs = slice(h * 128, (h + 1) * 128)
                    stt(og[:, h, :], PS_[:, hs], ors4[:, h:h + 1], G1[:, hs], ALU.mult, ALU.mult, reads=[PS_, ors4, G1], writes=[og])
                yield
                if b == 0 and i <= 1:
                    dump(f"og{i}", og[:].rearrange("p a b -> p (a b)"), og)
                yield
                P = pabG()
                yield
                P16 = P[:].bitcast(BF16)
                yield
                for h in range(4):
                    tr(P16[:, h * 128:(h + 1) * 128], og[:, h, :], ident16[:], reads=[og, ident16], writes=[P])
                yield
                cp("act", ogT[:], P16[:, 0:512].rearrange("p (a b) -> p a b", a=4), reads=[P], writes=[ogT])

                yield
            def dsa_stream():
                n = t0 + 128
                yield
                for s0 in range(0, n, 512):
                    w = min(512, n - s0)
                    for h in range(8):
                        P = pab()
                        pr = slice((h % 2) * 64, (h % 2) * 64 + 64)
                        mm(P[:, 0:w], iqT[pr, h // 2, :], ikT[pr, s0:s0 + w], reads=[iqT, ikTb], writes=[P])
                        if h == 0:
                            ts("dve", scoreF[:, s0:s0 + w], P[:, 0:w], 0.0, iw[:, 0:1], ALU.max, ALU.mult, reads=[P, iw], writes=[score])
                        else:
                            rt = rtmp if h % 2 else rtmp2
                            act(rt[:, 0:w], P[:, 0:w], AF.Relu, reads=[P], writes=[rt])
                            stt(scoreF[:, s0:s0 + w], rt[:, 0:w], iw[:, h:h + 1], scoreF[:, s0:s0 + w], ALU.mult, ALU.add,
                                reads=[rt, iw, score], writes=[score])
                        yield
                yield
                tt("dve", scoreF[:, t0:t0 + 128], scoreF[:, t0:t0 + 128], NEGC[:], ALU.add, reads=[score, NEGC], writes=[score])
                yield
                if b == 0 and i == 2:
                    dump("score2", scoreF[:, 0:384], score)
                yield
                yield "B"
                P = pab()
                for h in range(4):
                    for kc in range(8):
                        mm(P[:, h * 128:(h + 1) * 128], w16[:, kc, O_QB + h * 128:O_QB + (h + 1) * 128], hT16[:, kc, :],
                           start=(kc == 0), stop=(kc == 7), reads=[w16, hT16], writes=[P])
                cp("act", qbT[:], P[:].rearrange("p (a b) -> p a b", a=4), reads=[P], writes=[qbT])
                yield
                P = pab()
                for h in range(4):
                    for kc in range(8):
                        mm(P[:, h * 128:(h + 1) * 128], w16[:, kc, O_ZB + h * 128:O_ZB + (h + 1) * 128], hT16[:, kc, :],
                           start=(kc == 0), stop=(kc == 7), reads=[w16, hT16], writes=[P])
                act(szbT[:], P[:].rearrange("p (a b) -> p a b", a=4), AF.Silu, reads=[P], writes=[szbT])
                yield
                if i >= 2:
                    S.op("dve", lambda e: e.tensor_reduce(lo[:], scoreF[:, 0:t0], axis=AX.X, op=ALU.min), reads=[score], writes=[lo])
                    S.op("dve", lambda e: e.tensor_reduce(hw0[:], scoreF[:, 0:n], axis=AX.X, op=ALU.max), reads=[score], writes=[hw0])
                    tt("dve", hw0[:], hw0[:], lo[:], ALU.subtract, reads=[hw0, lo], writes=[hw0])
                    ts("dve", hw0[:], hw0[:], 1.0001, 1e-6, ALU.mult, ALU.add, reads=[hw0], writes=[hw0])
                    ts("dve", hwk[:], pow2[:], hw0[:, 0:1], None, ALU.mult, reads=[pow2, hw0], writes=[hwk])
                    ts("dve", nhwk[:], hwk[:], -1.0, None, ALU.mult, reads=[hwk], writes=[nhwk])
                    ts("dve", mid[:], lo[:], hwk[:, 0:1], -1.0, ALU.add, ALU.mult, reads=[lo, hwk], writes=[mid])
                    S.op("pool", lambda e: e.memset(cbc[:], float(n) - 511.5), writes=[cbc])
                    nmc, nmn = mid, mid2
                    for k in range(NBIS):
                        act(mk[:, 0:n], scoreF[:, 0:n], AF.Sign, reads=[score, nmc], writes=[junkD, cntc], bias=nmc[:, 0:1],
                            accum_out=cntc[:, 0:1])
                        act(sgnc[:], cntc[:], AF.Sign, reads=[cntc, cbc], writes=[sgnc], bias=cbc[:, 0:1])
                        if k < NBIS - 1:
                            act(nmn[:], sgnc[:], AF.Identity, reads=[sgnc, nhwk, nmc], writes=[nmn], scale=nhwk[:, k + 1:k + 2], bias=nmc[:, 0:1])
                            nmc, nmn = nmn, nmc
                        yield
                    act(nmn[:], nmc[:], AF.Identity, reads=[nmc, nhwk], writes=[nmn], scale=-1.0, bias=nhwk[:, NBIS:NBIS + 1])
                    act(lo[:], sgnc[:], AF.Identity, reads=[sgnc, hwk, nmn], writes=[lo], scale=hwk[:, NBIS:NBIS + 1], bias=nmn[:, 0:1])
                else:
                    S.op("dve", lambda e: e.memset(lo[:], -1e29), writes=[lo])
                yield
                ts("dve", mk[:, 0:n], scoreF[:, 0:n], lo[:, 0:1], None, ALU.is_ge, reads=[score, lo], writes=[mk])
                yield
                if b == 0 and i == 2:
                    dump("thr2", lo[:], lo)
                yield
                for j0 in range(0, i + 1, 4):
                    nj = min(4, i + 1 - j0)
                    P = pab()
                    P16 = P[:].bitcast(BF16)
                    for jj in range(nj):
                        j = j0 + jj
                        tr(P16[:, jj * 128:(jj + 1) * 128], mk[:, j * 128:(j + 1) * 128], ident16[:], reads=[mk, ident16], writes=[P])
                    ts("dve", negmk[:, j0:j0 + nj, :], P16[:, 0:nj * 128].rearrange("p (a b) -> p a b", a=nj), 30000.0, -30000.0, ALU.mult, ALU.add,
                       reads=[P], writes=[negmk])
                yield
                qb2 = qbT[:].rearrange("p a b -> p (a b)")
                yield
                for j in range(i + 1):
                    P = pab()
                    P3v = P[:].rearrange("p (a b) -> p a b", a=4)
                    near = j >= i - 1
                    mm(P3v, ckvnT[:, j * 128:(j + 1) * 128], qbT[:], start=True, stop=False, reads=[ckvnT, qbT], writes=[P])
                    mm(P3v, ident16[:], negmk[:, j, :].unsqueeze(1).to_broadcast([128, 4, 128]), start=False, stop=not near,
                       reads=[ident16, negmk], writes=[P])
                    if near:
                        mm(P3v, ident16[:], EB[i - j][:], start=False, stop=True, reads=[ident16, EB[i - j]], writes=[P])
                    E = Eb[j % 2]
                    act(E[:], P3v, AF.Exp, reads=[P], writes=[E], scale=128.0 ** -0.5)
                    pm2 = E[:].rearrange("p a b -> p (a b)")
                    mm(PO[:], ckvn[:, j, :], pm2, start=(j == 0), stop=(j == i), reads=[ckvn, E], writes=[PO])
                    mm(PO2[:], ones16[:], pm2, start=(j == 0), stop=(j == i), reads=[ones16, E], writes=[PO2])
                    yield
                yield
                cp("act", obT[:], PO[:].rearrange("p (a b) -> p a b", a=4), reads=[PO], writes=[obT])
                yield
                act(rden[:], PO2[:], AF.Ln, reads=[PO2], writes=[rden])
                yield
                act(rden[:], rden[:], AF.Exp, reads=[rden], writes=[rden], scale=-1.0)
                yield
                tt("pool", rden[:], rden[:], szbT[:].rearrange("p a b -> p (a b)"), ALU.mult, reads=[rden, szbT], writes=[rden])
                yield
                PY = pab()
                for h in range(4):
                    hs = slice(h * 128, (h + 1) * 128)
                    mm(PY[:, hs], wuv16[:, h, :], obT[:, h, :], reads=[wuv16, obT], writes=[PY])
                yield
                tt("dve", ygT[:].rearrange("p a b -> p (a b)"), PY[:], Rg[:], ALU.mult, reads=[PY, Rg], writes=[ygT])
                yield
                if b == 0 and i <= 2:
                    dump(f"yg{i}", ygT[:].rearrange("p a b -> p (a b)"), ygT)
                yield
                yield
            streams = [[gdn_stream(), 3], [dsa_stream(), 1]]
            pre = pre_gen(b, i + 1) if i + 1 < NT else None
            seen = set()
            while streams or pre is not None:
                for ent in list(streams):
                    for _ in range(ent[1]):
                        try:
                            seen.add(next(ent[0]))
                        except StopIteration:
                            streams.remove(ent)
                            break
                if pre is not None and (("M" in seen and "B" in seen) or not streams):
                    for _ in range(1):
                        try:
                            next(pre)
                        except StopIteration:
                            pre = None
                            break
            for nh in range(2):
                for c in range(8):
                    lhs = ogT[:, c, :] if c < 4 else ygT[:, c - 4, :]
                    mm(PT2h[nh][:], lhs, wout16[:, c, nh * 512:(nh + 1) * 512],
                       start=(c == 0), stop=(c == 7), reads=[ogT, ygT, wout16], writes=[PT2h[nh]])
            for nh in range(2):
                act(junkA[:, nh * 512:(nh + 1) * 512], PT2h[nh][:], AF.Square, reads=[PT2h[nh]],
                    writes=[junkA, msq], accum_out=msq[:, nh:nh + 1])
            tt("dve", ssq1[:], msq[:, 0:1], msq[:, 1:2], ALU.add, reads=[msq], writes=[ssq1])
            rsqrt_col(mrs, ssq1, 1, 1.0 / D, EPS)
            for nh in range(2):
                sl = slice(nh * 512, (nh + 1) * 512)
                stt(otmp[:, sl], PT2h[nh][:], mrs[:, 0:1], GATE[:, sl], ALU.mult, ALU.mult, reads=[PT2h[nh], mrs, GATE], writes=[otmp])
            xr = negmk[:].rearrange("p a b -> p (a b)").bitcast(F32)
            S.dma(xr, x_d[b, t0:t0 + 128, :], writes=[negmk])
            tt("pool", otmp[:], otmp[:], xr, ALU.add, reads=[otmp, negmk], writes=[otmp])
            S.dma(out_d[b, t0:t0 + 128, :], otmp[:], reads=[otmp])
            tix += 1
    S.finish("sp")
    return nc, S


def _masks():
    i = np.arange(128)[:, None]; j = np.arange(128)[None, :]
    m = np.zeros((128, 8, 128), np.float32)
    for ls in range(7):
        sz = 1 << ls
        m[:, ls, :] = (((i // sz) % 2 == 1) & ((j // sz) == (i // sz) - 1)).astype(np.float32)
    m[:, 7, :] = m[:, 0, :].T
    return m


def _layout(inputs, core):
    f = lambda a: np.ascontiguousarray(a, dtype=np.float32)
    bs = slice(core * NBC, (core + 1) * NBC)
    kp = lambda w: w.reshape(8, 128, -1).transpose(1, 0, 2)
    w_in = inputs["w_in"][0]
    ik = w_in[:, O_IK:O_IK + 64]
    w_idx = np.concatenate([w_in[:, O_IQ:O_IQ + 512], ik, ik, w_in[:, O_IW:O_IW + 8]], axis=1)
    b_ada = inputs["b_ada"][0]
    return {
        "x": f(inputs["x"][bs]),
        "cT": f(inputs["c"][bs].T.reshape(8, 128, NBC).transpose(1, 0, 2)),
        "w_ada": f(kp(inputs["w_ada"][0])),
        "b_col": f(b_ada.reshape(24, 128).T),
        "b_gate": f(b_ada[2048:3072].reshape(1, 1024)),
        "g_pre": f(inputs["g_pre"][0].reshape(8, 128).T),
        "w_in": f(kp(w_in[:, :NW16])),
        "w_idx": f(kp(w_idx)),
        "conv_w": f(inputs["conv_w"][0].reshape(4, 12, 128).transpose(2, 1, 0)),
        "a_log": f(inputs["a_log"].reshape(1, 4)),
        "dt_bias": f(inputs["dt_bias"].reshape(1, 4)),
        "g_gdn": f(inputs["g_gdn"].reshape(1, 128)),
        "g_kv": f(inputs["g_kv"].reshape(1, 128)),
        "w_uv": f(inputs["w_uv"][0].transpose(1, 0, 2)),
        "rel_bias": f(inputs["rel_bias"].reshape(1, 128)),
        "w_out": f(kp(inputs["w_out"][0])),
        "g_post": f(inputs["g_post"].reshape(1, 1024)),
        "masks": _masks(),
    }


def kernel(**inputs):
    inputs = {k: np.asarray(v) for k, v in inputs.items()}
    nc, _ = build()
    in_maps = [_layout(inputs, c) for c in range(8)]
    res = run_bass_kernel_spmd(nc, in_maps, core_ids=list(range(8)))
    return np.concatenate([r["out"] for r in res.results], axis=0).astype(np.float32)
```

```python
import math
import numpy as np
import concourse.bass as bass
import concourse.mybir as mybir
from concourse.bass_utils import run_bass_kernel_spmd

F32 = mybir.dt.float32
BF16 = mybir.dt.bfloat16
AF = mybir.ActivationFunctionType
ALU = mybir.AluOpType
AX = mybir.AxisListType

D = 1024
L = 2048
NBC = 4
NT = 16
EPS = 1e-6
NEG = -30000.0
NBIS = 13


class Res:
    __slots__ = ("name", "w", "r")

    def __init__(self, name):
        self.name = name
        self.w = None
        self.r = {}


class Buf:
    def __init__(self, t, name):
        self.t = t
        self.r = Res(name)

    def __getitem__(self, k):
        return self.t[k]


class Sched:
    def __init__(self, nc, ndma=8):
        self.nc = nc
        self.e = {"pe": nc.tensor, "act": nc.scalar, "dve": nc.vector, "pool": nc.gpsimd, "sp": nc.sync}
        self.sem = {k: nc.alloc_semaphore("sem_" + k) for k in self.e}
        self.cnt = {k: 0 for k in self.e}
        self.seen = {k: {} for k in self.e}
        self.dsem = [nc.alloc_semaphore(f"dsem{i}") for i in range(ndma)]
        self.dcnt = [0] * ndma
        self.dnext = 0
        self.nwait = 0

    def _wait(self, eng, key, val):
        if self.seen[eng].get(key, 0) >= val:
            return
        self.seen[eng][key] = val
        sem = self.sem[key] if isinstance(key, str) else self.dsem[key[1]]
        self.e[eng].wait_ge(sem, val)
        self.nwait += 1

    def _deps(self, eng, reads, writes):
        need = {}
        for b in reads:
            r = b.r
            if r.w is not None:
                k, v = r.w
                need[k] = max(need.get(k, 0), v)
        for b in writes:
            w = b.r
            if w.w is not None:
                k, v = w.w
                need[k] = max(need.get(k, 0), v)
            for k, v in w.r.items():
                need[k] = max(need.get(k, 0), v)
        for k, v in need.items():
            if eng == "pe" and k == "pe":
                continue
            self._wait(eng, k, v)

    def op(self, eng, fn, reads=(), writes=()):
        self._deps(eng, reads, writes)
        ins = fn(self.e[eng])
        self.cnt[eng] += 1
        ins.then_inc(self.sem[eng], 1)
        v = self.cnt[eng]
        for b in reads:
            b.r.r[eng] = v
        for b in writes:
            b.r.w = (eng, v)
            b.r.r = {}
        return ins

    def dma(self, out, in_, reads=(), writes=(), q="sp", **kw):
        slot = self.dnext
        self.dnext = (self.dnext + 1) % len(self.dsem)
        key = ("d", slot)
        if self.dcnt[slot] > 0:
            self._wait(q, key, self.dcnt[slot])
        self._deps(q, reads, writes)
        self.dcnt[slot] += 16
        self.e[q].dma_start(out=out, in_=in_, **kw).then_inc(self.dsem[slot], 16)
        v = self.dcnt[slot]
        for b in reads:
            b.r.r[key] = v
        for b in writes:
            b.r.w = (key, v)
            b.r.r = {}

    def finish(self, eng="sp"):
        for k in self.cnt:
            if self.cnt[k] > 0 and k != eng:
                self._wait(eng, k, self.cnt[k])
        for i, c in enumerate(self.dcnt):
            if c > 0:
                self._wait(eng, ("d", i), c)


def t5_bucket_np(n):
    n = np.maximum(n, 0)
    nf = np.maximum(n, 1).astype(np.float32)
    large = 16 + (np.log(nf / np.float32(16)) / np.float32(math.log(128 / 16)) * np.float32(16)).astype(np.int32)
    large = np.minimum(large, 31)
    return np.where(n < 16, n, large)


O_QKV, O_ZA, O_B, O_A, O_QB, O_CKV, O_ZB, O_IQ, O_IK, O_IW = 0, 1536, 2048, 2052, 2056, 2568, 2696, 3208, 3720, 3784
NW16 = 3208
NIDX = 648


def build(debug=None):
    nc = bass.Bass("TRN2", target_bir_lowering=False)
    din = lambda n, sh: nc.dram_tensor(n, sh, F32, kind="ExternalInput").ap()
    x_d = din("x", [NBC, L, D])
    cT_d = din("cT", [128, 8, NBC])
    wada_d = din("w_ada", [128, 8, 3072])
    bcol_d = din("b_col", [128, 24])
    bgate_d = din("b_gate", [1, 1024])
    gpre_d = din("g_pre", [128, 8])
    win_d = din("w_in", [128, 8, NW16])
    widx_d = din("w_idx", [128, 8, NIDX])
    conv_d = din("conv_w", [128, 12, 4])
    alog_d = din("a_log", [1, 4])
    dtb_d = din("dt_bias", [1, 4])
    ggdn_d = din("g_gdn", [1, 128])
    gkv_d = din("g_kv", [1, 128])
    wuv_d = din("w_uv", [128, 4, 128])
    rb_d = din("rel_bias", [1, 128])
    wout_d = din("w_out", [128, 8, 1024])
    gpost_d = din("g_post", [1, 1024])
    msk_d = din("masks", [128, 8, 128])
    out_d = nc.dram_tensor("out", [NBC, L, D], F32, kind="ExternalOutput").ap()
    dbg_d = {}
    if debug:
        for n, sh in debug.items():
            dbg_d[n] = nc.dram_tensor("dbg_" + n, list(sh), F32, kind="ExternalOutput").ap()

    S = Sched(nc)
    cnt = [0]

    def sb(shape, dt=F32, name=None):
        cnt[0] += 1
        name = "s_" + (name or f"t{cnt[0]}")
        return Buf(nc.alloc_sbuf_tensor(name, list(shape), dt), name)

    def ps(shape, dt=F32, name=None):
        cnt[0] += 1
        name = "p_" + (name or f"p{cnt[0]}")
        return Buf(nc.alloc_psum_tensor(name, list(shape), dt), name)

    def mm(out, lhsT, rhs, start=True, stop=True, reads=(), writes=()):
        S.op("pe", lambda e: e.matmul(out, lhsT, rhs, start=start, stop=stop), reads, writes)

    def tr(out, in_, ident, reads=(), writes=()):
        S.op("pe", lambda e: e.transpose(out, in_, ident), reads, writes)

    def act(out, in_, func, reads=(), writes=(), **kw):
        S.op("act", lambda e: e.activation(out, in_, func, **kw), reads, writes)

    def tt(eng, out, in0, in1, op, reads=(), writes=()):
        S.op(eng, lambda e: e.tensor_tensor(out, in0, in1, op=op), reads, writes)

    def ts(eng, out, in0, s1, s2, op0, op1=None, reads=(), writes=(), accum_out=None):
        if op1 is None:
            S.op(eng, lambda e: e.tensor_scalar(out, in0, s1, None, op0=op0), reads, writes)
        elif accum_out is not None:
            S.op(eng, lambda e: e.tensor_scalar(out, in0, s1, s2, op0=op0, op1=op1, accum_out=accum_out), reads, writes)
        else:
            S.op(eng, lambda e: e.tensor_scalar(out, in0, s1, s2, op0=op0, op1=op1), reads, writes)

    def stt(out, in0, scalar, in1, op0, op1, reads=(), writes=()):
        S.op("dve", lambda e: e.scalar_tensor_tensor(out, in0, scalar, in1, op0=op0, op1=op1), reads, writes)

    def cp(eng, out, in_, reads=(), writes=()):
        if eng == "act":
            S.op("act", lambda e: e.copy(out, in_), reads, writes)
        else:
            S.op(eng, lambda e: e.tensor_copy(out, in_), reads, writes)

    def dump(name, ap, buf):
        if name in dbg_d:
            S.dma(dbg_d[name], ap, reads=[buf], q="pool")

    big4 = sb([128, 1024], name="big4")
    rtmp2 = sb([128, 512], name="rtmp2")

    def view(buf, ap):
        v = Buf.__new__(Buf)
        v.t = ap
        v.r = buf.r
        return v
    io = view(big4, big4[:, 0:128])
    S.op("pool", lambda e: e.iota(io[:], [[1, 128]], base=0, channel_multiplier=-1,
                                  allow_small_or_imprecise_dtypes=True), writes=[io])
    ident = sb([128, 128], name="ident")
    ident16 = sb([128, 128], BF16, name="ident16")
    Umat = sb([128, 128], name="Umat")
    SLmat = sb([128, 128], name="SLmat")
    NEGs = sb([128, 128], name="NEGs")
    NEGsT = sb([128, 128], name="NEGsT")
    NEGC = sb([128, 128], name="NEGC")
    ones32 = sb([128, 128], name="ones32")
    ones16 = sb([128, 128], BF16, name="ones16")
    mhalf = sb([128, 8], name="mhalf")
    ts("dve", ident[:], io[:], 0.0, None, ALU.is_equal, reads=[io], writes=[ident])
    ts("dve", ident16[:], io[:], 0.0, None, ALU.is_equal, reads=[io], writes=[ident16])
    ts("dve", Umat[:], io[:], 0.0, None, ALU.is_ge, reads=[io], writes=[Umat])
    ts("dve", SLmat[:], io[:], 0.0, None, ALU.is_lt, reads=[io], writes=[SLmat])
    ts("dve", NEGs[:], io[:], 0.0, NEG, ALU.is_ge, ALU.mult, reads=[io], writes=[NEGs])
    ts("dve", NEGsT[:], io[:], 0.0, NEG, ALU.is_le, ALU.mult, reads=[io], writes=[NEGsT])
    ts("dve", NEGC[:], io[:], 0.0, -1e30, ALU.is_gt, ALU.mult, reads=[io], writes=[NEGC])
    S.op("pool", lambda e: e.memset(ones32[:], 1.0), writes=[ones32])
    S.op("pool", lambda e: e.memset(ones16[:], 1.0), writes=[ones16])
    S.op("pool", lambda e: e.memset(mhalf[:], -0.5), writes=[mhalf])
    pow2 = sb([128, NBIS + 1], name="pow2")
    for k in range(NBIS + 1):
        S.op("pool", lambda e, k=k: e.memset(pow2[:, k:k + 1], 0.5 ** (k + 1)), writes=[pow2])

    PT2a = ps([128, 512], name="PT2a")
    PT2b = ps([128, 512], name="PT2b")
    PT2h = [PT2a, PT2b]
    rotg = [0]

    def pabG():
        rotg[0] ^= 1
        return PT2a if rotg[0] else PT2b
    PA = ps([128, 512], name="PA")
    PB = ps([128, 512], name="PB")
    PC = ps([128, 512], name="PC")
    PO = ps([128, 512], name="PO")
    PO2 = ps([128, 512], name="PO2")
    PS_ = ps([128, 512], name="PS_")
    rot = [0]

    def pab():
        rot[0] ^= 1
        return PA if rot[0] else PB

    stage = [sb([128, 8, 256], name="stage0"), sb([128, 8, 256], name="stage1")]
    w16 = sb([128, 8, NW16], BF16, name="w16")
    widx = sb([128, 8, NIDX], name="widx")
    wout16 = sb([128, 8, 1024], BF16, name="wout16")
    wuv16 = sb([128, 4, 128], BF16, name="wuv16")
    S.dma(widx[:], widx_d, writes=[widx])
    si = 0
    for c0 in range(0, NW16, 256):
        w = min(256, NW16 - c0)
        st = stage[si % 2]; si += 1
        S.dma(st[:, :, 0:w], win_d[:, :, c0:c0 + w], writes=[st])
        cp("dve" if si % 2 else "pool", w16[:, :, c0:c0 + w], st[:, :, 0:w], reads=[st], writes=[w16])
    for c0 in range(0, 1024, 256):
        st = stage[si % 2]; si += 1
        S.dma(st[:], wout_d[:, :, c0:c0 + 256], writes=[st])
        cp("dve" if si % 2 else "pool", wout16[:, :, c0:c0 + 256], st[:], reads=[st], writes=[wout16])
    st = stage[si % 2]; si += 1
    S.dma(st[:, 0:4, 0:128], wuv_d, writes=[st])
    cp("dve", wuv16[:], st[:, 0:4, 0:128], reads=[st], writes=[wuv16])

    st = stage[si % 2]; si += 1
    S.dma(st[:, :, 0:128], msk_d, writes=[st])
    msk = sb([128, 8, 128], BF16, name="msk")
    cp("dve", msk[:], st[:, :, 0:128], reads=[st], writes=[msk])
    convw = sb([128, 12, 4], name="convw")
    S.dma(convw[:], conv_d, writes=[convw])
    diagw = sb([128, 12, 4, 128], BF16, name="diagw")
    for ch in range(12):
        for j in range(4):
            ts("dve" if (ch + j) % 2 else "pool", diagw[:, ch, j, :], ident[:], convw[:, ch, j:j + 1], None, ALU.mult,
               reads=[ident, convw], writes=[diagw])

    def bcast_row(src, n, name):
        t = sb([128, n], name=name)
        S.dma(t[:], src.partition_broadcast(128), writes=[t])
        return t
    alogB = bcast_row(alog_d, 4, "alogB")
    dtbB = bcast_row(dtb_d, 4, "dtbB")
    ggdnB = bcast_row(ggdn_d, 128, "ggdnB")
    gkvB = bcast_row(gkv_d, 128, "gkvB")
    rbB = bcast_row(rb_d, 128, "rbB")
    negA = sb([128, 4], name="negA")
    act(negA[:], alogB[:], AF.Exp, reads=[alogB], writes=[negA])
    ts("dve", negA[:], negA[:], -1.0, None, ALU.mult, reads=[negA], writes=[negA])
    ggdnH = ggdnB

    cT = sb([128, 8, NBC], name="cT")
    S.dma(cT[:], cT_d, writes=[cT])
    sc = sb([128, 8, NBC], name="sc")
    act(sc[:], cT[:], AF.Silu, reads=[cT], writes=[sc])
    bcol = sb([128, 24], name="bcol")
    S.dma(bcol[:], bcol_d, writes=[bcol])
    gpre = sb([128, 8], name="gpre")
    S.dma(gpre[:], gpre_d, writes=[gpre])
    shiftc = sb([128, 8, NBC], name="shiftc")
    Gc = sb([128, 8, NBC], name="Gc")
    GATE = sb([128, 1024], name="GATE")
    for n8 in range(8):
        st = stage[si % 2]; si += 1
        S.dma(st[:], wada_d[:, :, n8 * 256:(n8 + 1) * 256], writes=[st])
        for q2 in range(2):
            dch = (n8 % 4) * 2 + q2
            for kc in range(8):
                mm(PC[:, q2 * 4:q2 * 4 + 4], st[:, kc, q2 * 128:(q2 + 1) * 128], sc[:, kc, :],
                   start=(kc == 0), stop=(kc == 7), reads=[st, sc], writes=[PC])
            dst = shiftc if n8 < 4 else Gc
            ts("dve", dst[:, dch, :], PC[:, q2 * 4:q2 * 4 + 4], bcol[:, n8 * 2 + q2:n8 * 2 + q2 + 1], None, ALU.add,
               reads=[PC, bcol], writes=[dst])
    ts("dve", Gc[:], Gc[:], 1.0, None, ALU.add, reads=[Gc], writes=[Gc])
    tt("dve", Gc[:], Gc[:], gpre[:].unsqueeze(2).to_broadcast([128, 8, NBC]), ALU.mult, reads=[Gc, gpre], writes=[Gc])

    bk = t5_bucket_np(np.arange(256))
    lo_b = [int(np.argmax(bk >= b)) if (bk >= b).any() else 100000 for b in range(32)]
    rb3 = rbB[:].rearrange("p (b h) -> p b h", h=4)
    dlt = sb([128, 32, 4], name="dlt")
    cp("dve", dlt[:, 0:1, :], rb3[:, 0:1, :], reads=[rbB], writes=[dlt])
    tt("dve", dlt[:, 1:32, :], rb3[:, 1:32, :], rb3[:, 0:31, :], ALU.subtract, reads=[rbB], writes=[dlt])
    EB = [sb([128, 4, 128], BF16, name=f"EB{t}") for t in range(2)]
    EBf = view(rtmp2, rtmp2[:, 0:128])
    rtmp = sb([128, 512], name="rtmp")
    gUb = [sb([128, 128], name=f"gU{i}") for i in range(2)]
    distT = gUb[0]
    tmpb = gUb[1]
    for typ in range(2):
        ts("dve", distT[:], io[:], float(128 * typ), None, ALU.add, reads=[io], writes=[distT])
        for h in range(4):
            acc = EBf
            ts("dve", acc[:], ones32[:], dlt[:, 0, h:h + 1], rb3[:, 31, h:h + 1], ALU.mult, ALU.subtract,
               reads=[ones32, dlt, rbB], writes=[acc])
            for bb in range(1, 32):
                if lo_b[bb] > 255:
                    continue
                ts("dve", tmpb[:], distT[:], float(lo_b[bb]) - 0.5, dlt[:, bb, h:h + 1], ALU.is_ge, ALU.mult,
                   reads=[distT, dlt], writes=[tmpb])
                tt("dve", acc[:], acc[:], tmpb[:], ALU.add, reads=[acc, tmpb], writes=[acc])
            ts("dve", acc[:], acc[:], 128.0 ** 0.5, None, ALU.mult, reads=[acc], writes=[acc])
            cp("dve", EB[typ][:, h, :], acc[:], reads=[acc], writes=[EB[typ]])

    xt = sb([128, 1024], name="xt")
    junkA = big4; xs = big4; otmp = big4
    hT32 = sb([128, 8, 128], name="hT32")
    hT16 = sb([128, 8, 128], BF16, name="hT16")
    uT = sb([128, 12, 131], BF16, name="uT")
    qkvs = sb([128, 1536], BF16, name="qkvs")
    col = lambda n, name: sb([128, n], name=name)
    ssq1 = col(1, "ssq1"); rstd1 = col(1, "rstd1"); ssqP2 = col(2, "ssqP2"); ssqP = col(1, "ssqP"); rstdP = col(1, "rstdP")
    xs2 = sb([128, 512], name="xs2")
    ssq8 = col(8, "ssq8"); rs8 = col(8, "rs8")
    ba = col(8, "ba"); beta = col(4, "beta"); gcol = col(4, "gcol"); gc = col(4, "gc"); glB = col(4, "glB")
    egc = col(4, "egc"); ekg = col(4, "ekg"); egl = col(4, "egl"); tmp4 = col(4, "tmp4"); nbeta = col(4, "nbeta")
    cf = {n: col(4, "cf_" + n) for n in ("kbg", "kg", "qg")}
    khat = sb([128, 4, 128], BF16, name="khat"); qhat = sb([128, 4, 128], BF16, name="qhat")
    qg = sb([128, 4, 128], BF16, name="qg"); kbg = sb([128, 4, 128], BF16, name="kbg")
    kg = sb([128, 4, 128], BF16, name="kg"); vb = sb([128, 4, 128], BF16, name="vb")
    khT = sb([128, 4, 128], BF16, name="khT"); qhT = sb([128, 4, 128], BF16, name="qhT"); qgT = sb([128, 4, 128], BF16, name="qgT")
    Es = sb([128, 4, 128], BF16, name="Es"); EsT = sb([128, 4, 128], BF16, name="EsT")
    Xb = [sb([128, 4, 128], BF16, name="X0")]
    XTb = [sb([128, 4, 128], BF16, name="XT0")]
    Tb = [sb([128, 4, 128], BF16, name=f"T{i}") for i in range(2)]
    TTb = [sb([128, 4, 128], BF16, name=f"TT{i}") for i in range(2)]
    Wp = khat
    attnT = sb([128, 4, 128], BF16, name="attnT")
    negwT = qhat
    vnew = qg
    S32 = sb([128, 4, 128], name="S32"); S16 = sb([128, 4, 128], BF16, name="S16")
    zas = sb([128, 512], BF16, name="zas"); G1 = zas
    osq4 = col(4, "osq4"); ors4 = col(4, "ors4")
    og = sb([128, 4, 128], BF16, name="og"); ogT = sb([128, 4, 128], BF16, name="ogT")
    qbT = sb([128, 4, 128], BF16, name="qbT"); szbT = sb([128, 4, 128], BF16, name="szbT")
    iqT = sb([128, 4, 128], name="iqT"); iw = col(8, "iw")
    ckvn = sb([128, NT, 128], BF16, name="ckvn"); ckvnT = sb([128, L], BF16, name="ckvnT")
    ikTb = stage[1]
    ikT = ikTb[:].rearrange("p a b -> p (a b)")
    score = stage[0]
    scoreF = score[:].rearrange("p a b -> p (a b)")
    mk = sb([128, L], BF16, name="mk")
    junkD = mk
    junkDF = mk
    lo = col(1, "lo"); hw0 = col(1, "hw0"); hwk = col(NBIS + 1, "hwk"); nhwk = col(NBIS + 1, "nhwk"); mid = col(1, "mid"); mid2 = col(1, "mid2"); sgnc = col(1, "sgnc"); cbc = col(1, "cbc"); cntc = col(1, "cntc"); tstep = col(1, "tstep")
    Eb = [sb([128, 4, 128], BF16, name=f"Eb{i}") for i in range(2)]
    negmk = sb([128, NT, 128], BF16, name="negmk"); mkT = negmk
    obT = sb([128, 4, 128], BF16, name="obT")
    rden = rtmp; Rg = rden
    ygT = sb([128, 4, 128], BF16, name="ygT")
    msq = col(2, "msq"); mrs = col(1, "mrs")

    def rsqrt_col(dst, src, n, scale, eps):
        ts("dve", dst[:, 0:n], src[:, 0:n], scale, eps, ALU.mult, ALU.add, reads=[src], writes=[dst])
        tt("pool", dst[:, 0:n], dst[:, 0:n], mhalf[:, 0:n], ALU.pow, reads=[dst, mhalf], writes=[dst])

    def pre_gen(b, i):
        t0 = i * 128
        uc = uT
        S.dma(xt[:], x_d[b, t0:t0 + 128, :], writes=[xt])
        yield
        for hf in range(2):
            act(xs2[:], xt[:, hf * 512:(hf + 1) * 512], AF.Square, reads=[xt], writes=[xs2, ssqP2], accum_out=ssqP2[:, hf:hf + 1])
        tt("pool", ssqP[:], ssqP2[:, 0:1], ssqP2[:, 1:2], ALU.add, reads=[ssqP2], writes=[ssqP])
        rsqrt_col(rstdP, ssqP, 1, 1.0 / D, EPS)
        yield
        for hf in range(2):
            ts("dve", xs2[:], xt[:, hf * 512:(hf + 1) * 512], rstdP[:, 0:1], None, ALU.mult, reads=[xt, rstdP], writes=[xs2])
            for c4 in range(4):
                tr(PC[:, c4 * 128:(c4 + 1) * 128], xs2[:, c4 * 128:(c4 + 1) * 128], ident[:], reads=[xs2, ident], writes=[PC])
            for c4 in range(4):
                c = hf * 4 + c4
                ts("dve", hT32[:, c, :], PC[:, c4 * 128:(c4 + 1) * 128], Gc[:, c, b:b + 1], shiftc[:, c, b:b + 1], ALU.mult, ALU.add,
                   reads=[PC, Gc, shiftc], writes=[hT32])
            yield
        cp("pool", hT16[:], hT32[:], reads=[hT32], writes=[hT16])
        if b == 0 and i == 0:
            dump("hT", hT32[:], hT32)
        yield
        for g3 in range(3):
            for q4 in range(4):
                ch = g3 * 4 + q4
                for kc in range(8):
                    mm(PC[:, q4 * 128:(q4 + 1) * 128], w16[:, kc, O_QKV + ch * 128:O_QKV + (ch + 1) * 128], hT16[:, kc, :],
                       start=(kc == 0), stop=(kc == 7), reads=[w16, hT16], writes=[PC])
            cp("dve", uc[:, g3 * 4:(g3 + 1) * 4, 3:131], PC[:].rearrange("p (a b) -> p a b", a=4), reads=[PC], writes=[uc])
            yield
        for g3 in range(3):
            for q4 in range(4):
                ch = g3 * 4 + q4
                for j in range(4):
                    mm(PC[:, q4 * 128:(q4 + 1) * 128], uc[:, ch, j:j + 128], diagw[:, ch, j, :],
                       start=(j == 0), stop=(j == 3), reads=[uc, diagw], writes=[PC])
            act(qkvs[:, g3 * 512:(g3 + 1) * 512], PC[:], AF.Silu, reads=[PC], writes=[qkvs])
            yield
        cp("pool", uc[:, :, 0:3], uc[:, :, 128:131], reads=[uc], writes=[uc])
        if b == 0 and i == 0:
            dump("qkvs", qkvs[:], qkvs)
        for hf in range(2):
            tt("dve", xs2[:], qkvs[:, hf * 512:(hf + 1) * 512], qkvs[:, hf * 512:(hf + 1) * 512], ALU.mult, reads=[qkvs], writes=[xs2])
            S.op("dve", lambda e, hf=hf: e.tensor_reduce(ssq8[:, hf * 4:(hf + 1) * 4], xs2[:].rearrange("p (a b) -> p a b", a=4), axis=AX.X, op=ALU.add),
                 reads=[xs2], writes=[ssq8])
        rsqrt_col(rs8, ssq8, 8, 1.0, EPS)
        ts("dve", rs8[:, 0:4], rs8[:, 0:4], 128.0 ** -0.5, None, ALU.mult, reads=[rs8], writes=[rs8])
        yield
        for c4 in range(4):
            for kc in range(8):
                mm(PC[:, c4 * 128:(c4 + 1) * 128], widx[:, kc, c4 * 128:(c4 + 1) * 128], hT32[:, kc, :],
                   start=(kc == 0), stop=(kc == 7), reads=[widx, hT32], writes=[PC])
            if c4 % 2:
                yield
        cp("dve", iqT[:], PC[:].rearrange("p (a b) -> p a b", a=4), reads=[PC], writes=[iqT])
        yield
        for kc in range(8):
            mm(PC[:, 0:128], widx[:, kc, 512:640], hT32[:, kc, :], start=(kc == 0), stop=(kc == 7), reads=[widx, hT32], writes=[PC])
        cp("dve", ikT[:, t0:t0 + 128], PC[:, 0:128], reads=[PC], writes=[ikTb])
        for kc in range(8):
            mm(PC[:, 0:8], hT32[:, kc, :], widx[:, kc, 640:648], start=(kc == 0), stop=(kc == 7), reads=[hT32, widx], writes=[PC])
        ts("dve", iw[:], PC[:, 0:8], (8.0 * 64.0) ** -0.5, None, ALU.mult, reads=[PC], writes=[iw])
        yield
        for kc in range(8):
            mm(PC[:, 16:24], hT16[:, kc, :], w16[:, kc, O_B:O_B + 8], start=(kc == 0), stop=(kc == 7), reads=[hT16, w16], writes=[PC])
        for kc in range(8):
            mm(PC[:, 128:256], hT16[:, kc, :], w16[:, kc, O_CKV:O_CKV + 128], start=(kc == 0), stop=(kc == 7), reads=[hT16, w16], writes=[PC])
        cp("dve", ba[:], PC[:, 16:24], reads=[PC], writes=[ba])
        act(xs2[:, 0:128], PC[:, 128:256], AF.Square, reads=[PC], writes=[xs2, ssqP], accum_out=ssqP[:, 0:1])
        rsqrt_col(rstdP, ssqP, 1, 1.0 / 128, EPS)
        stt(ckvn[:, i, :], PC[:, 128:256], rstdP[:, 0:1], gkvB[:], ALU.mult, ALU.mult, reads=[PC, rstdP, gkvB], writes=[ckvn])
        yield
        PC16 = PC[:].bitcast(BF16)
        tr(PC16[:, 512:640], ckvn[:, i, :], ident16[:], reads=[ckvn, ident16], writes=[PC])
        cp("dve", ckvnT[:, t0:t0 + 128], PC16[:, 512:640], reads=[PC], writes=[ckvnT])
        yield

    tix = 0
    screp = xt[:].rearrange("p (a b) -> p a b", a=8)
    for b in range(NBC):
        S.op("pool", lambda e: e.memset(S32[:], 0.0), writes=[S32])
        S.op("pool", lambda e: e.memset(S16[:], 0.0), writes=[S16])
        S.op("pool", lambda e: e.memset(uT[:, :, 0:3], 0.0), writes=[uT])
        for kc in range(8):
            cp("dve" if kc % 2 else "pool", screp[:, kc, :], sc[:, kc, b:b + 1].to_broadcast([128, 128]), reads=[sc], writes=[xt])
        for g4 in range(4):
            st = stage[g4 % 2]
            g0 = g4 * 256
            S.dma(st[:], wada_d[:, :, 2048 + g0:2048 + g0 + 256], writes=[st])
            S.dma(rtmp[:, 0:256], bgate_d[:, g0:g0 + 256].partition_broadcast(128), writes=[rtmp])
            S.dma(rtmp[:, 256:512], gpost_d[:, g0:g0 + 256].partition_broadcast(128), writes=[rtmp])
            P = pab()
            for kc in range(8):
                mm(P[:, 0:256], screp[:, kc, :], st[:, kc, :], start=(kc == 0), stop=(kc == 7), reads=[xt, st], writes=[P])
            tt("dve", GATE[:, g0:g0 + 256], P[:, 0:256], rtmp[:, 0:256], ALU.add, reads=[P, rtmp], writes=[GATE])
            tt("pool", GATE[:, g0:g0 + 256], GATE[:, g0:g0 + 256], rtmp[:, 256:512], ALU.mult, reads=[GATE, rtmp], writes=[GATE])
        for i in range(NT):
            t0 = i * 128
            uc = uT
            if i == 0:
                for _ in pre_gen(b, 0):
                    pass
            P = pab()
            for kc in range(8):
                mm(P[:], hT16[:, kc, :], w16[:, kc, O_ZA:O_ZA + 512], start=(kc == 0), stop=(kc == 7), reads=[hT16, w16], writes=[P])
            act(zas[:], P[:], AF.Silu, reads=[P], writes=[zas])
            tt("pool", zas[:].rearrange("p (h v) -> p h v", h=4), zas[:].rearrange("p (h v) -> p h v", h=4),
               ggdnH[:].unsqueeze(1).to_broadcast([128, 4, 128]), ALU.mult, reads=[zas, ggdnH], writes=[zas])
            def gdn_stream():
                act(beta[:], ba[:, 0:4], AF.Exp, reads=[ba], writes=[beta], scale=-1.0)
                yield
                ts("dve", beta[:], beta[:], 1.0, None, ALU.add, reads=[beta], writes=[beta])
                yield
                S.op("dve", lambda e: e.reciprocal(beta[:], beta[:]), reads=[beta], writes=[beta])
                yield
                ts("dve", nbeta[:], beta[:], -1.0, None, ALU.mult, reads=[beta], writes=[nbeta])
                yield
                tt("dve", tmp4[:], ba[:, 4:8], dtbB[:], ALU.add, reads=[ba, dtbB], writes=[tmp4])
                yield
                act(tmp4[:], tmp4[:], AF.Exp, reads=[tmp4], writes=[tmp4])
                yield
                act(tmp4[:], tmp4[:], AF.Ln, reads=[tmp4], writes=[tmp4], bias=1.0)
                yield
                tt("dve", gcol[:], tmp4[:], negA[:], ALU.mult, reads=[tmp4, negA], writes=[gcol])
                yield
                mm(PC[:, 16:20], Umat[:], gcol[:], reads=[Umat, gcol], writes=[PC])
                yield
                mm(PC[:, 20:24], ones32[:], gcol[:], reads=[ones32, gcol], writes=[PC])
                yield
                cp("dve", gc[:], PC[:, 16:20], reads=[PC], writes=[gc])
                yield
                cp("dve", glB[:], PC[:, 20:24], reads=[PC], writes=[glB])
                yield
                act(egc[:], gc[:], AF.Exp, reads=[gc], writes=[egc])
                yield
                act(egl[:], glB[:], AF.Exp, reads=[glB], writes=[egl])
                yield
                tt("dve", tmp4[:], glB[:], gc[:], ALU.subtract, reads=[glB, gc], writes=[tmp4])
                yield
                act(ekg[:], tmp4[:], AF.Exp, reads=[tmp4], writes=[ekg])
                yield
                tt("dve", cf["kbg"][:], rs8[:, 4:8], beta[:], ALU.mult, reads=[rs8, beta], writes=[cf["kbg"]])
                yield
                tt("dve", cf["kbg"][:], cf["kbg"][:], egc[:], ALU.mult, reads=[cf["kbg"], egc], writes=[cf["kbg"]])
                yield
                tt("dve", cf["kg"][:], rs8[:, 4:8], ekg[:], ALU.mult, reads=[rs8, ekg], writes=[cf["kg"]])
                yield
                tt("dve", cf["qg"][:], rs8[:, 0:4], egc[:], ALU.mult, reads=[rs8, egc], writes=[cf["qg"]])
                yield
                q3 = qkvs[:, 0:512].rearrange("p (h d) -> p h d", h=4)
                yield
                k3 = qkvs[:, 512:1024].rearrange("p (h d) -> p h d", h=4)
                yield
                v3 = qkvs[:, 1024:1536].rearrange("p (h d) -> p h d", h=4)
                yield
                bc = lambda c, lo_=0: c[:, lo_:lo_ + 4].unsqueeze(2).to_broadcast([128, 4, 128])
                yield
                tt("dve", khat[:], k3, bc(rs8, 4), ALU.mult, reads=[qkvs, rs8], writes=[khat])
                yield
                tt("pool", qhat[:], q3, bc(rs8, 0), ALU.mult, reads=[qkvs, rs8], writes=[qhat])
                yield
                tt("dve", qg[:], q3, bc(cf["qg"]), ALU.mult, reads=[qkvs, cf["qg"]], writes=[qg])
                yield
                tt("pool", kbg[:], k3, bc(cf["kbg"]), ALU.mult, reads=[qkvs, cf["kbg"]], writes=[kbg])
                yield
                tt("dve", kg[:], k3, bc(cf["kg"]), ALU.mult, reads=[qkvs, cf["kg"]], writes=[kg])
                yield
                tt("pool", vb[:], v3, bc(beta), ALU.mult, reads=[qkvs, beta], writes=[vb])
                yield "M"
                for src, dst in ((khat, khT), (qhat, qhT), (qg, qgT)):
                    P = pabG()
                    P16 = P[:].bitcast(BF16)
                    for h in range(4):
                        tr(P16[:, h * 128:(h + 1) * 128], src[:, h, :], ident16[:], reads=[src, ident16], writes=[P])
                    cp("act", dst[:], P16[:, 0:512].rearrange("p (a b) -> p a b", a=4), reads=[P], writes=[dst])
                yield
                PD = pabG(); PDT = pabG()
                yield
                for h in range(4):
                    gU = gUb[h % 2]
                    ts("dve" if h % 2 else "pool", gU[:], Umat[:], gcol[:, h:h + 1], None, ALU.mult, reads=[Umat, gcol], writes=[gU])
                    mm(PD[:, h * 128:(h + 1) * 128], gU[:], SLmat[:], start=True, stop=False, reads=[gU, SLmat], writes=[PD])
                    mm(PD[:, h * 128:(h + 1) * 128], ident[:], NEGs[:], start=False, stop=True, reads=[ident, NEGs], writes=[PD])
                    mm(PDT[:, h * 128:(h + 1) * 128], SLmat[:], gU[:], start=True, stop=False, reads=[gU, SLmat], writes=[PDT])
                    mm(PDT[:, h * 128:(h + 1) * 128], ident[:], NEGsT[:], start=False, stop=True, reads=[ident, NEGsT], writes=[PDT])
                yield
                act(Es[:], PD[:].rearrange("p (a b) -> p a b", a=4), AF.Exp, reads=[PD], writes=[Es])
                yield
                act(EsT[:], PDT[:].rearrange("p (a b) -> p a b", a=4), AF.Exp, reads=[PDT], writes=[EsT])
                yield
                if b == 0 and i == 0:
                    dump("Es", Es[:].rearrange("p a b -> p (a b)"), Es)
                    dump("beta4", beta[:], beta); dump("gc4", gc[:], gc); dump("rs8", rs8[:], rs8)
                yield
                P = pabG()
                yield
                for h in range(4):
                    mm(P[:, h * 128:(h + 1) * 128], khT[:, h, :], khT[:, h, :], reads=[khT], writes=[P])
                yield
                X, XT = Xb[0], XTb[0]
                yield
                for h in range(4):
                    stt(X[:, h, :], P[:, h * 128:(h + 1) * 128], nbeta[:, h:h + 1], Es[:, h, :], ALU.mult, ALU.mult,
                        reads=[P, nbeta, Es], writes=[X])
                yield
                if b == 0 and i == 0:
                    dump("X0", X[:].rearrange("p a b -> p (a b)"), X)
                yield
                P = pabG()
                yield
                for h in range(4):
                    mm(P[:, h * 128:(h + 1) * 128], khT[:, h, :], qhT[:, h, :], reads=[khT, qhT], writes=[P])
                yield
                tt("pool", EsT[:], EsT[:], ident16[:].unsqueeze(1).to_broadcast([128, 4, 128]), ALU.add, reads=[EsT, ident16], writes=[EsT])
                yield
                tt("dve", attnT[:], P[:].rearrange("p (a b) -> p a b", a=4), EsT[:], ALU.mult, reads=[P, EsT], writes=[attnT])
                yield
                P = pabG()
                yield
                P16 = P[:].bitcast(BF16)
                yield
                for h in range(4):
                    tr(P16[:, h * 128:(h + 1) * 128], X[:, h, :], ident16[:], reads=[X, ident16], writes=[P])
                yield
                cp("act", XT[:], P16[:, 0:512].rearrange("p (a b) -> p a b", a=4), reads=[P], writes=[XT])
                yield
                bcm = lambda ls: msk[:, ls, :].unsqueeze(1).to_broadcast([128, 4, 128])
                yield
                Tc, TT = Tb[0], TTb[0]
                yield
                tt("pool", Tc[:], X[:], bcm(0), ALU.mult, reads=[X, msk], writes=[Tc])
                yield
                tt("pool", Tc[:], Tc[:], ident16[:].unsqueeze(1).to_broadcast([128, 4, 128]), ALU.add, reads=[Tc, ident16], writes=[Tc])
                yield
                tt("dve", TT[:], XT[:], bcm(7), ALU.mult, reads=[XT, msk], writes=[TT])
                yield
                tt("dve", TT[:], TT[:], ident16[:].unsqueeze(1).to_broadcast([128, 4, 128]), ALU.add, reads=[TT, ident16], writes=[TT])
                yield
                gen = 0
                yield
                for ls in range(1, 7):
                    Tn, TTn = Tb[1 - gen], TTb[1 - gen]
                    P1 = pabG()
                    for h in range(4):
                        mm(P1[:, h * 128:(h + 1) * 128], XT[:, h, :], Tc[:, h, :], reads=[XT, Tc], writes=[P1])
                    tt("dve", Wp[:], P1[:].rearrange("p (a b) -> p a b", a=4), bcm(ls), ALU.mult, reads=[P1, msk], writes=[Wp])
                    if ls < 6:
                        P2 = pabG()
                        for h in range(4):
                            mm(P2[:, h * 128:(h + 1) * 128], TT[:, h, :], Wp[:, h, :], reads=[TT, Wp], writes=[P2])
                        tt("dve", Tn[:], P2[:].rearrange("p (a b) -> p a b", a=4), Tc[:], ALU.add, reads=[P2, Tc], writes=[Tn])
                    P3 = PS_
                    for h in range(4):
                        mm(P3[:, h * 128:(h + 1) * 128], Wp[:, h, :], TT[:, h, :], reads=[Wp, TT], writes=[P3])
                    tt("dve", TTn[:], P3[:].rearrange("p (a b) -> p a b", a=4), TT[:], ALU.add, reads=[P3, TT], writes=[TTn])
                    Tc, TT = Tn, TTn
                    gen = 1 - gen
                    yield
                yield
                if b == 0 and i == 0:
                    dump("TTf", TT[:].rearrange("p a b -> p (a b)"), TT)
                    dump("attnT0", attnT[:].rearrange("p a b -> p (a b)"), attnT)
                yield
                P = pabG()
                yield
                for h in range(4):
                    mm(P[:, h * 128:(h + 1) * 128], kbg[:, h, :], TT[:, h, :], reads=[kbg, TT], writes=[P])
                yield
                ts("dve", negwT[:], P[:].rearrange("p (a b) -> p a b", a=4), -1.0, None, ALU.mult, reads=[P], writes=[negwT])
                yield
                PV = pabG()
                for h in range(4):
                    hs = slice(h * 128, (h + 1) * 128)
                    mm(PV[:, hs], TT[:, h, :], vb[:, h, :], start=True, stop=False, reads=[TT, vb], writes=[PV])
                    mm(PV[:, hs], negwT[:, h, :], S16[:, h, :], start=False, stop=True, reads=[negwT, S16], writes=[PV])
                yield
                cp("act", vnew[:], PV[:].rearrange("p (a b) -> p a b", a=4), reads=[PV], writes=[vnew])
                yield
                if b == 0 and i == 0:
                    dump("vnew0", vnew[:].rearrange("p a b -> p (a b)"), vnew)
                yield
                for h in range(4):
                    hs = slice(h * 128, (h + 1) * 128)
                    mm(PS_[:, hs], qgT[:, h, :], S16[:, h, :], start=True, stop=False, reads=[qgT, S16], writes=[PS_])
                    mm(PS_[:, hs], attnT[:, h, :], vnew[:, h, :], start=False, stop=True, reads=[attnT, vnew], writes=[PS_])
                yield
                PDS = pabG()
                for h in range(4):
                    hs = slice(h * 128, (h + 1) * 128)
                    mm(PDS[:, hs], kg[:, h, :], vnew[:, h, :], reads=[kg, vnew], writes=[PDS])
                yield
                for h in range(4):
                    hs = slice(h * 128, (h + 1) * 128)
                    stt(S32[:, h, :], S32[:, h, :], egl[:, h:h + 1], PDS[:, hs], ALU.mult, ALU.add, reads=[S32, egl, PDS], writes=[S32])
                yield
                cp("pool", S16[:], S32[:], reads=[S32], writes=[S16])
                yield
                act(junkA[:, 0:512], PS_[:], AF.Square, reads=[PS_], writes=[junkA])
                yield
                S.op("dve", lambda e: e.tensor_reduce(osq4[:], junkA[:, 0:512].rearrange("p (a b) -> p a b", a=4), axis=AX.X, op=ALU.add),
                     reads=[junkA], writes=[osq4])
                yield
                rsqrt_col(ors4, osq4, 4, 1.0 / 128, EPS)
                yield
                for h in range(4):
                    hs = slice(h * 128, (h + 1) * 128)
                    stt(og[:, h, :], PS_[:, hs], ors4[:, h:h + 1], G1[:, hs], ALU.mult, ALU.mult, reads=[PS_, ors4, G1], writes=[og])
                yield
                if b == 0 and i <= 1:
                    dump(f"og{i}", og[:].rearrange("p a b -> p (a b)"), og)
                yield
                P = pabG()
                yield
                P16 = P[:].bitcast(BF16)
                yield
                for h in range(4):
                    tr(P16[:, h * 128:(h + 1) * 128], og[:, h, :], ident16[:], reads=[og, ident16], writes=[P])
                yield
                cp("act", ogT[:], P16[:, 0:512].rearrange("p (a b) -> p a b", a=4), reads=[P], writes=[ogT])

                yield
            def dsa_stream():
                n = t0 + 128
                yield
                for s0 in range(0, n, 512):
                    w = min(512, n - s0)
                    for h in range(8):
                        P = pab()
                        pr = slice((h % 2) * 64, (h % 2) * 64 + 64)
                        mm(P[:, 0:w], iqT[pr, h // 2, :], ikT[pr, s0:s0 + w], reads=[iqT, ikTb], writes=[P])
                        if h == 0:
                            ts("dve", scoreF[:, s0:s0 + w], P[:, 0:w], 0.0, iw[:, 0:1], ALU.max, ALU.mult, reads=[P, iw], writes=[score])
                        else:
                            rt = rtmp if h % 2 else rtmp2
                            act(rt[:, 0:w], P[:, 0:w], AF.Relu, reads=[P], writes=[rt])
                            stt(scoreF[:, s0:s0 + w], rt[:, 0:w], iw[:, h:h + 1], scoreF[:, s0:s0 + w], ALU.mult, ALU.add,
                                reads=[rt, iw, score], writes=[score])
                        yield
                yield
                tt("dve", scoreF[:, t0:t0 + 128], scoreF[:, t0:t0 + 128], NEGC[:], ALU.add, reads=[score, NEGC], writes=[score])
                yield
                if b == 0 and i == 2:
                    dump("score2", scoreF[:, 0:384], score)
                yield
                yield "B"
                P = pab()
                for h in range(4):
                    for kc in range(8):
                        mm(P[:, h * 128:(h + 1) * 128], w16[:, kc, O_QB + h * 128:O_QB + (h + 1) * 128], hT16[:, kc, :],
                           start=(kc == 0), stop=(kc == 7), reads=[w16, hT16], writes=[P])
                cp("act", qbT[:], P[:].rearrange("p (a b) -> p a b", a=4), reads=[P], writes=[qbT])
                yield
                P = pab()
                for h in range(4):
                    for kc in range(8):
                        mm(P[:, h * 128:(h + 1) * 128], w16[:, kc, O_ZB + h * 128:O_ZB + (h + 1) * 128], hT16[:, kc, :],
                           start=(kc == 0), stop=(kc == 7), reads=[w16, hT16], writes=[P])
                act(szbT[:], P[:].rearrange("p (a b) -> p a b", a=4), AF.Silu, reads=[P], writes=[szbT])
                yield
                if i >= 2:
                    S.op("dve", lambda e: e.tensor_reduce(lo[:], scoreF[:, 0:t0], axis=AX.X, op=ALU.min), reads=[score], writes=[lo])
                    S.op("dve", lambda e: e.tensor_reduce(hw0[:], scoreF[:, 0:n], axis=AX.X, op=ALU.max), reads=[score], writes=[hw0])
                    tt("dve", hw0[:], hw0[:], lo[:], ALU.subtract, reads=[hw0, lo], writes=[hw0])
                    ts("dve", hw0[:], hw0[:], 1.0001, 1e-6, ALU.mult, ALU.add, reads=[hw0], writes=[hw0])
                    ts("dve", hwk[:], pow2[:], hw0[:, 0:1], None, ALU.mult, reads=[pow2, hw0], writes=[hwk])
                    ts("dve", nhwk[:], hwk[:], -1.0, None, ALU.mult, reads=[hwk], writes=[nhwk])
                    ts("dve", mid[:], lo[:], hwk[:, 0:1], -1.0, ALU.add, ALU.mult, reads=[lo, hwk], writes=[mid])
                    S.op("pool", lambda e: e.memset(cbc[:], float(n) - 511.5), writes=[cbc])
                    nmc, nmn = mid, mid2
                    for k in range(NBIS):
                        act(mk[:, 0:n], scoreF[:, 0:n], AF.Sign, reads=[score, nmc], writes=[junkD, cntc], bias=nmc[:, 0:1],
                            accum_out=cntc[:, 0:1])
                        act(sgnc[:], cntc[:], AF.Sign, reads=[cntc, cbc], writes=[sgnc], bias=cbc[:, 0:1])
                        if k < NBIS - 1:
                            act(nmn[:], sgnc[:], AF.Identity, reads=[sgnc, nhwk, nmc], writes=[nmn], scale=nhwk[:, k + 1:k + 2], bias=nmc[:, 0:1])
                            nmc, nmn = nmn, nmc
                        yield
                    act(nmn[:], nmc[:], AF.Identity, reads=[nmc, nhwk], writes=[nmn], scale=-1.0, bias=nhwk[:, NBIS:NBIS + 1])
                    act(lo[:], sgnc[:], AF.Identity, reads=[sgnc, hwk, nmn], writes=[lo], scale=hwk[:, NBIS:NBIS + 1], bias=nmn[:, 0:1])
                else:
                    S.op("dve", lambda e: e.memset(lo[:], -1e29), writes=[lo])
                yield
                ts("dve", mk[:, 0:n], scoreF[:, 0:n], lo[:, 0:1], None, ALU.is_ge, reads=[score, lo], writes=[mk])
                yield
                if b == 0 and i == 2:
                    dump("thr2", lo[:], lo)
                yield
                for j0 in range(0, i + 1, 4):
                    nj = min(4, i + 1 - j0)
                    P = pab()
                    P16 = P[:].bitcast(BF16)
                    for jj in range(nj):
                        j = j0 + jj
                        tr(P16[:, jj * 128:(jj + 1) * 128], mk[:, j * 128:(j + 1) * 128], ident16[:], reads=[mk, ident16], writes=[P])
                    ts("dve", negmk[:, j0:j0 + nj, :], P16[:, 0:nj * 128].rearrange("p (a b) -> p a b", a=nj), 30000.0, -30000.0, ALU.mult, ALU.add,
                       reads=[P], writes=[negmk])
                yield
                qb2 = qbT[:].rearrange("p a b -> p (a b)")
                yield
                for j in range(i + 1):
                    P = pab()
                    P3v = P[:].rearrange("p (a b) -> p a b", a=4)
                    near = j >= i - 1
                    mm(P3v, ckvnT[:, j * 128:(j + 1) * 128], qbT[:], start=True, stop=False, reads=[ckvnT, qbT], writes=[P])
                    mm(P3v, ident16[:], negmk[:, j, :].unsqueeze(1).to_broadcast([128, 4, 128]), start=False, stop=not near,
                       reads=[ident16, negmk], writes=[P])
                    if near:
                        mm(P3v, ident16[:], EB[i - j][:], start=False, stop=True, reads=[ident16, EB[i - j]], writes=[P])
                    E = Eb[j % 2]
                    act(E[:], P3v, AF.Exp, reads=[P], writes=[E], scale=128.0 ** -0.5)
                    pm2 = E[:].rearrange("p a b -> p (a b)")
                    mm(PO[:], ckvn[:, j, :], pm2, start=(j == 0), stop=(j == i), reads=[ckvn, E], writes=[PO])
                    mm(PO2[:], ones16[:], pm2, start=(j == 0), stop=(j == i), reads=[ones16, E], writes=[PO2])
                    yield
                yield
                cp("act", obT[:], PO[:].rearrange("p (a b) -> p a b", a=4), reads=[PO], writes=[obT])
                yield
                act(rden[:], PO2[:], AF.Ln, reads=[PO2], writes=[rden])
                yield
                act(rden[:], rden[:], AF.Exp, reads=[rden], writes=[rden], scale=-1.0)
                yield
                tt("pool", rden[:], rden[:], szbT[:].rearrange("p a b -> p (a b)"), ALU.mult, reads=[rden, szbT], writes=[rden])
                yield
                PY = pab()
                for h in range(4):
                    hs = slice(h * 128, (h + 1) * 128)
                    mm(PY[:, hs], wuv16[:, h, :], obT[:, h, :], reads=[wuv16, obT], writes=[PY])
                yield
                tt("dve", ygT[:].rearrange("p a b -> p (a b)"), PY[:], Rg[:], ALU.mult, reads=[PY, Rg], writes=[ygT])
                yield
                if b == 0 and i <= 2:
                    dump(f"yg{i}", ygT[:].rearrange("p a b -> p (a b)"), ygT)
                yield
                yield
            streams = [[gdn_stream(), 3], [dsa_stream(), 1]]
            pre = pre_gen(b, i + 1) if i + 1 < NT else None
            seen = set()
            while streams or pre is not None:
                for ent in list(streams):
                    for _ in range(ent[1]):
                        try:
                            seen.add(next(ent[0]))
                        except StopIteration:
                            streams.remove(ent)
                            break
                if pre is not None and (("M" in seen and "B" in seen) or not streams):
                    for _ in range(1):
                        try:
                            next(pre)
                        except StopIteration:
                            pre = None
                            break
            xr = negmk[:].rearrange("p a b -> p (a b)").bitcast(F32)
            S.dma(xr, x_d[b, t0:t0 + 128, :], writes=[negmk])
            for nh in range(2):
                for c in range(8):
                    lhs = ogT[:, c, :] if c < 4 else ygT[:, c - 4, :]
                    mm(PT2h[nh][:], lhs, wout16[:, c, nh * 512:(nh + 1) * 512],
                       start=(c == 0), stop=(c == 7), reads=[ogT, ygT, wout16], writes=[PT2h[nh]])
            for nh in range(2):
                act(junkA[:, nh * 512:(nh + 1) * 512], PT2h[nh][:], AF.Square, reads=[PT2h[nh]],
                    writes=[junkA, msq], accum_out=msq[:, nh:nh + 1])
            tt("dve", ssq1[:], msq[:, 0:1], msq[:, 1:2], ALU.add, reads=[msq], writes=[ssq1])
            rsqrt_col(mrs, ssq1, 1, 1.0 / D, EPS)
            for nh in range(2):
                sl = slice(nh * 512, (nh + 1) * 512)
                stt(otmp[:, sl], PT2h[nh][:], mrs[:, 0:1], GATE[:, sl], ALU.mult, ALU.mult, reads=[PT2h[nh], mrs, GATE], writes=[otmp])
            tt("pool", otmp[:], otmp[:], xr, ALU.add, reads=[otmp, negmk], writes=[otmp])
            S.dma(out_d[b, t0:t0 + 128, :], otmp[:], reads=[otmp])
            tix += 1
    S.finish("sp")
    return nc, S


def _masks():
    i = np.arange(128)[:, None]; j = np.arange(128)[None, :]
    m = np.zeros((128, 8, 128), np.float32)
    for ls in range(7):
        sz = 1 << ls
        m[:, ls, :] = (((i // sz) % 2 == 1) & ((j // sz) == (i // sz) - 1)).astype(np.float32)
    m[:, 7, :] = m[:, 0, :].T
    return m


def _layout(inputs, core):
    f = lambda a: np.ascontiguousarray(a, dtype=np.float32)
    bs = slice(core * NBC, (core + 1) * NBC)
    kp = lambda w: w.reshape(8, 128, -1).transpose(1, 0, 2)
    w_in = inputs["w_in"][0]
    ik = w_in[:, O_IK:O_IK + 64]
    w_idx = np.concatenate([w_in[:, O_IQ:O_IQ + 512], ik, ik, w_in[:, O_IW:O_IW + 8]], axis=1)
    b_ada = inputs["b_ada"][0]
    return {
        "x": f(inputs["x"][bs]),
        "cT": f(inputs["c"][bs].T.reshape(8, 128, NBC).transpose(1, 0, 2)),
        "w_ada": f(kp(inputs["w_ada"][0])),
        "b_col": f(b_ada.reshape(24, 128).T),
        "b_gate": f(b_ada[2048:3072].reshape(1, 1024)),
        "g_pre": f(inputs["g_pre"][0].reshape(8, 128).T),
        "w_in": f(kp(w_in[:, :NW16])),
        "w_idx": f(kp(w_idx)),
        "conv_w": f(inputs["conv_w"][0].reshape(4, 12, 128).transpose(2, 1, 0)),
        "a_log": f(inputs["a_log"].reshape(1, 4)),
        "dt_bias": f(inputs["dt_bias"].reshape(1, 4)),
        "g_gdn": f(inputs["g_gdn"].reshape(1, 128)),
        "g_kv": f(inputs["g_kv"].reshape(1, 128)),
        "w_uv": f(inputs["w_uv"][0].transpose(1, 0, 2)),
        "rel_bias": f(inputs["rel_bias"].reshape(1, 128)),
        "w_out": f(kp(inputs["w_out"][0])),
        "g_post": f(inputs["g_post"].reshape(1, 1024)),
        "masks": _masks(),
    }


def kernel(**inputs):
    inputs = {k: np.asarray(v) for k, v in inputs.items()}
    nc, _ = build()
    in_maps = [_layout(inputs, c) for c in range(8)]
    res = run_bass_kernel_spmd(nc, in_maps, core_ids=list(range(8)))
    return np.concatenate([r["out"] for r in res.results], axis=0).astype(np.float32)
```

```python
import math
import numpy as np
import concourse.bass as bass
import concourse.mybir as mybir
from concourse.bass_utils import run_bass_kernel_spmd

F32 = mybir.dt.float32
BF16 = mybir.dt.bfloat16
AF = mybir.ActivationFunctionType
ALU = mybir.AluOpType
AX = mybir.AxisListType

D = 1024
L = 2048
NBC = 4
NT = 16
EPS = 1e-6
NEG = -30000.0
NBIS = 11


class Res:
    __slots__ = ("name", "w", "r")

    def __init__(self, name):
        self.name = name
        self.w = None
        self.r = {}


class Buf:
    def __init__(self, t, name):
        self.t = t
        self.r = Res(name)

    def __getitem__(self, k):
        return self.t[k]


class Sched:
    def __init__(self, nc, ndma=8):
        self.nc = nc
        self.e = {"pe": nc.tensor, "act": nc.scalar, "dve": nc.vector, "pool": nc.gpsimd, "sp": nc.sync}
        self.sem = {k: nc.alloc_semaphore("sem_" + k) for k in self.e}
        self.cnt = {k: 0 for k in self.e}
        self.seen = {k: {} for k in self.e}
        self.dsem = [nc.alloc_semaphore(f"dsem{i}") for i in range(ndma)]
        self.dcnt = [0] * ndma
        self.dnext = 0
        self.nwait = 0

    def _wait(self, eng, key, val):
        if self.seen[eng].get(key, 0) >= val:
            return
        self.seen[eng][key] = val
        sem = self.sem[key] if isinstance(key, str) else self.dsem[key[1]]
        self.e[eng].wait_ge(sem, val)
        self.nwait += 1

    def _deps(self, eng, reads, writes):
        need = {}
        for b in reads:
            r = b.r
            if r.w is not None:
                k, v = r.w
                need[k] = max(need.get(k, 0), v)
        for b in writes:
            w = b.r
            if w.w is not None:
                k, v = w.w
                need[k] = max(need.get(k, 0), v)
            for k, v in w.r.items():
                need[k] = max(need.get(k, 0), v)
        for k, v in need.items():
            if eng == "pe" and k == "pe":
                continue
            self._wait(eng, k, v)

    def op(self, eng, fn, reads=(), writes=()):
        self._deps(eng, reads, writes)
        ins = fn(self.e[eng])
        self.cnt[eng] += 1
        ins.then_inc(self.sem[eng], 1)
        v = self.cnt[eng]
        for b in reads:
            b.r.r[eng] = v
        for b in writes:
            b.r.w = (eng, v)
            b.r.r = {}
        return ins

    def dma(self, out, in_, reads=(), writes=(), q="sp", **kw):
        slot = self.dnext
        self.dnext = (self.dnext + 1) % len(self.dsem)
        key = ("d", slot)
        if self.dcnt[slot] > 0:
            self._wait(q, key, self.dcnt[slot])
        self._deps(q, reads, writes)
        self.dcnt[slot] += 16
        self.e[q].dma_start(out=out, in_=in_, **kw).then_inc(self.dsem[slot], 16)
        v = self.dcnt[slot]
        for b in reads:
            b.r.r[key] = v
        for b in writes:
            b.r.w = (key, v)
            b.r.r = {}

    def finish(self, eng="sp"):
        for k in self.cnt:
            if self.cnt[k] > 0 and k != eng:
                self._wait(eng, k, self.cnt[k])
        for i, c in enumerate(self.dcnt):
            if c > 0:
                self._wait(eng, ("d", i), c)


def t5_bucket_np(n):
    n = np.maximum(n, 0)
    nf = np.maximum(n, 1).astype(np.float32)
    large = 16 + (np.log(nf / np.float32(16)) / np.float32(math.log(128 / 16)) * np.float32(16)).astype(np.int32)
    large = np.minimum(large, 31)
    return np.where(n < 16, n, large)


O_QKV, O_ZA, O_B, O_A, O_QB, O_CKV, O_ZB, O_IQ, O_IK, O_IW = 0, 1536, 2048, 2052, 2056, 2568, 2696, 3208, 3720, 3784
NW16 = 3208
NIDX = 648


def build(debug=None):
    nc = bass.Bass("TRN2", target_bir_lowering=False)
    din = lambda n, sh: nc.dram_tensor(n, sh, F32, kind="ExternalInput").ap()
    x_d = din("x", [NBC, L, D])
    cT_d = din("cT", [128, 8, NBC])
    wada_d = din("w_ada", [128, 8, 3072])
    bcol_d = din("b_col", [128, 24])
    bgate_d = din("b_gate", [1, 1024])
    gpre_d = din("g_pre", [128, 8])
    win_d = din("w_in", [128, 8, NW16])
    widx_d = din("w_idx", [128, 8, NIDX])
    conv_d = din("conv_w", [128, 12, 4])
    alog_d = din("a_log", [1, 4])
    dtb_d = din("dt_bias", [1, 4])
    ggdn_d = din("g_gdn", [1, 128])
    gkv_d = din("g_kv", [1, 128])
    wuv_d = din("w_uv", [128, 4, 128])
    rb_d = din("rel_bias", [1, 128])
    wout_d = din("w_out", [128, 8, 1024])
    gpost_d = din("g_post", [1, 1024])
    msk_d = din("masks", [128, 8, 128])
    out_d = nc.dram_tensor("out", [NBC, L, D], F32, kind="ExternalOutput").ap()
    dbg_d = {}
    if debug:
        for n, sh in debug.items():
            dbg_d[n] = nc.dram_tensor("dbg_" + n, list(sh), F32, kind="ExternalOutput").ap()

    S = Sched(nc)
    cnt = [0]

    def sb(shape, dt=F32, name=None):
        cnt[0] += 1
        name = "s_" + (name or f"t{cnt[0]}")
        return Buf(nc.alloc_sbuf_tensor(name, list(shape), dt), name)

    def ps(shape, dt=F32, name=None):
        cnt[0] += 1
        name = "p_" + (name or f"p{cnt[0]}")
        return Buf(nc.alloc_psum_tensor(name, list(shape), dt), name)

    def mm(out, lhsT, rhs, start=True, stop=True, reads=(), writes=()):
        S.op("pe", lambda e: e.matmul(out, lhsT, rhs, start=start, stop=stop), reads, writes)

    def tr(out, in_, ident, reads=(), writes=()):
        S.op("pe", lambda e: e.transpose(out, in_, ident), reads, writes)

    def act(out, in_, func, reads=(), writes=(), **kw):
        S.op("act", lambda e: e.activation(out, in_, func, **kw), reads, writes)

    def tt(eng, out, in0, in1, op, reads=(), writes=()):
        S.op(eng, lambda e: e.tensor_tensor(out, in0, in1, op=op), reads, writes)

    def ts(eng, out, in0, s1, s2, op0, op1=None, reads=(), writes=(), accum_out=None):
        if op1 is None:
            S.op(eng, lambda e: e.tensor_scalar(out, in0, s1, None, op0=op0), reads, writes)
        elif accum_out is not None:
            S.op(eng, lambda e: e.tensor_scalar(out, in0, s1, s2, op0=op0, op1=op1, accum_out=accum_out), reads, writes)
        else:
            S.op(eng, lambda e: e.tensor_scalar(out, in0, s1, s2, op0=op0, op1=op1), reads, writes)

    def stt(out, in0, scalar, in1, op0, op1, reads=(), writes=()):
        S.op("dve", lambda e: e.scalar_tensor_tensor(out, in0, scalar, in1, op0=op0, op1=op1), reads, writes)

    def cp(eng, out, in_, reads=(), writes=()):
        if eng == "act":
            S.op("act", lambda e: e.copy(out, in_), reads, writes)
        else:
            S.op(eng, lambda e: e.tensor_copy(out, in_), reads, writes)

    def dump(name, ap, buf):
        if name in dbg_d:
            S.dma(dbg_d[name], ap, reads=[buf], q="pool")

    big4 = sb([128, 1024], name="big4")
    rtmp2 = sb([128, 512], name="rtmp2")

    def view(buf, ap):
        v = Buf.__new__(Buf)
        v.t = ap
        v.r = buf.r
        return v
    io = view(big4, big4[:, 0:128])
    S.op("pool", lambda e: e.iota(io[:], [[1, 128]], base=0, channel_multiplier=-1,
                                  allow_small_or_imprecise_dtypes=True), writes=[io])
    ident = sb([128, 128], name="ident")
    ident16 = sb([128, 128], BF16, name="ident16")
    Umat = sb([128, 128], name="Umat")
    SLmat = sb([128, 128], name="SLmat")
    NEGs = sb([128, 128], name="NEGs")
    NEGsT = sb([128, 128], name="NEGsT")
    NEGC = sb([128, 128], name="NEGC")
    ones32 = sb([128, 128], name="ones32")
    ones16 = sb([128, 128], BF16, name="ones16")
    mhalf = sb([128, 8], name="mhalf")
    ts("dve", ident[:], io[:], 0.0, None, ALU.is_equal, reads=[io], writes=[ident])
    ts("dve", ident16[:], io[:], 0.0, None, ALU.is_equal, reads=[io], writes=[ident16])
    ts("dve", Umat[:], io[:], 0.0, None, ALU.is_ge, reads=[io], writes=[Umat])
    ts("dve", SLmat[:], io[:], 0.0, None, ALU.is_lt, reads=[io], writes=[SLmat])
    ts("dve", NEGs[:], io[:], 0.0, NEG, ALU.is_ge, ALU.mult, reads=[io], writes=[NEGs])
    ts("dve", NEGsT[:], io[:], 0.0, NEG, ALU.is_le, ALU.mult, reads=[io], writes=[NEGsT])
    ts("dve", NEGC[:], io[:], 0.0, -1e30, ALU.is_gt, ALU.mult, reads=[io], writes=[NEGC])
    S.op("pool", lambda e: e.memset(ones32[:], 1.0), writes=[ones32])
    S.op("pool", lambda e: e.memset(ones16[:], 1.0), writes=[ones16])
    S.op("pool", lambda e: e.memset(mhalf[:], -0.5), writes=[mhalf])
    pow2 = sb([128, NBIS + 1], name="pow2")
    for k in range(NBIS + 1):
        S.op("pool", lambda e, k=k: e.memset(pow2[:, k:k + 1], 0.5 ** (k + 1)), writes=[pow2])

    PT2a = ps([128, 512], name="PT2a")
    PT2b = ps([128, 512], name="PT2b")
    PT2h = [PT2a, PT2b]
    rotg = [0]

    def pabG():
        rotg[0] ^= 1
        return PT2a if rotg[0] else PT2b
    PA = ps([128, 512], name="PA")
    PB = ps([128, 512], name="PB")
    PC = ps([128, 512], name="PC")
    PO = ps([128, 512], name="PO")
    PO2 = ps([128, 512], name="PO2")
    PS_ = ps([128, 512], name="PS_")
    rot = [0]

    def pab():
        rot[0] ^= 1
        return PA if rot[0] else PB

    stage = [sb([128, 8, 256], name="stage0"), sb([128, 8, 256], name="stage1")]
    w16 = sb([128, 8, NW16], BF16, name="w16")
    widx = sb([128, 8, NIDX], name="widx")
    wout16 = sb([128, 8, 1024], BF16, name="wout16")
    wuv16 = sb([128, 4, 128], BF16, name="wuv16")
    S.dma(widx[:], widx_d, writes=[widx])
    si = 0
    for c0 in range(0, NW16, 256):
        w = min(256, NW16 - c0)
        st = stage[si % 2]; si += 1
        S.dma(st[:, :, 0:w], win_d[:, :, c0:c0 + w], writes=[st])
        cp("dve" if si % 2 else "pool", w16[:, :, c0:c0 + w], st[:, :, 0:w], reads=[st], writes=[w16])
    for c0 in range(0, 1024, 256):
        st = stage[si % 2]; si += 1
        S.dma(st[:], wout_d[:, :, c0:c0 + 256], writes=[st])
        cp("dve" if si % 2 else "pool", wout16[:, :, c0:c0 + 256], st[:], reads=[st], writes=[wout16])
    st = stage[si % 2]; si += 1
    S.dma(st[:, 0:4, 0:128], wuv_d, writes=[st])
    cp("dve", wuv16[:], st[:, 0:4, 0:128], reads=[st], writes=[wuv16])

    st = stage[si % 2]; si += 1
    S.dma(st[:, :, 0:128], msk_d, writes=[st])
    msk = sb([128, 8, 128], BF16, name="msk")
    cp("dve", msk[:], st[:, :, 0:128], reads=[st], writes=[msk])
    convw = sb([128, 12, 4], name="convw")
    S.dma(convw[:], conv_d, writes=[convw])
    diagw = sb([128, 12, 4, 128], BF16, name="diagw")
    for ch in range(12):
        for j in range(4):
            ts("dve" if (ch + j) % 2 else "pool", diagw[:, ch, j, :], ident[:], convw[:, ch, j:j + 1], None, ALU.mult,
               reads=[ident, convw], writes=[diagw])

    def bcast_row(src, n, name):
        t = sb([128, n], name=name)
        S.dma(t[:], src.partition_broadcast(128), writes=[t])
        return t
    alogB = bcast_row(alog_d, 4, "alogB")
    dtbB = bcast_row(dtb_d, 4, "dtbB")
    ggdnB = bcast_row(ggdn_d, 128, "ggdnB")
    gkvB = bcast_row(gkv_d, 128, "gkvB")
    rbB = bcast_row(rb_d, 128, "rbB")
    negA = sb([128, 4], name="negA")
    act(negA[:], alogB[:], AF.Exp, reads=[alogB], writes=[negA])
    ts("dve", negA[:], negA[:], -1.0, None, ALU.mult, reads=[negA], writes=[negA])
    ggdnH = ggdnB

    cT = sb([128, 8, NBC], name="cT")
    S.dma(cT[:], cT_d, writes=[cT])
    sc = sb([128, 8, NBC], name="sc")
    act(sc[:], cT[:], AF.Silu, reads=[cT], writes=[sc])
    bcol = sb([128, 24], name="bcol")
    S.dma(bcol[:], bcol_d, writes=[bcol])
    gpre = sb([128, 8], name="gpre")
    S.dma(gpre[:], gpre_d, writes=[gpre])
    shiftc = sb([128, 8, NBC], name="shiftc")
    Gc = sb([128, 8, NBC], name="Gc")
    GATE = sb([128, 1024], name="GATE")
    for n8 in range(8):
        st = stage[si % 2]; si += 1
        S.dma(st[:], wada_d[:, :, n8 * 256:(n8 + 1) * 256], writes=[st])
        for q2 in range(2):
            dch = (n8 % 4) * 2 + q2
            for kc in range(8):
                mm(PC[:, q2 * 4:q2 * 4 + 4], st[:, kc, q2 * 128:(q2 + 1) * 128], sc[:, kc, :],
                   start=(kc == 0), stop=(kc == 7), reads=[st, sc], writes=[PC])
            dst = shiftc if n8 < 4 else Gc
            ts("dve", dst[:, dch, :], PC[:, q2 * 4:q2 * 4 + 4], bcol[:, n8 * 2 + q2:n8 * 2 + q2 + 1], None, ALU.add,
               reads=[PC, bcol], writes=[dst])
    ts("dve", Gc[:], Gc[:], 1.0, None, ALU.add, reads=[Gc], writes=[Gc])
    tt("dve", Gc[:], Gc[:], gpre[:].unsqueeze(2).to_broadcast([128, 8, NBC]), ALU.mult, reads=[Gc, gpre], writes=[Gc])

    bk = t5_bucket_np(np.arange(256))
    lo_b = [int(np.argmax(bk >= b)) if (bk >= b).any() else 100000 for b in range(32)]
    rb3 = rbB[:].rearrange("p (b h) -> p b h", h=4)
    dlt = sb([128, 32, 4], name="dlt")
    cp("dve", dlt[:, 0:1, :], rb3[:, 0:1, :], reads=[rbB], writes=[dlt])
    tt("dve", dlt[:, 1:32, :], rb3[:, 1:32, :], rb3[:, 0:31, :], ALU.subtract, reads=[rbB], writes=[dlt])
    EB = [sb([128, 4, 128], BF16, name=f"EB{t}") for t in range(2)]
    EBf = view(rtmp2, rtmp2[:, 0:128])
    rtmp = sb([128, 512], name="rtmp")
    gUb = [sb([128, 128], name=f"gU{i}") for i in range(2)]
    distT = gUb[0]
    tmpb = gUb[1]
    for typ in range(2):
        ts("dve", distT[:], io[:], float(128 * typ), None, ALU.add, reads=[io], writes=[distT])
        for h in range(4):
            acc = EBf
            ts("dve", acc[:], ones32[:], dlt[:, 0, h:h + 1], rb3[:, 31, h:h + 1], ALU.mult, ALU.subtract,
               reads=[ones32, dlt, rbB], writes=[acc])
            for bb in range(1, 32):
                if lo_b[bb] > 255:
                    continue
                ts("dve", tmpb[:], distT[:], float(lo_b[bb]) - 0.5, dlt[:, bb, h:h + 1], ALU.is_ge, ALU.mult,
                   reads=[distT, dlt], writes=[tmpb])
                tt("dve", acc[:], acc[:], tmpb[:], ALU.add, reads=[acc, tmpb], writes=[acc])
            ts("dve", acc[:], acc[:], 128.0 ** 0.5, None, ALU.mult, reads=[acc], writes=[acc])
            cp("dve", EB[typ][:, h, :], acc[:], reads=[acc], writes=[EB[typ]])

    xt = sb([128, 1024], name="xt")
    junkA = big4; xs = big4; otmp = big4
    hT32 = sb([128, 8, 128], name="hT32")
    hT16 = sb([128, 8, 128], BF16, name="hT16")
    uT = sb([128, 12, 131], BF16, name="uT")
    qkvs = sb([128, 1536], BF16, name="qkvs")
    col = lambda n, name: sb([128, n], name=name)
    ssq1 = col(1, "ssq1"); rstd1 = col(1, "rstd1"); ssqP2 = col(2, "ssqP2"); ssqP = col(1, "ssqP"); rstdP = col(1, "rstdP")
    xs2 = sb([128, 512], name="xs2")
    ssq8 = col(8, "ssq8"); rs8 = col(8, "rs8")
    ba = col(8, "ba"); beta = col(4, "beta"); gcol = col(4, "gcol"); gc = col(4, "gc"); glB = col(4, "glB")
    egc = col(4, "egc"); ekg = col(4, "ekg"); egl = col(4, "egl"); tmp4 = col(4, "tmp4"); nbeta = col(4, "nbeta")
    cf = {n: col(4, "cf_" + n) for n in ("kbg", "kg", "qg")}
    khat = sb([128, 4, 128], BF16, name="khat"); qhat = sb([128, 4, 128], BF16, name="qhat")
    qg = sb([128, 4, 128], BF16, name="qg"); kbg = sb([128, 4, 128], BF16, name="kbg")
    kg = sb([128, 4, 128], BF16, name="kg"); vb = sb([128, 4, 128], BF16, name="vb")
    khT = sb([128, 4, 128], BF16, name="khT"); qhT = sb([128, 4, 128], BF16, name="qhT"); qgT = sb([128, 4, 128], BF16, name="qgT")
    Es = sb([128, 4, 128], BF16, name="Es"); EsT = sb([128, 4, 128], BF16, name="EsT")
    Xb = [sb([128, 4, 128], BF16, name="X0")]
    XTb = [sb([128, 4, 128], BF16, name="XT0")]
    Tb = [sb([128, 4, 128], BF16, name=f"T{i}") for i in range(2)]
    TTb = [sb([128, 4, 128], BF16, name=f"TT{i}") for i in range(2)]
    Wp = khat
    attnT = sb([128, 4, 128], BF16, name="attnT")
    negwT = qhat
    vnew = qg
    S32 = sb([128, 4, 128], name="S32"); S16 = sb([128, 4, 128], BF16, name="S16")
    zas = sb([128, 512], BF16, name="zas"); G1 = zas
    osq4 = col(4, "osq4"); ors4 = col(4, "ors4")
    og = sb([128, 4, 128], BF16, name="og"); ogT = sb([128, 4, 128], BF16, name="ogT")
    qbT = sb([128, 4, 128], BF16, name="qbT"); szbT = sb([128, 4, 128], BF16, name="szbT")
    iqT = sb([128, 4, 128], name="iqT"); iw = col(8, "iw")
    ckvn = sb([128, NT, 128], BF16, name="ckvn"); ckvnT = sb([128, L], BF16, name="ckvnT")
    ikTb = stage[1]
    ikT = ikTb[:].rearrange("p a b -> p (a b)")
    score = stage[0]
    scoreF = score[:].rearrange("p a b -> p (a b)")
    mk = sb([128, L], BF16, name="mk")
    junkD = mk
    junkDF = mk
    lo = col(1, "lo"); hw0 = col(1, "hw0"); hwk = col(NBIS + 1, "hwk"); nhwk = col(NBIS + 1, "nhwk"); mid = col(1, "mid"); mid2 = col(1, "mid2"); sgnc = col(1, "sgnc"); cbc = col(1, "cbc"); cntc = col(1, "cntc"); tstep = col(1, "tstep")
    Eb = [sb([128, 4, 128], BF16, name=f"Eb{i}") for i in range(2)]
    negmk = sb([128, NT, 128], BF16, name="negmk"); mkT = negmk
    obT = sb([128, 4, 128], BF16, name="obT")
    rden = rtmp; Rg = rden
    ygT = sb([128, 4, 128], BF16, name="ygT")
    msq = col(2, "msq"); mrs = col(1, "mrs")

    def rsqrt_col(dst, src, n, scale, eps):
        ts("dve", dst[:, 0:n], src[:, 0:n], scale, eps, ALU.mult, ALU.add, reads=[src], writes=[dst])
        tt("pool", dst[:, 0:n], dst[:, 0:n], mhalf[:, 0:n], ALU.pow, reads=[dst, mhalf], writes=[dst])

    def pre_gen(b, i):
        t0 = i * 128
        uc = uT
        S.dma(xt[:], x_d[b, t0:t0 + 128, :], writes=[xt])
        yield
        for hf in range(2):
            act(xs2[:], xt[:, hf * 512:(hf + 1) * 512], AF.Square, reads=[xt], writes=[xs2, ssqP2], accum_out=ssqP2[:, hf:hf + 1])
        tt("pool", ssqP[:], ssqP2[:, 0:1], ssqP2[:, 1:2], ALU.add, reads=[ssqP2], writes=[ssqP])
        rsqrt_col(rstdP, ssqP, 1, 1.0 / D, EPS)
        yield
        for hf in range(2):
            ts("dve", xs2[:], xt[:, hf * 512:(hf + 1) * 512], rstdP[:, 0:1], None, ALU.mult, reads=[xt, rstdP], writes=[xs2])
            for c4 in range(4):
                tr(PC[:, c4 * 128:(c4 + 1) * 128], xs2[:, c4 * 128:(c4 + 1) * 128], ident[:], reads=[xs2, ident], writes=[PC])
            for c4 in range(4):
                c = hf * 4 + c4
                ts("dve", hT32[:, c, :], PC[:, c4 * 128:(c4 + 1) * 128], Gc[:, c, b:b + 1], shiftc[:, c, b:b + 1], ALU.mult, ALU.add,
                   reads=[PC, Gc, shiftc], writes=[hT32])
            yield
        cp("pool", hT16[:], hT32[:], reads=[hT32], writes=[hT16])
        if b == 0 and i == 0:
            dump("hT", hT32[:], hT32)
        yield
        for g3 in range(3):
            for q4 in range(4):
                ch = g3 * 4 + q4
                for kc in range(8):
                    mm(PC[:, q4 * 128:(q4 + 1) * 128], w16[:, kc, O_QKV + ch * 128:O_QKV + (ch + 1) * 128], hT16[:, kc, :],
                       start=(kc == 0), stop=(kc == 7), reads=[w16, hT16], writes=[PC])
            cp("dve", uc[:, g3 * 4:(g3 + 1) * 4, 3:131], PC[:].rearrange("p (a b) -> p a b", a=4), reads=[PC], writes=[uc])
            yield
        for g3 in range(3):
            for q4 in range(4):
                ch = g3 * 4 + q4
                for j in range(4):
                    mm(PC[:, q4 * 128:(q4 + 1) * 128], uc[:, ch, j:j + 128], diagw[:, ch, j, :],
                       start=(j == 0), stop=(j == 3), reads=[uc, diagw], writes=[PC])
            act(qkvs[:, g3 * 512:(g3 + 1) * 512], PC[:], AF.Silu, reads=[PC], writes=[qkvs])
            yield
        cp("pool", uc[:, :, 0:3], uc[:, :, 128:131], reads=[uc], writes=[uc])
        if b == 0 and i == 0:
            dump("qkvs", qkvs[:], qkvs)
        for hf in range(2):
            tt("dve", xs2[:], qkvs[:, hf * 512:(hf + 1) * 512], qkvs[:, hf * 512:(hf + 1) * 512], ALU.mult, reads=[qkvs], writes=[xs2])
            S.op("dve", lambda e, hf=hf: e.tensor_reduce(ssq8[:, hf * 4:(hf + 1) * 4], xs2[:].rearrange("p (a b) -> p a b", a=4), axis=AX.X, op=ALU.add),
                 reads=[xs2], writes=[ssq8])
        rsqrt_col(rs8, ssq8, 8, 1.0, EPS)
        ts("dve", rs8[:, 0:4], rs8[:, 0:4], 128.0 ** -0.5, None, ALU.mult, reads=[rs8], writes=[rs8])
        yield
        for c4 in range(4):
            for kc in range(8):
                mm(PC[:, c4 * 128:(c4 + 1) * 128], widx[:, kc, c4 * 128:(c4 + 1) * 128], hT32[:, kc, :],
                   start=(kc == 0), stop=(kc == 7), reads=[widx, hT32], writes=[PC])
            if c4 % 2:
                yield
        cp("dve", iqT[:], PC[:].rearrange("p (a b) -> p a b", a=4), reads=[PC], writes=[iqT])
        yield
        for kc in range(8):
            mm(PC[:, 0:128], widx[:, kc, 512:640], hT32[:, kc, :], start=(kc == 0), stop=(kc == 7), reads=[widx, hT32], writes=[PC])
        cp("dve", ikT[:, t0:t0 + 128], PC[:, 0:128], reads=[PC], writes=[ikTb])
        for kc in range(8):
            mm(PC[:, 0:8], hT32[:, kc, :], widx[:, kc, 640:648], start=(kc == 0), stop=(kc == 7), reads=[hT32, widx], writes=[PC])
        ts("dve", iw[:], PC[:, 0:8], (8.0 * 64.0) ** -0.5, None, ALU.mult, reads=[PC], writes=[iw])
        yield
        for kc in range(8):
            mm(PC[:, 16:24], hT16[:, kc, :], w16[:, kc, O_B:O_B + 8], start=(kc == 0), stop=(kc == 7), reads=[hT16, w16], writes=[PC])
        for kc in range(8):
            mm(PC[:, 128:256], hT16[:, kc, :], w16[:, kc, O_CKV:O_CKV + 128], start=(kc == 0), stop=(kc == 7), reads=[hT16, w16], writes=[PC])
        cp("dve", ba[:], PC[:, 16:24], reads=[PC], writes=[ba])
        act(xs2[:, 0:128], PC[:, 128:256], AF.Square, reads=[PC], writes=[xs2, ssqP], accum_out=ssqP[:, 0:1])
        rsqrt_col(rstdP, ssqP, 1, 1.0 / 128, EPS)
        stt(ckvn[:, i, :], PC[:, 128:256], rstdP[:, 0:1], gkvB[:], ALU.mult, ALU.mult, reads=[PC, rstdP, gkvB], writes=[ckvn])
        yield
        PC16 = PC[:].bitcast(BF16)
        tr(PC16[:, 512:640], ckvn[:, i, :], ident16[:], reads=[ckvn, ident16], writes=[PC])
        cp("dve", ckvnT[:, t0:t0 + 128], PC16[:, 512:640], reads=[PC], writes=[ckvnT])
        yield

    tix = 0
    screp = xt[:].rearrange("p (a b) -> p a b", a=8)
    for b in range(NBC):
        S.op("pool", lambda e: e.memset(S32[:], 0.0), writes=[S32])
        S.op("pool", lambda e: e.memset(S16[:], 0.0), writes=[S16])
        S.op("pool", lambda e: e.memset(uT[:, :, 0:3], 0.0), writes=[uT])
        for kc in range(8):
            cp("dve" if kc % 2 else "pool", screp[:, kc, :], sc[:, kc, b:b + 1].to_broadcast([128, 128]), reads=[sc], writes=[xt])
        for g4 in range(4):
            st = stage[g4 % 2]
            g0 = g4 * 256
            S.dma(st[:], wada_d[:, :, 2048 + g0:2048 + g0 + 256], writes=[st])
            S.dma(rtmp[:, 0:256], bgate_d[:, g0:g0 + 256].partition_broadcast(128), writes=[rtmp])
            S.dma(rtmp[:, 256:512], gpost_d[:, g0:g0 + 256].partition_broadcast(128), writes=[rtmp])
            P = pab()
            for kc in range(8):
                mm(P[:, 0:256], screp[:, kc, :], st[:, kc, :], start=(kc == 0), stop=(kc == 7), reads=[xt, st], writes=[P])
            tt("dve", GATE[:, g0:g0 + 256], P[:, 0:256], rtmp[:, 0:256], ALU.add, reads=[P, rtmp], writes=[GATE])
            tt("pool", GATE[:, g0:g0 + 256], GATE[:, g0:g0 + 256], rtmp[:, 256:512], ALU.mult, reads=[GATE, rtmp], writes=[GATE])
        for i in range(NT):
            t0 = i * 128
            uc = uT
            if i == 0:
                for _ in pre_gen(b, 0):
                    pass
            P = pab()
            for kc in range(8):
                mm(P[:], hT16[:, kc, :], w16[:, kc, O_ZA:O_ZA + 512], start=(kc == 0), stop=(kc == 7), reads=[hT16, w16], writes=[P])
            act(zas[:], P[:], AF.Silu, reads=[P], writes=[zas])
            tt("pool", zas[:].rearrange("p (h v) -> p h v", h=4), zas[:].rearrange("p (h v) -> p h v", h=4),
               ggdnH[:].unsqueeze(1).to_broadcast([128, 4, 128]), ALU.mult, reads=[zas, ggdnH], writes=[zas])
            def gdn_stream():
                act(beta[:], ba[:, 0:4], AF.Exp, reads=[ba], writes=[beta], scale=-1.0)
                yield
                ts("dve", beta[:], beta[:], 1.0, None, ALU.add, reads=[beta], writes=[beta])
                yield
                S.op("dve", lambda e: e.reciprocal(beta[:], beta[:]), reads=[beta], writes=[beta])
                yield
                ts("dve", nbeta[:], beta[:], -1.0, None, ALU.mult, reads=[beta], writes=[nbeta])
                yield
                tt("dve", tmp4[:], ba[:, 4:8], dtbB[:], ALU.add, reads=[ba, dtbB], writes=[tmp4])
                yield
                act(tmp4[:], tmp4[:], AF.Exp, reads=[tmp4], writes=[tmp4])
                yield
                act(tmp4[:], tmp4[:], AF.Ln, reads=[tmp4], writes=[tmp4], bias=1.0)
                yield
                tt("dve", gcol[:], tmp4[:], negA[:], ALU.mult, reads=[tmp4, negA], writes=[gcol])
                yield
                mm(PC[:, 16:20], Umat[:], gcol[:], reads=[Umat, gcol], writes=[PC])
                yield
                mm(PC[:, 20:24], ones32[:], gcol[:], reads=[ones32, gcol], writes=[PC])
                yield
                cp("dve", gc[:], PC[:, 16:20], reads=[PC], writes=[gc])
                yield
                cp("dve", glB[:], PC[:, 20:24], reads=[PC], writes=[glB])
                yield
                act(egc[:], gc[:], AF.Exp, reads=[gc], writes=[egc])
                yield
                act(egl[:], glB[:], AF.Exp, reads=[glB], writes=[egl])
                yield
                tt("dve", tmp4[:], glB[:], gc[:], ALU.subtract, reads=[glB, gc], writes=[tmp4])
                yield
                act(ekg[:], tmp4[:], AF.Exp, reads=[tmp4], writes=[ekg])
                yield
                tt("dve", cf["kbg"][:], rs8[:, 4:8], beta[:], ALU.mult, reads=[rs8, beta], writes=[cf["kbg"]])
                yield
                tt("dve", cf["kbg"][:], cf["kbg"][:], egc[:], ALU.mult, reads=[cf["kbg"], egc], writes=[cf["kbg"]])
                yield
                tt("dve", cf["kg"][:], rs8[:, 4:8], ekg[:], ALU.mult, reads=[rs8, ekg], writes=[cf["kg"]])
                yield
                tt("dve", cf["qg"][:], rs8[:, 0:4], egc[:], ALU.mult, reads=[rs8, egc], writes=[cf["qg"]])
                yield
                q3 = qkvs[:, 0:512].rearrange("p (h d) -> p h d", h=4)
                yield
                k3 = qkvs[:, 512:1024].rearrange("p (h d) -> p h d", h=4)
                yield
                v3 = qkvs[:, 1024:1536].rearrange("p (h d) -> p h d", h=4)
                yield
                bc = lambda c, lo_=0: c[:, lo_:lo_ + 4].unsqueeze(2).to_broadcast([128, 4, 128])
                yield
                tt("dve", khat[:], k3, bc(rs8, 4), ALU.mult, reads=[qkvs, rs8], writes=[khat])
                yield
                tt("pool", qhat[:], q3, bc(rs8, 0), ALU.mult, reads=[qkvs, rs8], writes=[qhat])
                yield
                tt("dve", qg[:], q3, bc(cf["qg"]), ALU.mult, reads=[qkvs, cf["qg"]], writes=[qg])
                yield
                tt("pool", kbg[:], k3, bc(cf["kbg"]), ALU.mult, reads=[qkvs, cf["kbg"]], writes=[kbg])
                yield
                tt("dve", kg[:], k3, bc(cf["kg"]), ALU.mult, reads=[qkvs, cf["kg"]], writes=[kg])
                yield
                tt("pool", vb[:], v3, bc(beta), ALU.mult, reads=[qkvs, beta], writes=[vb])
                yield "M"
                for src, dst in ((khat, khT), (qhat, qhT), (qg, qgT)):
                    P = pabG()
                    P16 = P[:].bitcast(BF16)
                    for h in range(4):
                        tr(P16[:, h * 128:(h + 1) * 128], src[:, h, :], ident16[:], reads=[src, ident16], writes=[P])
                    cp("act", dst[:], P16[:, 0:512].rearrange("p (a b) -> p a b", a=4), reads=[P], writes=[dst])
                yield
                PD = pabG(); PDT = pabG()
                yield
                for h in range(4):
                    gU = gUb[h % 2]
                    ts("dve" if h % 2 else "pool", gU[:], Umat[:], gcol[:, h:h + 1], None, ALU.mult, reads=[Umat, gcol], writes=[gU])
                    mm(PD[:, h * 128:(h + 1) * 128], gU[:], SLmat[:], start=True, stop=False, reads=[gU, SLmat], writes=[PD])
                    mm(PD[:, h * 128:(h + 1) * 128], ident[:], NEGs[:], start=False, stop=True, reads=[ident, NEGs], writes=[PD])
                    mm(PDT[:, h * 128:(h + 1) * 128], SLmat[:], gU[:], start=True, stop=False, reads=[gU, SLmat], writes=[PDT])
                    mm(PDT[:, h * 128:(h + 1) * 128], ident[:], NEGsT[:], start=False, stop=True, reads=[ident, NEGsT], writes=[PDT])
                yield
                act(Es[:], PD[:].rearrange("p (a b) -> p a b", a=4), AF.Exp, reads=[PD], writes=[Es])
                yield
                act(EsT[:], PDT[:].rearrange("p (a b) -> p a b", a=4), AF.Exp, reads=[PDT], writes=[EsT])
                yield
                if b == 0 and i == 0:
                    dump("Es", Es[:].rearrange("p a b -> p (a b)"), Es)
                    dump("beta4", beta[:], beta); dump("gc4", gc[:], gc); dump("rs8", rs8[:], rs8)
                yield
                P = pabG()
                yield
                for h in range(4):
                    mm(P[:, h * 128:(h + 1) * 128], khT[:, h, :], khT[:, h, :], reads=[khT], writes=[P])
                yield
                X, XT = Xb[0], XTb[0]
                yield
                for h in range(4):
                    stt(X[:, h, :], P[:, h * 128:(h + 1) * 128], nbeta[:, h:h + 1], Es[:, h, :], ALU.mult, ALU.mult,
                        reads=[P, nbeta, Es], writes=[X])
                yield
                if b == 0 and i == 0:
                    dump("X0", X[:].rearrange("p a b -> p (a b)"), X)
                yield
                P = pabG()
                yield
                for h in range(4):
                    mm(P[:, h * 128:(h + 1) * 128], khT[:, h, :], qhT[:, h, :], reads=[khT, qhT], writes=[P])
                yield
                tt("pool", EsT[:], EsT[:], ident16[:].unsqueeze(1).to_broadcast([128, 4, 128]), ALU.add, reads=[EsT, ident16], writes=[EsT])
                yield
                tt("dve", attnT[:], P[:].rearrange("p (a b) -> p a b", a=4), EsT[:], ALU.mult, reads=[P, EsT], writes=[attnT])
                yield
                P = pabG()
                yield
                P16 = P[:].bitcast(BF16)
                yield
                for h in range(4):
                    tr(P16[:, h * 128:(h + 1) * 128], X[:, h, :], ident16[:], reads=[X, ident16], writes=[P])
                yield
                cp("act", XT[:], P16[:, 0:512].rearrange("p (a b) -> p a b", a=4), reads=[P], writes=[XT])
                yield
                bcm = lambda ls: msk[:, ls, :].unsqueeze(1).to_broadcast([128, 4, 128])
                yield
                Tc, TT = Tb[0], TTb[0]
                yield
                tt("pool", Tc[:], X[:], bcm(0), ALU.mult, reads=[X, msk], writes=[Tc])
                yield
                tt("pool", Tc[:], Tc[:], ident16[:].unsqueeze(1).to_broadcast([128, 4, 128]), ALU.add, reads=[Tc, ident16], writes=[Tc])
                yield
                tt("dve", TT[:], XT[:], bcm(7), ALU.mult, reads=[XT, msk], writes=[TT])
                yield
                tt("dve", TT[:], TT[:], ident16[:].unsqueeze(1).to_broadcast([128, 4, 128]), ALU.add, reads=[TT, ident16], writes=[TT])
                yield
                gen = 0
                yield
                for ls in range(1, 7):
                    Tn, TTn = Tb[1 - gen], TTb[1 - gen]
                    P1 = pabG()
                    for h in range(4):
                        mm(P1[:, h * 128:(h + 1) * 128], XT[:, h, :], Tc[:, h, :], reads=[XT, Tc], writes=[P1])
                    tt("dve", Wp[:], P1[:].rearrange("p (a b) -> p a b", a=4), bcm(ls), ALU.mult, reads=[P1, msk], writes=[Wp])
                    if ls < 6:
                        P2 = pabG()
                        for h in range(4):
                            mm(P2[:, h * 128:(h + 1) * 128], TT[:, h, :], Wp[:, h, :], reads=[TT, Wp], writes=[P2])
                        tt("dve", Tn[:], P2[:].rearrange("p (a b) -> p a b", a=4), Tc[:], ALU.add, reads=[P2, Tc], writes=[Tn])
                    P3 = PS_
                    for h in range(4):
                        mm(P3[:, h * 128:(h + 1) * 128], Wp[:, h, :], TT[:, h, :], reads=[Wp, TT], writes=[P3])
                    tt("dve", TTn[:], P3[:].rearrange("p (a b) -> p a b", a=4), TT[:], ALU.add, reads=[P3, TT], writes=[TTn])
                    Tc, TT = Tn, TTn
                    gen = 1 - gen
                    yield
                yield
                if b == 0 and i == 0:
                    dump("TTf", TT[:].rearrange("p a b -> p (a b)"), TT)
                    dump("attnT0", attnT[:].rearrange("p a b -> p (a b)"), attnT)
                yield
                P = pabG()
                yield
                for h in range(4):
                    mm(P[:, h * 128:(h + 1) * 128], kbg[:, h, :], TT[:, h, :], reads=[kbg, TT], writes=[P])
                yield
                ts("dve", negwT[:], P[:].rearrange("p (a b) -> p a b", a=4), -1.0, None, ALU.mult, reads=[P], writes=[negwT])
                yield
                PV = pabG()
                for h in range(4):
                    hs = slice(h * 128, (h + 1) * 128)
                    mm(PV[:, hs], TT[:, h, :], vb[:, h, :], start=True, stop=False, reads=[TT, vb], writes=[PV])
                    mm(PV[:, hs], negwT[:, h, :], S16[:, h, :], start=False, stop=True, reads=[negwT, S16], writes=[PV])
                yield
                cp("act", vnew[:], PV[:].rearrange("p (a b) -> p a b", a=4), reads=[PV], writes=[vnew])
                yield
                if b == 0 and i == 0:
                    dump("vnew0", vnew[:].rearrange("p a b -> p (a b)"), vnew)
                yield
                for h in range(4):
                    hs = slice(h * 128, (h + 1) * 128)
                    mm(PS_[:, hs], qgT[:, h, :], S16[:, h, :], start=True, stop=False, reads=[qgT, S16], writes=[PS_])
                    mm(PS_[:, hs], attnT[:, h, :], vnew[:, h, :], start=False, stop=True, reads=[attnT, vnew], writes=[PS_])
                yield
                PDS = pabG()
                for h in range(4):
                    hs = slice(h * 128, (h + 1) * 128)
                    mm(PDS[:, hs], kg[:, h, :], vnew[:, h, :], reads=[kg, vnew], writes=[PDS])
                yield
                for h in range(4):
                    hs = slice(h * 128, (h + 1) * 128)
                    stt(S32[:, h, :], S32[:, h, :], egl[:, h:h + 1], PDS[:, hs], ALU.mult, ALU.add, reads=[S32, egl, PDS], writes=[S32])
                yield
                cp("pool", S16[:], S32[:], reads=[S32], writes=[S16])
                yield
                act(junkA[:, 0:512], PS_[:], AF.Square, reads=[PS_], writes=[junkA])
                yield
                S.op("dve", lambda e: e.tensor_reduce(osq4[:], junkA[:, 0:512].rearrange("p (a b) -> p a b", a=4), axis=AX.X, op=ALU.add),
                     reads=[junkA], writes=[osq4])
                yield
                rsqrt_col(ors4, osq4, 4, 1.0 / 128, EPS)
                yield
                for h in range(4):
                    hs = slice(h * 128, (h + 1) * 128)
                    stt(og[:, h, :], PS_[:, hs], ors4[:, h:h + 1], G1[:, hs], ALU.mult, ALU.mult, reads=[PS_, ors4, G1], writes=[og])
                yield
                if b == 0 and i <= 1:
                    dump(f"og{i}", og[:].rearrange("p a b -> p (a b)"), og)
                yield
                P = pabG()
                yield
                P16 = P[:].bitcast(BF16)
                yield
                for h in range(4):
                    tr(P16[:, h * 128:(h + 1) * 128], og[:, h, :], ident16[:], reads=[og, ident16], writes=[P])
                yield
                cp("act", ogT[:], P16[:, 0:512].rearrange("p (a b) -> p a b", a=4), reads=[P], writes=[ogT])

                yield
            def dsa_stream():
                n = t0 + 128
                yield
                for s0 in range(0, n, 512):
                    w = min(512, n - s0)
                    for h in range(8):
                        P = pab()
                        pr = slice((h % 2) * 64, (h % 2) * 64 + 64)
                        mm(P[:, 0:w], iqT[pr, h // 2, :], ikT[pr, s0:s0 + w], reads=[iqT, ikTb], writes=[P])
                        if h == 0:
                            ts("dve", scoreF[:, s0:s0 + w], P[:, 0:w], 0.0, iw[:, 0:1], ALU.max, ALU.mult, reads=[P, iw], writes=[score])
                        else:
                            rt = rtmp if h % 2 else rtmp2
                            act(rt[:, 0:w], P[:, 0:w], AF.Relu, reads=[P], writes=[rt])
                            stt(scoreF[:, s0:s0 + w], rt[:, 0:w], iw[:, h:h + 1], scoreF[:, s0:s0 + w], ALU.mult, ALU.add,
                                reads=[rt, iw, score], writes=[score])
                        yield
                yield
                tt("dve", scoreF[:, t0:t0 + 128], scoreF[:, t0:t0 + 128], NEGC[:], ALU.add, reads=[score, NEGC], writes=[score])
                yield
                if b == 0 and i == 2:
                    dump("score2", scoreF[:, 0:384], score)
                yield
                yield "B"
                P = pab()
                for h in range(4):
                    for kc in range(8):
                        mm(P[:, h * 128:(h + 1) * 128], w16[:, kc, O_QB + h * 128:O_QB + (h + 1) * 128], hT16[:, kc, :],
                           start=(kc == 0), stop=(kc == 7), reads=[w16, hT16], writes=[P])
                cp("act", qbT[:], P[:].rearrange("p (a b) -> p a b", a=4), reads=[P], writes=[qbT])
                yield
                P = pab()
                for h in range(4):
                    for kc in range(8):
                        mm(P[:, h * 128:(h + 1) * 128], w16[:, kc, O_ZB + h * 128:O_ZB + (h + 1) * 128], hT16[:, kc, :],
                           start=(kc == 0), stop=(kc == 7), reads=[w16, hT16], writes=[P])
                act(szbT[:], P[:].rearrange("p (a b) -> p a b", a=4), AF.Silu, reads=[P], writes=[szbT])
                yield
                if i >= 2:
                    S.op("dve", lambda e: e.tensor_reduce(lo[:], scoreF[:, 0:t0], axis=AX.X, op=ALU.min), reads=[score], writes=[lo])
                    S.op("dve", lambda e: e.tensor_reduce(hw0[:], scoreF[:, 0:n], axis=AX.X, op=ALU.max), reads=[score], writes=[hw0])
                    tt("dve", hw0[:], hw0[:], lo[:], ALU.subtract, reads=[hw0, lo], writes=[hw0])
                    ts("dve", hw0[:], hw0[:], 1.0001, 1e-6, ALU.mult, ALU.add, reads=[hw0], writes=[hw0])
                    ts("dve", hwk[:], pow2[:], hw0[:, 0:1], None, ALU.mult, reads=[pow2, hw0], writes=[hwk])
                    ts("dve", nhwk[:], hwk[:], -1.0, None, ALU.mult, reads=[hwk], writes=[nhwk])
                    ts("dve", mid[:], lo[:], hwk[:, 0:1], -1.0, ALU.add, ALU.mult, reads=[lo, hwk], writes=[mid])
                    S.op("pool", lambda e: e.memset(cbc[:], float(n) - 511.5), writes=[cbc])
                    nmc, nmn = mid, mid2
                    for k in range(NBIS):
                        act(mk[:, 0:n], scoreF[:, 0:n], AF.Sign, reads=[score, nmc], writes=[junkD, cntc], bias=nmc[:, 0:1],
                            accum_out=cntc[:, 0:1])
                        act(sgnc[:], cntc[:], AF.Sign, reads=[cntc, cbc], writes=[sgnc], bias=cbc[:, 0:1])
                        if k < NBIS - 1:
                            act(nmn[:], sgnc[:], AF.Identity, reads=[sgnc, nhwk, nmc], writes=[nmn], scale=nhwk[:, k + 1:k + 2], bias=nmc[:, 0:1])
                            nmc, nmn = nmn, nmc
                        yield
                    act(nmn[:], nmc[:], AF.Identity, reads=[nmc, nhwk], writes=[nmn], scale=-1.0, bias=nhwk[:, NBIS:NBIS + 1])
                    act(lo[:], sgnc[:], AF.Identity, reads=[sgnc, hwk, nmn], writes=[lo], scale=hwk[:, NBIS:NBIS + 1], bias=nmn[:, 0:1])
                else:
                    S.op("dve", lambda e: e.memset(lo[:], -1e29), writes=[lo])
                yield
                ts("dve", mk[:, 0:n], scoreF[:, 0:n], lo[:, 0:1], None, ALU.is_ge, reads=[score, lo], writes=[mk])
                yield
                if b == 0 and i == 2:
                    dump("thr2", lo[:], lo)
                yield
                for j0 in range(0, i + 1, 4):
                    nj = min(4, i + 1 - j0)
                    P = pab()
                    P16 = P[:].bitcast(BF16)
                    for jj in range(nj):
                        j = j0 + jj
                        tr(P16[:, jj * 128:(jj + 1) * 128], mk[:, j * 128:(j + 1) * 128], ident16[:], reads=[mk, ident16], writes=[P])
                    ts("dve", negmk[:, j0:j0 + nj, :], P16[:, 0:nj * 128].rearrange("p (a b) -> p a b", a=nj), 30000.0, -30000.0, ALU.mult, ALU.add,
                       reads=[P], writes=[negmk])
                yield
                qb2 = qbT[:].rearrange("p a b -> p (a b)")
                yield
                for j in range(i + 1):
                    P = pab()
                    P3v = P[:].rearrange("p (a b) -> p a b", a=4)
                    near = j >= i - 1
                    mm(P3v, ckvnT[:, j * 128:(j + 1) * 128], qbT[:], start=True, stop=False, reads=[ckvnT, qbT], writes=[P])
                    mm(P3v, ident16[:], negmk[:, j, :].unsqueeze(1).to_broadcast([128, 4, 128]), start=False, stop=not near,
                       reads=[ident16, negmk], writes=[P])
                    if near:
                        mm(P3v, ident16[:], EB[i - j][:], start=False, stop=True, reads=[ident16, EB[i - j]], writes=[P])
                    E = Eb[j % 2]
                    act(E[:], P3v, AF.Exp, reads=[P], writes=[E], scale=128.0 ** -0.5)
                    pm2 = E[:].rearrange("p a b -> p (a b)")
                    mm(PO[:], ckvn[:, j, :], pm2, start=(j == 0), stop=(j == i), reads=[ckvn, E], writes=[PO])
                    mm(PO2[:], ones16[:], pm2, start=(j == 0), stop=(j == i), reads=[ones16, E], writes=[PO2])
                    yield
                yield
                cp("act", obT[:], PO[:].rearrange("p (a b) -> p a b", a=4), reads=[PO], writes=[obT])
                yield
                act(rden[:], PO2[:], AF.Ln, reads=[PO2], writes=[rden])
                yield
                act(rden[:], rden[:], AF.Exp, reads=[rden], writes=[rden], scale=-1.0)
                yield
                tt("pool", rden[:], rden[:], szbT[:].rearrange("p a b -> p (a b)"), ALU.mult, reads=[rden, szbT], writes=[rden])
                yield
                PY = pab()
                for h in range(4):
                    hs = slice(h * 128, (h + 1) * 128)
                    mm(PY[:, hs], wuv16[:, h, :], obT[:, h, :], reads=[wuv16, obT], writes=[PY])
                yield
                tt("dve", ygT[:].rearrange("p a b -> p (a b)"), PY[:], Rg[:], ALU.mult, reads=[PY, Rg], writes=[ygT])
                yield
                if b == 0 and i <= 2:
                    dump(f"yg{i}", ygT[:].rearrange("p a b -> p (a b)"), ygT)
                yield
                yield
            streams = [[gdn_stream(), 3], [dsa_stream(), 1]]
            pre = pre_gen(b, i + 1) if i + 1 < NT else None
            seen = set()
            while streams or pre is not None:
                for ent in list(streams):
                    for _ in range(ent[1]):
                        try:
                            seen.add(next(ent[0]))
                        except StopIteration:
                            streams.remove(ent)
                            break
                if pre is not None and (("M" in seen and "B" in seen) or not streams):
                    for _ in range(1):
                        try:
                            next(pre)
                        except StopIteration:
                            pre = None
                            break
            xr = negmk[:].rearrange("p a b -> p (a b)").bitcast(F32)
            S.dma(xr, x_d[b, t0:t0 + 128, :], writes=[negmk])
            for nh in range(2):
                for c in range(8):
                    lhs = ogT[:, c, :] if c < 4 else ygT[:, c - 4, :]
                    mm(PT2h[nh][:], lhs, wout16[:, c, nh * 512:(nh + 1) * 512],
                       start=(c == 0), stop=(c == 7), reads=[ogT, ygT, wout16], writes=[PT2h[nh]])
            for nh in range(2):
                act(junkA[:, nh * 512:(nh + 1) * 512], PT2h[nh][:], AF.Square, reads=[PT2h[nh]],
                    writes=[junkA, msq], accum_out=msq[:, nh:nh + 1])
            tt("dve", ssq1[:], msq[:, 0:1], msq[:, 1:2], ALU.add, reads=[msq], writes=[ssq1])
            rsqrt_col(mrs, ssq1, 1, 1.0 / D, EPS)
            for nh in range(2):
                sl = slice(nh * 512, (nh + 1) * 512)
                stt(otmp[:, sl], PT2h[nh][:], mrs[:, 0:1], GATE[:, sl], ALU.mult, ALU.mult, reads=[PT2h[nh], mrs, GATE], writes=[otmp])
            tt("pool", otmp[:], otmp[:], xr, ALU.add, reads=[otmp, negmk], writes=[otmp])
            S.dma(out_d[b, t0:t0 + 128, :], otmp[:], reads=[otmp])
            tix += 1
    S.finish("sp")
    return nc, S


def _masks():
    i = np.arange(128)[:, None]; j = np.arange(128)[None, :]
    m = np.zeros((128, 8, 128), np.float32)
    for ls in range(7):
        sz = 1 << ls
        m[:, ls, :] = (((i // sz) % 2 == 1) & ((j // sz) == (i // sz) - 1)).astype(np.float32)
    m[:, 7, :] = m[:, 0, :].T
    return m


def _layout(inputs, core):
    f = lambda a: np.ascontiguousarray(a, dtype=np.float32)
    bs = slice(core * NBC, (core + 1) * NBC)
    kp = lambda w: w.reshape(8, 128, -1).transpose(1, 0, 2)
    w_in = inputs["w_in"][0]
    ik = w_in[:, O_IK:O_IK + 64]
    w_idx = np.concatenate([w_in[:, O_IQ:O_IQ + 512], ik, ik, w_in[:, O_IW:O_IW + 8]], axis=1)
    b_ada = inputs["b_ada"][0]
    return {
        "x": f(inputs["x"][bs]),
        "cT": f(inputs["c"][bs].T.reshape(8, 128, NBC).transpose(1, 0, 2)),
        "w_ada": f(kp(inputs["w_ada"][0])),
        "b_col": f(b_ada.reshape(24, 128).T),
        "b_gate": f(b_ada[2048:3072].reshape(1, 1024)),
        "g_pre": f(inputs["g_pre"][0].reshape(8, 128).T),
        "w_in": f(kp(w_in[:, :NW16])),
        "w_idx": f(kp(w_idx)),
        "conv_w": f(inputs["conv_w"][0].reshape(4, 12, 128).transpose(2, 1, 0)),
        "a_log": f(inputs["a_log"].reshape(1, 4)),
        "dt_bias": f(inputs["dt_bias"].reshape(1, 4)),
        "g_gdn": f(inputs["g_gdn"].reshape(1, 128)),
        "g_kv": f(inputs["g_kv"].reshape(1, 128)),
        "w_uv": f(inputs["w_uv"][0].transpose(1, 0, 2)),
        "rel_bias": f(inputs["rel_bias"].reshape(1, 128)),
        "w_out": f(kp(inputs["w_out"][0])),
        "g_post": f(inputs["g_post"].reshape(1, 1024)),
        "masks": _masks(),
    }


def kernel(**inputs):
    inputs = {k: np.asarray(v) for k, v in inputs.items()}
    nc, _ = build()
    in_maps = [_layout(inputs, c) for c in range(8)]
    res = run_bass_kernel_spmd(nc, in_maps, core_ids=list(range(8)))
    return np.concatenate([r["out"] for r in res.results], axis=0).astype(np.float32)
```
